# Optimizing a Trainium2 kernel written in Bass

```python
import jax, jax.numpy as jnp
from jax import lax
import numpy as np

D_MODEL = 1024
BATCH = 8
SEQ = 4096
DEPTH = 2

GRID_W = 64
CTX_LEN = 256
N_EVEN = (DEPTH + 1) // 2
N_ODD = DEPTH // 2
EPS = 1e-6
A_CH = D_MODEL // 2
CONV_K = 31
B_CH = D_MODEL // 2
B_HEADS = 8
B_HD = B_CH // B_HEADS
CHUNK = 128
IN_E = 2 * A_CH + 2 * B_CH
QK_NOPE = 128
QK_ROPE = 64
V_HD = 128
MLA_HEADS = D_MODEL // V_HD
Q_RANK = 384
KV_RANK = 256
IN_O = Q_RANK + KV_RANK + QK_ROPE
ROPE_AXIS = QK_ROPE // 2
ROPE_FREQS = ROPE_AXIS // 2
ROPE_BASE = 10000.0
Q_BLOCK = 128
ATTN_SCALE = (QK_NOPE + QK_ROPE) ** -0.5
D_FF = 2816
N_EXPERTS = 8
TOP_K = 2
D_EXPERT = 3584

kernel_name = 'hybrid_conv_gmlp_mla_moe_dit'


def _rmsnorm(x, g):
    xf = x.astype(jnp.float32)
    y = xf * lax.rsqrt(jnp.mean(xf * xf, axis=-1, keepdims=True) + EPS)
    return (y * g.astype(jnp.float32)).astype(x.dtype)


def _layernorm(x, g, b):
    xf = x.astype(jnp.float32)
    mu = jnp.mean(xf, axis=-1, keepdims=True)
    var = jnp.mean(jnp.square(xf - mu), axis=-1, keepdims=True)
    y = (xf - mu) * lax.rsqrt(var + EPS)
    return (y * g.astype(jnp.float32) + b.astype(jnp.float32)).astype(x.dtype)


def _modulate(h, shift, scale):
    return h * (1 + scale[:, None, :]) + shift[:, None, :]


def _swiglu(h, w1, w3, w2):
    return (jax.nn.silu(h @ w1) * (h @ w3)) @ w2


def _rope2d(x, cos, sin):
    xs = x.reshape(*x.shape[:-1], 2, 2, ROPE_FREQS)
    x1, x2 = xs[..., 0, :], xs[..., 1, :]
    c = cos[None, :, None].astype(x.dtype)
    s = sin[None, :, None].astype(x.dtype)
    out = jnp.stack([x1 * c - x2 * s, x1 * s + x2 * c], axis=-2)
    return out.reshape(x.shape)


def _conv_gmlp_mixer(h, w_in, conv_w, conv_b, ln_a_g, ln_a_b, ln_v_g, ln_v_b, w_s, b_s, w_out):
    b, t, _ = h.shape
    z = h @ w_in
    a_val, a_gate, u, v = jnp.split(z, 4, axis=-1)
    a = a_val * jax.nn.sigmoid(a_gate)
    a = lax.conv_general_dilated(
        a, conv_w.reshape(CONV_K, 1, A_CH).astype(a.dtype), window_strides=(1,),
        padding=[(CONV_K // 2, CONV_K // 2)], dimension_numbers=('NWC', 'WIO', 'NWC'),
        feature_group_count=A_CH) + conv_b
    a = jax.nn.silu(_layernorm(a, ln_a_g, ln_a_b))
    u = jax.nn.gelu(u)
    v = _layernorm(jax.nn.gelu(v), ln_v_g, ln_v_b).reshape(b, t // CHUNK, CHUNK, B_HEADS, B_HD)
    sv = jnp.einsum('hij,bnjhd->bnihd', w_s, v) + b_s.T[None, None, :, :, None]
    bo = u * sv.reshape(b, t, B_CH)
    return jnp.concatenate([a, bo], axis=-1) @ w_out


def _mla_q(q_lat, g_q, w_uq, rope):
    b, t, _ = q_lat.shape
    q = (_rmsnorm(q_lat, g_q) @ w_uq).reshape(b, t, MLA_HEADS, QK_NOPE + QK_ROPE)
    if rope is not None:
        q = jnp.concatenate([q[..., :QK_NOPE], _rope2d(q[..., QK_NOPE:], *rope)], axis=-1)
    return q


def _mla_kv(kv_lat, k_pe, g_kv, w_ukv, rope):
    b, t, _ = kv_lat.shape
    kv = (_rmsnorm(kv_lat, g_kv) @ w_ukv).reshape(b, t, MLA_HEADS, QK_NOPE + V_HD)
    k_pe = k_pe[:, :, None, :]
    if rope is not None:
        k_pe = _rope2d(k_pe, *rope)
    k = jnp.concatenate([kv[..., :QK_NOPE], jnp.broadcast_to(k_pe, (b, t, MLA_HEADS, QK_ROPE))], axis=-1)
    return k, kv[..., QK_NOPE:]


def _attend(q, k, v):
    b, tq, h, dq = q.shape
    nb = tq // Q_BLOCK
    qb = jnp.moveaxis(q.reshape(b, nb, Q_BLOCK, h, dq), 1, 0)

    def block(qblk):
        s = jnp.einsum('bqhd,bkhd->bhqk', qblk, k).astype(jnp.float32) * ATTN_SCALE
        p = jax.nn.softmax(s, axis=-1).astype(v.dtype)
        return jnp.einsum('bhqk,bkhd->bqhd', p, v)

    o = lax.map(block, qb)
    return jnp.moveaxis(o, 0, 1).reshape(b, tq, h * v.shape[-1])


def _moe(h, router, w1, w3, w2):
    b, t, d = h.shape
    hf = h.reshape(b * t, d)
    logits = (hf @ router).astype(jnp.float32)
    top_v, top_i = lax.top_k(logits, TOP_K)
    top_w = jax.nn.softmax(top_v, axis=-1)
    gates = jnp.sum(jax.nn.one_hot(top_i, N_EXPERTS, dtype=jnp.float32) * top_w[..., None], axis=-2).astype(h.dtype)
    y = jnp.zeros_like(hf)
    for e in range(N_EXPERTS):
        y = y + gates[:, e:e + 1] * _swiglu(hf, w1[e], w3[e], w2[e])
    return y.reshape(b, t, d)


def setup_inputs(seed: int = 0) -> dict:
    key = jax.random.key(seed)
    ks = iter(jax.random.split(key, 64))

    def nrm(shape, s=1.0):
        return jax.random.normal(next(ks), shape, jnp.float32) * s

    def gain(shape):
        return 1.0 + nrm(shape, 0.02)

    D = D_MODEL
    return {
        'x': nrm((BATCH, SEQ, D)),
        'c': nrm((BATCH, D)),
        'ctx': nrm((BATCH, CTX_LEN, D)),
        'c_ctx': nrm((D,)),
        'w_ada': nrm((DEPTH, D, 6 * D), 0.5 * D ** -0.5),
        'b_ada': nrm((DEPTH, 6 * D), 0.02),
        'norm_g': gain((DEPTH, 2, D)),
        'e_w_in': nrm((N_EVEN, D, IN_E), D ** -0.5),
        'e_conv_w': nrm((N_EVEN, CONV_K, A_CH), CONV_K ** -0.5),
        'e_conv_b': nrm((N_EVEN, A_CH), 0.02),
        'e_ln_a_g': gain((N_EVEN, A_CH)),
        'e_ln_a_b': nrm((N_EVEN, A_CH), 0.02),
        'e_ln_v_g': gain((N_EVEN, B_CH)),
        'e_ln_v_b': nrm((N_EVEN, B_CH), 0.02),
        'e_w_s': nrm((N_EVEN, B_HEADS, CHUNK, CHUNK), CHUNK ** -0.5),
        'e_b_s': gain((N_EVEN, B_HEADS, CHUNK)),
        'e_w_out': nrm((N_EVEN, A_CH + B_CH, D), (A_CH + B_CH) ** -0.5),
        'e_ffn_w1': nrm((N_EVEN, D, D_FF), D ** -0.5),
        'e_ffn_w3': nrm((N_EVEN, D, D_FF), D ** -0.5),
        'e_ffn_w2': nrm((N_EVEN, D_FF, D), D_FF ** -0.5),
        'o_w_in': nrm((N_ODD, D, IN_O), D ** -0.5),
        'o_g_q': gain((N_ODD, Q_RANK)),
        'o_g_kv': gain((N_ODD, KV_RANK)),
        'o_w_uq': nrm((N_ODD, Q_RANK, MLA_HEADS * (QK_NOPE + QK_ROPE)), Q_RANK ** -0.5),
        'o_w_ukv': nrm((N_ODD, KV_RANK, MLA_HEADS * (QK_NOPE + V_HD)), KV_RANK ** -0.5),
        'o_w_o': nrm((N_ODD, MLA_HEADS * V_HD, D), (MLA_HEADS * V_HD) ** -0.5),
        'o_router': nrm((N_ODD, D, N_EXPERTS), D ** -0.5),
        'o_exp_w1': nrm((N_ODD, N_EXPERTS, D, D_EXPERT), D ** -0.5),
        'o_exp_w3': nrm((N_ODD, N_EXPERTS, D, D_EXPERT), D ** -0.5),
        'o_exp_w2': nrm((N_ODD, N_EXPERTS, D_EXPERT, D), D_EXPERT ** -0.5),
        'final_g': gain((D,)),
    }


def reference(x, c, ctx, c_ctx, w_ada, b_ada, norm_g,
              e_w_in, e_conv_w, e_conv_b, e_ln_a_g, e_ln_a_b, e_ln_v_g, e_ln_v_b,
              e_w_s, e_b_s, e_w_out, e_ffn_w1, e_ffn_w3, e_ffn_w2,
              o_w_in, o_g_q, o_g_kv, o_w_uq, o_w_ukv, o_w_o,
              o_router, o_exp_w1, o_exp_w3, o_exp_w2, final_g):
    b, t, _ = x.shape
    rows = t // GRID_W
    row = jnp.repeat(jnp.arange(rows), GRID_W).astype(jnp.float32)
    col = jnp.tile(jnp.arange(GRID_W), rows).astype(jnp.float32)
    inv_freq = ROPE_BASE ** (-jnp.arange(ROPE_FREQS, dtype=jnp.float32) / ROPE_FREQS)
    ang = jnp.stack([row[:, None] * inv_freq, col[:, None] * inv_freq], axis=1)
    rope = (jnp.cos(ang), jnp.sin(ang))

    silu_c = jax.nn.silu(c)
    silu_cc = jax.nn.silu(c_ctx)[None]

    for i in range(DEPTH):
        last = i == DEPTH - 1
        j = i // 2
        mx = jnp.split(silu_c @ w_ada[i] + b_ada[i], 6, axis=-1)
        mc = jnp.split(silu_cc @ w_ada[i] + b_ada[i], 6, axis=-1)
        hx = _modulate(_rmsnorm(x, norm_g[i, 0]), mx[0], mx[1])
        hc = _modulate(_rmsnorm(ctx, norm_g[i, 0]), mc[0], mc[1])

        if i % 2 == 0:
            params = (e_w_in[j], e_conv_w[j], e_conv_b[j], e_ln_a_g[j], e_ln_a_b[j],
                      e_ln_v_g[j], e_ln_v_b[j], e_w_s[j], e_b_s[j], e_w_out[j])
            x = x + mx[2][:, None] * _conv_gmlp_mixer(hx, *params)
            if not last:
                ctx = ctx + mc[2][:, None] * _conv_gmlp_mixer(hc, *params)
        else:
            zx = hx @ o_w_in[j]
            q_lat_x, kv_lat_x, kpe_x = jnp.split(zx, [Q_RANK, Q_RANK + KV_RANK], axis=-1)
            if last:
                zc = hc @ o_w_in[j][:, Q_RANK:]
                kv_lat_c, kpe_c = jnp.split(zc, [KV_RANK], axis=-1)
            else:
                zc = hc @ o_w_in[j]
                q_lat_c, kv_lat_c, kpe_c = jnp.split(zc, [Q_RANK, Q_RANK + KV_RANK], axis=-1)
            k_c, v_c = _mla_kv(kv_lat_c, kpe_c, o_g_kv[j], o_w_ukv[j], None)
            k_x, v_x = _mla_kv(kv_lat_x, kpe_x, o_g_kv[j], o_w_ukv[j], rope)
            q_x = _mla_q(q_lat_x, o_g_q[j], o_w_uq[j], rope)
            att_x = _attend(q_x, jnp.concatenate([k_c, k_x], axis=1), jnp.concatenate([v_c, v_x], axis=1))
            x = x + mx[2][:, None] * (att_x @ o_w_o[j])
            if not last:
                q_c = _mla_q(q_lat_c, o_g_q[j], o_w_uq[j], None)
                att_c = _attend(q_c, k_c, v_c)
                ctx = ctx + mc[2][:, None] * (att_c @ o_w_o[j])

        if i % 2 == 0:
            ffn = lambda h: _swiglu(h, e_ffn_w1[j], e_ffn_w3[j], e_ffn_w2[j])
        else:
            ffn = lambda h: _moe(h, o_router[j], o_exp_w1[j], o_exp_w3[j], o_exp_w2[j])
        hx = _modulate(_rmsnorm(x, norm_g[i, 1]), mx[3], mx[4])
        x = x + mx[5][:, None] * ffn(hx)
        if not last:
            hc = _modulate(_rmsnorm(ctx, norm_g[i, 1]), mc[3], mc[4])
            ctx = ctx + mc[5][:, None] * ffn(hc)

    return _rmsnorm(x, final_g)
```

```python
import numpy as np
from contextlib import ExitStack
import concourse.bass as bass
import concourse.mybir as mybir
from concourse.bass_utils import run_bass_kernel_spmd

F32 = mybir.dt.float32
BF16 = mybir.dt.bfloat16
AF = mybir.ActivationFunctionType
ALU = mybir.AluOpType

T, TC, D, KD = 4096, 256, 1024, 8
EPS = 1e-6
NCORES = 8
SEM_LIMIT = 30000
DEBUG = False
DBG_NAMES = []
DBG_OUT = {}


class Buf:
    __slots__ = ("name", "w", "r", "dsem", "dcnt")

    def __init__(self, name):
        self.name = name
        self.w = {}
        self.r = {}
        self.dsem = None
        self.dcnt = 0


class Sched:
    def __init__(self, nc, ctx):
        self.nc = nc
        self.ctx = ctx
        self.prog = {e: [] for e in ("tensor", "vector", "scalar", "gpsimd", "sync")}
        self.esem = {}
        self.ecnt = {}
        self.nsem = 0
        self.allsems = {}
        for e in ("tensor", "vector", "scalar", "gpsimd"):
            self._new_esem(e)
        self.seen = {e: {} for e in self.prog}
        self.n_inst = 0
        self.n_wait = 0

    def _alloc_sem(self, name):
        self.nsem += 1
        s = self.ctx.enter_context(self.nc.semaphore(f"{name}_{self.nsem}"))
        self.allsems[id(s)] = [s, 0]
        return s

    def _new_esem(self, e):
        self.esem[e] = self._alloc_sem(f"e_{e}")
        self.ecnt[e] = 0

    def buf(self, name):
        return Buf(name)

    def bufs(self, name, n):
        return [Buf(f"{name}{i}") for i in range(n)]

    def _need(self, eng, toks):
        out = {}
        for (sem, val, teng) in toks:
            if teng == eng and eng == "tensor":
                continue
            k = id(sem)
            if self.seen[eng].get(k, 0) >= val:
                continue
            if k not in out or out[k][1] < val:
                out[k] = (sem, val)
        for k, (sem, val) in out.items():
            self.seen[eng][k] = val
        return list(out.values())

    def _deps(self, eng, reads, writes, join=False):
        toks = []
        for b in reads:
            toks += b.w.values()
        for b in writes:
            if not join:
                toks += b.w.values()
            toks += b.r.values()
        return self._need(eng, toks)

    def _record(self, tok, reads, writes, join=False):
        k = id(tok[0])
        self.allsems[k][1] = max(self.allsems[k][1], tok[1])
        for b in reads:
            b.r[k] = tok
        for b in writes:
            if join:
                b.w[k] = tok
            else:
                b.w = {k: tok}
                b.r = {}

    def op(self, eng, fn, reads=(), writes=()):
        reads = [b for b in reads if b is not None]
        writes = [b for b in writes if b is not None]
        waits = self._deps(eng, reads, writes)
        if self.ecnt[eng] >= SEM_LIMIT:
            self._new_esem(eng)
        sem = self.esem[eng]
        self.ecnt[eng] += 1
        tok = (sem, self.ecnt[eng], eng)
        self._record(tok, reads, writes)
        self.n_inst += 1
        self.n_wait += len(waits)

        def run(e, waits=waits, fn=fn, sem=sem):
            for (s, v) in waits:
                e.wait_ge(s, v)
            fn(e).then_inc(sem, 1)
        self.prog[eng].append(run)

    def dma(self, queue, out, in_, reads=(), writes=(), join=False, **kw):
        reads = [b for b in reads if b is not None]
        writes = [b for b in writes if b is not None]
        waits = self._deps(queue, reads, writes, join)
        sb = writes[0] if writes else reads[0]
        if sb.dsem is None:
            sb.dsem = self._alloc_sem("d")
        sb.dcnt += 16
        tok = (sb.dsem, sb.dcnt, "dma")
        self._record(tok, reads, writes, join)
        self.n_inst += 1
        self.n_wait += len(waits)

        def run(e, waits=waits, sem=sb.dsem):
            for (s, v) in waits:
                e.wait_ge(s, v)
            e.dma_start(out=out, in_=in_, **kw).then_inc(sem, 16)
        self.prog[queue].append(run)

    def dma_fn(self, queue, fn, reads=(), writes=(), join=False):
        reads = [b for b in reads if b is not None]
        writes = [b for b in writes if b is not None]
        waits = self._deps(queue, reads, writes, join)
        sb = writes[0] if writes else reads[0]
        if sb.dsem is None:
            sb.dsem = self._alloc_sem("d")
        sb.dcnt += 16
        tok = (sb.dsem, sb.dcnt, "dma")
        self._record(tok, reads, writes, join)
        self.n_inst += 1

        def run(e, waits=waits, sem=sb.dsem, fn=fn):
            for (s, v) in waits:
                e.wait_ge(s, v)
            fn(e).then_inc(sem, 16)
        self.prog[queue].append(run)

    def barrier(self):
        toks = [(s, v, "x") for (s, v) in self.allsems.values() if v > 0]
        for eng in self.prog:
            waits = self._need(eng, toks)

            def run(e, waits=waits):
                for (s, v) in waits:
                    e.wait_ge(s, v)
            self.prog[eng].append(run)

    def emit(self):
        with self.nc.Block() as block:
            @block.sync
            def _(e):
                for f in self.prog["sync"]:
                    f(e)

            @block.tensor
            def _(e):
                for f in self.prog["tensor"]:
                    f(e)

            @block.vector
            def _(e):
                for f in self.prog["vector"]:
                    f(e)

            @block.scalar
            def _(e):
                for f in self.prog["scalar"]:
                    f(e)

            @block.gpsimd
            def _(e):
                for f in self.prog["gpsimd"]:
                    f(e)


class Rot:
    def __init__(self, S, alloc, name, k, shape, dt):
        self.t = [alloc(f"{name}{i}", shape, dt) for i in range(k)]
        self.b = S.bufs(name, k)
        self.i = 0

    def next(self):
        i = self.i
        self.i = (i + 1) % len(self.t)
        return self.t[i], self.b[i]


VOFF = {}


def _vlayout():
    off = 0
    for name, n in [("sc", 16), ("bada", 192), ("ng", 64), ("convw", 124), ("convb", 4), ("lnag", 4),
                    ("lnab", 4), ("gq", 3), ("gkv", 2), ("fg", 8)]:
        VOFF[name] = off
        off += n
    return off


NV = _vlayout()


def build(stop_after="D"):
    nc = bass.Bass("TRN2", target_bir_lowering=False)

    def din(name, shape):
        return nc.dram_tensor(name, list(shape), F32, kind="ExternalInput").ap()

    xT_in = din("xT", [D, T])
    cT_in = din("cT", [D, TC])
    vecs_in = din("vecs", [128, NV])
    ident_in = din("ident", [128, 128])
    lnv_in = din("lnv", [128, 2, 512])
    bs_in = din("bs", [128, 4, 128])
    wsT_in = din("wsT", [128, 8, 128])
    rope_in = din("rope", [128, 2, T])
    sel_in = din("sel", [8, 8, 128])
    w_ada = din("w_ada", [2, D, 6 * D])
    e_w_in = din("e_w_in", [D, 2048])
    e_w_out = din("e_w_out", [D, D])
    f_w1 = din("e_ffn_w1", [D, 2816])
    f_w3 = din("e_ffn_w3", [D, 2816])
    f_w2 = din("e_ffn_w2", [2816, D])
    o_w_in = din("o_w_in_x", [D, 896])
    o_wuq = din("o_wuq", [384, 2048])
    o_wukv = din("o_wukv", [256, 2048])
    o_w_o = din("o_w_o", [D, D])
    o_router = din("o_router", [D, 8])
    NROW = 8 * 7 * 128 * 2
    x_w1 = din("xw1r", [NROW, 2048])
    x_w3 = din("xw3r", [NROW, 2048])
    x_w2 = din("xw2r", [NROW, 2048])
    utri_in = din("utri", [128, 128])
    tokid_in = nc.dram_tensor("tokid", [128, 64, 16], mybir.dt.int32, kind="ExternalInput").ap()
    oobfill_in = nc.dram_tensor("oobfill", [128, 96 * 16], mybir.dt.int32, kind="ExternalInput").ap()
    slot2tok = nc.dram_tensor("slot2tok", [24 * 512, 16], mybir.dt.int32).ap()
    ytok = nc.dram_tensor("ytok", [2 * T + 24 * 512, D], F32).ap()
    ibase_in = din("ibase", [128, 7])
    NS = 24 * 512
    hsort = nc.dram_tensor("hsort", [NS, D], BF16).ap()
    ysort = nc.dram_tensor("ysort", [NS, D], F32).ap()
    outT = nc.dram_tensor("outT", [D, T], F32, kind="ExternalOutput").ap()
    xs = nc.dram_tensor("xs", [D, T], F32).ap()
    cs = nc.dram_tensor("cs", [D, TC], F32).ap()

    def dview(ap):
        return ap.rearrange("(j p) n -> p j n", p=128)

    with ExitStack() as ctx:
        S = Sched(nc, ctx)

        uid = [0]

        def dbg_dump(name, ap, shape, reads, dt=F32):
            if not DEBUG:
                return
            t = nc.dram_tensor("dbg_" + name, list(shape), dt, kind="ExternalOutput").ap()
            S.dma("sync", t, ap, reads=reads)
            DBG_NAMES.append("dbg_" + name)

        def mk_alloc(stack):
            def alloc(name, shape, dt=F32):
                uid[0] += 1
                return stack.enter_context(nc.sbuf_tensor(f"sb{uid[0]}_{name}", list(shape), dt))
            return alloc
        galloc = mk_alloc(ctx)

        banks = [ctx.enter_context(nc.psum_tensor(f"pb{i}", [128, 512], F32)) for i in range(8)]
        bank_b = S.bufs("pb", 8)
        bstate = {"i": 0}

        def bank(subset=None):
            if subset is None:
                i = bstate["i"]
                bstate["i"] = (i + 1) % 8
            else:
                i = subset[bstate.setdefault(id(subset), 0) % len(subset)]
                bstate[id(subset)] += 1
            return banks[i], bank_b[i]

        def mm(out, lhsT, rhs, start, stop, reads, wb):
            S.op("tensor", lambda e: e.matmul(out, lhsT=lhsT, rhs=rhs, start=start, stop=stop), reads=reads, writes=[wb])

        def act(out, in_, func, reads, writes, **kw):
            S.op("scalar", lambda e: e.activation(out=out, in_=in_, func=func, **kw), reads=reads, writes=writes)

        def tt(out, a, b, op, reads, writes, eng="vector"):
            S.op(eng, lambda e: e.tensor_tensor(out=out, in0=a, in1=b, op=op), reads=reads, writes=writes)

        def stt(out, in0, scalar, in1, op0, op1, reads, writes, eng="vector"):
            S.op(eng, lambda e: e.scalar_tensor_tensor(out=out, in0=in0, scalar=scalar, in1=in1, op0=op0, op1=op1),
                 reads=reads, writes=writes)

        def ts(out, in0, s1, s2, op0, op1, reads, writes, eng="vector"):
            if s2 is None:
                S.op(eng, lambda e: e.tensor_scalar(out=out, in0=in0, scalar1=s1, scalar2=None, op0=op0), reads=reads, writes=writes)
            else:
                S.op(eng, lambda e: e.tensor_scalar(out=out, in0=in0, scalar1=s1, scalar2=s2, op0=op0, op1=op1),
                     reads=reads, writes=writes)

        def vcopy(out, in_, reads, writes, eng="vector"):
            S.op(eng, lambda e: e.tensor_copy(out=out, in_=in_), reads=reads, writes=writes)

        def recip(out, in_, reads, writes):
            S.op("vector", lambda e: e.reciprocal(out=out, in_=in_), reads=reads, writes=writes)

        def memset(ap, val, writes):
            S.op("vector", lambda e: e.memset(ap, val), writes=writes)

        vecs = galloc("vecs", [128, NV]); Bvecs = S.buf("vecs")
        ident = galloc("ident", [128, 128]); Bident = S.buf("ident")
        ones = galloc("ones", [128, 128]); Bones = S.buf("ones")
        onesb = galloc("onesb", [128, 128], BF16); Bonesb = S.buf("onesb")
        epsc = galloc("epsc", [128, 1]); Beps = S.buf("eps")
        modT = galloc("modT", [128, 2 * 6 * 8 * 2]); Bmod = S.buf("modT")
        Gt = galloc("Gt", [128, 2 * 2 * 8 * 2]); BG = S.buf("Gt")
        scs = galloc("scs", [128, 16]); Bscs = S.buf("scs")
        S.dma("sync", vecs[:], vecs_in, writes=[Bvecs])
        S.dma("sync", ident[:], ident_in, writes=[Bident])
        memset(ones[:], 1.0, [Bones])
        memset(onesb[:], 1.0, [Bonesb])
        memset(epsc[:], EPS, [Beps])

        def vv(name, idx, n=1):
            o = VOFF[name] + idx
            return vecs[:, o:o + n]

        def mod(i, v, j, col):
            o = ((i * 6 + v) * 8 + j) * 2 + col
            return modT[:, o:o + 1]

        def Gs(i, n, j, col):
            o = ((i * 2 + n) * 8 + j) * 2 + col
            return Gt[:, o:o + 1]

        with ExitStack() as px:
            alloc = mk_alloc(px)
            wa = Rot(S, alloc, "wa", 2, [128, 8, 1024], F32)
            act(scs[:], vv("sc", 0, 16), AF.Silu, [Bvecs], [Bscs])
            for i in range(2):
                wv = w_ada[i].rearrange("(k p) n -> p k n", p=128)
                for v in range(6):
                    wt, wb = wa.next()
                    S.dma("sync", wt[:], wv[:, :, v * 1024:(v + 1) * 1024], writes=[wb])
                    pb, pbb = bank()
                    for j in range(8):
                        for k in range(8):
                            mm(pb[:, j * 2:j * 2 + 2], wt[:, k, j * 128:(j + 1) * 128], scs[:, k * 2:k * 2 + 2],
                               k == 0, k == 7, [wb, Bscs], pbb)
                    o = (i * 6 + v) * 16
                    tt(modT[:, o:o + 16], pb[:, 0:16], vecs[:, VOFF["bada"] + o:VOFF["bada"] + o + 16], ALU.add,
                       [pbb, Bvecs], [Bmod])
            for i in range(2):
                for n in range(2):
                    vs = 1 if n == 0 else 4
                    o = (i * 2 + n) * 16
                    om = (i * 6 + vs) * 16
                    stt(Gt[:, o:o + 16], modT[:, om:om + 16], 1.0, vecs[:, VOFF["ng"] + o:VOFF["ng"] + o + 16],
                        ALU.add, ALU.mult, [Bmod, Bvecs], [BG])
        S.barrier()

        def rms_stat(xt, Bx, n, tmps, nchunks, inv_d):
            sqr, rst = tmps
            pb, pbb = bank()
            for j in range(nchunks):
                sq, sqb = sqr.next()
                act(sq[:, :n], xt(j), AF.Square, [Bx(j) if callable(Bx) else Bx], [sqb])
                mm(pb[:, :n], ones[:], sq[:, :n], j == 0, j == nchunks - 1, [Bones, sqb], pbb)
            r1, r1b = rst.next()
            act(r1[:, :n], pb[:, :n], AF.Sqrt, [pbb, Beps], [r1b], bias=epsc[:], scale=inv_d)
            r2, r2b = rst.next()
            recip(r2[:, :n], r1[:, :n], [r1b], [r2b])
            return r2, r2b

        def norm_mod(xblk, Bx, n, i, nn, col, hT, Bh, tmps, h32=None, Bh32=None):
            sqr, rst, tmr = tmps
            r, rb = rms_stat(lambda j: xblk[:, j, :n], Bx, n, (sqr, rst), 8, 1.0 / D)
            vsh = 0 if nn == 0 else 3
            for j in range(8):
                tm, tmb = tmr.next()
                stt(tm[:, :n], xblk[:, j, :n], Gs(i, nn, j, col), r[:, :n], ALU.mult, ALU.mult, [Bx, BG, rb], [tmb])
                if h32 is not None:
                    act(h32[:, j, :n], tm[:, :n], AF.Identity, [tmb, Bmod], [Bh32[j]], bias=mod(i, vsh, j, col), scale=1.0)
                    if hT is not None:
                        vcopy(hT[:, j, :n], h32[:, j, :n], [Bh32[j]], [Bh[j]], eng="gpsimd")
                else:
                    act(hT[:, j, :n], tm[:, :n], AF.Identity, [tmb, Bmod], [Bh[j]], bias=mod(i, vsh, j, col), scale=1.0)

        def load_w_bf(dst, dst_b, src_view, nparts=1, axis=1):
            S.dma("gpsimd", dst, src_view, writes=[dst_b])

        def phase_A():
            with ExitStack() as px:
                alloc = mk_alloc(px)
                win = alloc("win", [128, 8, 2048], BF16); Bwin = S.buf("win")
                wout = alloc("wout", [128, 8, 1024], BF16); Bwout = S.buf("wout")
                wsT = alloc("wsT", [128, 8, 128], BF16); BwsT = S.buf("wsT")
                lnv = alloc("lnv", [128, 2, 512]); Blnv = S.buf("lnv")
                bsb = alloc("bsb", [128, 4, 128]); Bbs = S.buf("bs")
                a_pad = alloc("a_pad", [128, 4, T + 30], BF16); Bap = S.bufs("ap", 4)
                xr = Rot(S, alloc, "xa", 2, [128, 8, 512], F32)
                hT = alloc("hT", [128, 8, 512], BF16); Bh = S.bufs("h", 8)
                uT = alloc("uT", [128, 4, 512], BF16); Bu = S.bufs("u", 4)
                vtm = alloc("vtm", [128, 4, 512], BF16); Bv = S.bufs("v", 4)
                ac = alloc("ac", [128, 4, 512]); Bac = S.bufs("ac", 4)
                a2T = alloc("a2T", [128, 4, 512], BF16); Ba2 = S.bufs("a2", 4)
                boT = alloc("boT", [128, 4, 512], BF16); Bbo = S.bufs("bo", 4)
                sqr = Rot(S, alloc, "sqa", 2, [128, 512], F32)
                rst = Rot(S, alloc, "rsa", 4, [128, 512], F32)
                tmr = Rot(S, alloc, "tma", 3, [128, 512], F32)
                diag = alloc("diag", [128, 4, 31, 128], BF16); Bdiag = S.buf("diag")
                for c in range(4):
                    for k in range(31):
                        ts(diag[:, c, k, :], ident[:], vv("convw", c * 31 + k), None, ALU.mult, None, [Bident, Bvecs, Bdiag], [Bdiag])
                smr = Rot(S, alloc, "sma", 8, [128, 8], F32)
                tmps = (sqr, rst, tmr)
                S.dma("gpsimd", win[:], e_w_in.rearrange("(k p) n -> p k n", p=128), writes=[Bwin])
                S.dma("gpsimd", wout[:], e_w_out.rearrange("(k p) n -> p k n", p=128), writes=[Bwout])
                S.dma("gpsimd", wsT[:], wsT_in, writes=[BwsT])
                S.dma("sync", lnv[:], lnv_in, writes=[Blnv])
                S.dma("sync", bsb[:], bs_in, writes=[Bbs])

                def seq(src, dst, dst_bufs, ntok, col):
                    nb = min(512, ntok)
                    nblk = ntok // nb
                    sv, dv = dview(src), dview(dst)
                    for c in range(4):
                        memset(a_pad[:, c, :], 0.0, [Bap[c]])
                    xq = {}

                    def xload(tb):
                        if tb < nblk:
                            xb_, xbb_ = xr.next()
                            S.dma("sync", xb_[:, :, :nb], sv[:, :, tb * nb:(tb + 1) * nb], writes=[xbb_])
                            xq[tb] = (xb_, xbb_)
                    xload(0)
                    for tb in range(nblk):
                        xload(tb + 1)
                        xb, xbb = xq.pop(tb)
                        norm_mod(xb, xbb, nb, 0, 0, col, hT, Bh, tmps)
                        for c in range(4):
                            pv, pvb = bank()
                            pg, pgb = bank()
                            for k in range(8):
                                mm(pv[:, :nb], win[:, k, c * 128:(c + 1) * 128], hT[:, k, :nb], k == 0, k == 7, [Bwin, Bh[k]], pvb)
                            for k in range(8):
                                mm(pg[:, :nb], win[:, k, 512 + c * 128:512 + (c + 1) * 128], hT[:, k, :nb], k == 0, k == 7,
                                   [Bwin, Bh[k]], pgb)
                            sg, sgb = tmr.next()
                            act(sg[:, :nb], pg[:, :nb], AF.Sigmoid, [pgb], [sgb])
                            tt(a_pad[:, c, 15 + tb * nb:15 + (tb + 1) * nb], pv[:, :nb], sg[:, :nb], ALU.mult, [pvb, sgb], [Bap[c]])
                    xload(0)
                    for tb in range(nblk):
                        xload(tb + 1)
                        xb, xbb = xq.pop(tb)
                        norm_mod(xb, xbb, nb, 0, 0, col, hT, Bh, tmps)
                        for c in range(4):
                            pu, pub = bank()
                            for k in range(8):
                                mm(pu[:, :nb], win[:, k, 1024 + c * 128:1024 + (c + 1) * 128], hT[:, k, :nb], k == 0, k == 7,
                                   [Bwin, Bh[k]], pub)
                            act(uT[:, c, :nb], pu[:, :nb], AF.Gelu_apprx_tanh, [pub], [Bu[c]])
                        for t4 in range(nb // 128):
                            pv, pvb = bank()
                            for k in range(8):
                                mm(pv[:, :], hT[:, k, t4 * 128:(t4 + 1) * 128], win[:, k, 1536:2048], k == 0, k == 7, [Bh[k], Bwin], pvb)
                            gv, gvb = tmr.next()
                            act(gv[:, :], pv[:, :], AF.Gelu_apprx_tanh, [pvb], [gvb])
                            st, stb = smr.next()
                            S.op("vector", lambda e, st=st, gv=gv: e.bn_stats(out=st[:, 0:6], in_=gv[:, :]), reads=[gvb], writes=[stb])
                            mv, mvb = smr.next()
                            S.op("vector", lambda e, st=st, mv=mv: e.bn_aggr(out=mv[:, 0:2], in_=st[:, 0:6]), reads=[stb], writes=[mvb])
                            sd, sdb = smr.next()
                            act(sd[:, 0:1], mv[:, 1:2], AF.Sqrt, [mvb, Beps], [sdb], bias=epsc[:], scale=1.0)
                            rs, rsb = smr.next()
                            recip(rs[:, 0:1], sd[:, 0:1], [sdb], [rsb])
                            g2, g2b = tmr.next()
                            ts(g2[:, :], gv[:, :], mv[:, 0:1], rs[:, 0:1], ALU.subtract, ALU.mult, [gvb, mvb, rsb], [g2b])
                            g3, g3b = tmr.next()
                            tt(g3[:, :], g2[:, :], lnv[:, 0, :], ALU.mult, [g2b, Blnv], [g3b], eng="gpsimd")
                            tt(vtm[:, t4, :], g3[:, :], lnv[:, 1, :], ALU.add, [g3b, Blnv], [Bv[t4]], eng="gpsimd")
                        for c in range(4):
                            base = tb * nb
                            pcv_, pcvb_ = bank()
                            for k in range(31):
                                mm(pcv_[:, :nb], diag[:, c, k, :], a_pad[:, c, base + k:base + k + nb], k == 0, k == 30, [Bdiag, Bap[c]], pcvb_)
                            act(ac[:, c, :nb], pcv_[:, :nb], AF.Identity, [pcvb_, Bvecs], [Bac[c]], bias=vv("convb", c), scale=1.0)
                        p1, p1b = bank()
                        p2, p2b = bank()
                        for c in range(4):
                            mm(p1[:, :nb], ones[:], ac[:, c, :nb], c == 0, c == 3, [Bones, Bac[c]], p1b)
                        for c in range(4):
                            sq, sqb = sqr.next()
                            act(sq[:, :nb], ac[:, c, :nb], AF.Square, [Bac[c]], [sqb])
                            mm(p2[:, :nb], ones[:], sq[:, :nb], c == 0, c == 3, [Bones, sqb], p2b)
                        mean, meanb = rst.next()
                        act(mean[:, :nb], p1[:, :nb], AF.Copy, [p1b], [meanb], scale=1.0 / 512)
                        msq, msqb = rst.next()
                        tt(msq[:, :nb], mean[:, :nb], mean[:, :nb], ALU.mult, [meanb], [msqb])
                        var, varb = rst.next()
                        stt(var[:, :nb], p2[:, :nb], 1.0 / 512, msq[:, :nb], ALU.mult, ALU.subtract, [p2b, msqb], [varb])
                        sd, sdb = tmr.next()
                        act(sd[:, :nb], var[:, :nb], AF.Sqrt, [varb, Beps], [sdb], bias=epsc[:], scale=1.0)
                        rs, rsb = rst.next()
                        recip(rs[:, :nb], sd[:, :nb], [sdb], [rsb])
                        for c in range(4):
                            y1, y1b = tmr.next()
                            tt(y1[:, :nb], ac[:, c, :nb], mean[:, :nb], ALU.subtract, [Bac[c], meanb], [y1b])
                            y2, y2b = tmr.next()
                            tt(y2[:, :nb], y1[:, :nb], rs[:, :nb], ALU.mult, [y1b, rsb], [y2b])
                            act(a2T[:, c, :nb], y2[:, :nb], AF.Silu, [y2b, Bvecs], [Ba2[c]], bias=vv("lnab", c), scale=vv("lnag", c))
                        for t4 in range(nb // 128):
                            ps_, psb = bank()
                            for jp in range(4):
                                for hh in range(2):
                                    h = 2 * jp + hh
                                    mm(ps_[hh * 64:(hh + 1) * 64, jp * 128:(jp + 1) * 128], vtm[:, t4, h * 64:(h + 1) * 64], wsT[:, h, :],
                                       True, True, [Bv[t4], BwsT], psb)
                            sv1, sv1b = tmr.next()
                            tt(sv1[:, :], ps_[:, :], bsb[:].rearrange("p a b -> p (a b)"), ALU.add, [psb, Bbs], [sv1b])
                            tt(boT[:, :, t4 * 128:(t4 + 1) * 128], sv1[:, :].rearrange("p (a b) -> p a b", a=4),
                               uT[:, :, t4 * 128:(t4 + 1) * 128], ALU.mult, [sv1b] + Bu, Bbo)
                        xn, xnb = xb, xbb
                        for dm in range(8):
                            po, pob = bank()
                            for c in range(4):
                                mm(po[:, :nb], wout[:, c, dm * 128:(dm + 1) * 128], a2T[:, c, :nb], c == 0, False, [Bwout, Ba2[c]], pob)
                            for c in range(4):
                                mm(po[:, :nb], wout[:, 4 + c, dm * 128:(dm + 1) * 128], boT[:, c, :nb], False, c == 3, [Bwout, Bbo[c]], pob)
                            stt(xn[:, dm, :nb], po[:, :nb], mod(0, 2, dm, col), xb[:, dm, :nb], ALU.mult, ALU.add,
                                [pob, Bmod, xbb], [xnb])
                        S.dma("sync", dv[:, :, tb * nb:(tb + 1) * nb], xn[:, :, :nb], reads=[xnb], writes=[dst_bufs[tb]])

                seq(cT_in, cs, Bcs, TC, 1)
                seq(xT_in, xs, Bxs, T, 0)

        def ffn_engine(alloc):
            st = {}
            st["w1"] = Rot(S, alloc, "w1g", 3, [128, 8, 512], BF16)
            st["w3"] = Rot(S, alloc, "w3g", 3, [128, 8, 512], BF16)
            st["w2"] = Rot(S, alloc, "w2g", 3, [128, 4, 1024], BF16)
            st["g"] = Rot(S, alloc, "gg", 8, [128, 512], BF16)
            st["s1"] = Rot(S, alloc, "s1", 3, [128, 512], F32)
            st["t2"] = Rot(S, alloc, "t2", 3, [128, 512], F32)
            return st

        def static_loader(w1, w3, w2):
            w1v = w1.rearrange("(k p) f -> p k f", p=128)
            w3v = w3.rearrange("(k p) f -> p k f", p=128)

            def load(f0, gs, w1t, w1b, w3t, w3b, w2t, w2b):
                S.dma("gpsimd", w1t[:, :, :gs * 128], w1v[:, :, f0 * 128:(f0 + gs) * 128], writes=[w1b])
                S.dma("gpsimd", w3t[:, :, :gs * 128], w3v[:, :, f0 * 128:(f0 + gs) * 128], writes=[w3b])
                S.dma("gpsimd", w2t[:, :gs, :], w2[f0 * 128:(f0 + gs) * 128, :].rearrange("(f p) d -> p f d", p=128), writes=[w2b])
            return load

        def ffn_run(st, hT, Bh, n, loaders, nchunks, gb, yacc, By, next_loader=None):
            groups = []
            f0 = 0
            while f0 < nchunks:
                gs = min(4, nchunks - f0)
                groups.append((f0, gs))
                f0 += gs
            items = [(ei, ld, f0, gs) for ei, ld in enumerate(loaders) for (f0, gs) in groups]
            nI = len(items)
            wt = {}

            def issue_load(i):
                ei, ld, f0, gs = items[i]
                w1t, w1b = st["w1"].next()
                w3t, w3b = st["w3"].next()
                w2t, w2b = st["w2"].next()
                ld(f0, gs, w1t, w1b, w3t, w3b, w2t, w2b)
                wt[i] = (w1t, w1b, w3t, w3b, w2t, w2b)

            def up(i):
                ei, ld, f0, gs = items[i]
                w1t, w1b, w3t, w3b, w2t, w2b = wt[i]
                gts = []
                for fi in range(gs):
                    pa, pab = bank()
                    pb_, pbb = bank()
                    for k in range(8):
                        mm(pa[:, :n], w1t[:, k, fi * 128:(fi + 1) * 128], hT[:, k, :n], k == 0, k == 7, [w1b, Bh[k]], pab)
                    for k in range(8):
                        mm(pb_[:, :n], w3t[:, k, fi * 128:(fi + 1) * 128], hT[:, k, :n], k == 0, k == 7, [w3b, Bh[k]], pbb)
                    s1, s1b = st["s1"].next()
                    act(s1[:, :n], pa[:, :n], AF.Silu, [pab], [s1b])
                    g, gbuf = st["g"].next()
                    if gb is None:
                        tt(g[:, :n], s1[:, :n], pb_[:, :n], ALU.mult, [s1b, pbb], [gbuf])
                    else:
                        t2, t2b = st["t2"].next()
                        tt(t2[:, :n], s1[:, :n], pb_[:, :n], ALU.mult, [s1b, pbb], [t2b])
                        gap, gapb = gb(ei)
                        tt(g[:, :n], t2[:, :n], gap, ALU.mult, [t2b, gapb], [gbuf], eng="gpsimd")
                    gts.append((g, gbuf))
                return gts

            def down(i, gts):
                ei, ld, f0, gs = items[i]
                w1t, w1b, w3t, w3b, w2t, w2b = wt.pop(i)
                for dm in range(8):
                    pd, pdb = bank()
                    for fi in range(gs):
                        mm(pd[:, :n], w2t[:, fi, dm * 128:(dm + 1) * 128], gts[fi][0][:, :n], fi == 0, fi == gs - 1,
                           [w2b, gts[fi][1]], pdb)
                    if i == 0:
                        act(yacc[:, dm, :n], pd[:, :n], AF.Copy, [pdb], [By[dm]])
                    else:
                        tt(yacc[:, dm, :n], yacc[:, dm, :n], pd[:, :n], ALU.add, [By[dm], pdb], [By[dm]])

            pre = st.pop("pre", None)
            if pre is not None:
                wt.update(pre)
            else:
                issue_load(0)
                issue_load(1)
            nxt = {}
            prev = None
            for i in range(nI + 1):
                cur = up(i) if i < nI else None
                if prev is not None:
                    down(i - 1, prev)
                if i + 2 < nI:
                    issue_load(i + 2)
                elif next_loader is not None and i + 2 - nI < 2:
                    kk = i + 2 - nI
                    f0n, gsn = groups[kk]
                    w1t, w1b = st["w1"].next()
                    w3t, w3b = st["w3"].next()
                    w2t, w2b = st["w2"].next()
                    next_loader(f0n, gsn, w1t, w1b, w3t, w3b, w2t, w2b)
                    nxt[kk] = (w1t, w1b, w3t, w3b, w2t, w2b)
                prev = cur
            if next_loader is not None:
                st["pre"] = nxt

        def phase_B():
            with ExitStack() as px:
                alloc = mk_alloc(px)
                st = ffn_engine(alloc)
                xr = Rot(S, alloc, "xb", 3, [128, 8, 512], F32)
                hTs = [alloc(f"hTb{i}", [128, 8, 512], BF16) for i in range(2)]
                Bhs_ = [S.bufs(f"hb{i}_", 8) for i in range(2)]
                yacc = alloc("yaccb", [128, 8, 512]); By = S.bufs("yb", 8)
                sqr = Rot(S, alloc, "sqb", 2, [128, 512], F32)
                rst = Rot(S, alloc, "rsb", 2, [128, 512], F32)
                tmr = Rot(S, alloc, "tmb", 3, [128, 512], F32)
                tmps = (sqr, rst, tmr)

                fl = static_loader(f_w1, f_w3, f_w2)
                blocks = [(cs, Bcs, 0, TC, 1)] + [(xs, Bxs, tb, 512, 0) for tb in range(8)]

                xl = {}

                def load(i):
                    if i >= len(blocks):
                        return
                    buf_ap, bufs, tb, nb, col = blocks[i]
                    v = dview(buf_ap)
                    xb, xbb = xr.next()
                    S.dma("sync", xb[:, :, :nb], v[:, :, tb * nb:(tb + 1) * nb], reads=[bufs[tb]], writes=[xbb])
                    xl[i] = (xb, xbb)

                def prep(i):
                    buf_ap, bufs, tb, nb, col = blocks[i]
                    xb, xbb = xl[i]
                    norm_mod(xb, xbb, nb, 0, 1, col, hTs[i % 2], Bhs_[i % 2], tmps)
                    return xb, xbb

                load(0)
                load(1)
                cur = prep(0)
                for i in range(len(blocks)):
                    buf_ap, bufs, tb, nb, col = blocks[i]
                    v = dview(buf_ap)
                    load(i + 2)
                    nxt = prep(i + 1) if i + 1 < len(blocks) else None
                    xb, xbb = cur
                    ffn_run(st, hTs[i % 2], Bhs_[i % 2], nb, [fl], 22, None, yacc, By, next_loader=fl if i + 1 < len(blocks) else None)
                    for dm in range(8):
                        stt(xb[:, dm, :nb], yacc[:, dm, :nb], mod(0, 5, dm, col), xb[:, dm, :nb], ALU.mult, ALU.add,
                            [By[dm], Bmod, xbb], [xbb])
                    S.dma("sync", v[:, :, tb * nb:(tb + 1) * nb], xb[:, :, :nb], reads=[xbb], writes=[bufs[tb]])
                    cur = nxt

        NK = TC + T
        SCALE = 192.0 ** -0.5

        def phase_C():
            with ExitStack() as px:
                alloc = mk_alloc(px)
                qn = alloc("qn", [128, 3, T], BF16); Bqn = S.bufs("qn", 8)
                kvn = alloc("kvn", [128, 2, NK], BF16); Bkvn = S.bufs("kvn", 9)
                kpe = alloc("kpe", [128, NK], BF16); Bkpe = S.bufs("kpe", 9)
                qrp = alloc("qrp", [128, 4, T], BF16); Bqrp = S.bufs("qrp", 8)
                Batt = [S.bufs(f"att{h}_", 8) for h in range(8)]
                wuq = alloc("wuq", [128, 3, 1024], BF16); Bwuq = S.buf("wuq")
                wukv = alloc("wukv", [128, 2, 2048], BF16); Bwukv = S.buf("wukv")
                S.dma("gpsimd", wuq[:], o_wuq.rearrange("(k p) n -> p k n", p=128)[:, :, 0:1024], writes=[Bwuq])
                S.dma("gpsimd", wukv[:], o_wukv.rearrange("(k p) n -> p k n", p=128), writes=[Bwukv])
                with ExitStack() as p1:
                    al1 = mk_alloc(p1)
                    wuqr = al1("wuqr", [128, 3, 1024], BF16); Bwuqr = S.buf("wuqr")
                    S.dma("gpsimd", wuqr[:], o_wuq.rearrange("(k p) n -> p k n", p=128)[:, :, 1024:2048], writes=[Bwuqr])
                    wi = al1("wi", [128, 8, 896], BF16); Bwi = S.buf("wi")
                    S.dma("gpsimd", wi[:], o_w_in.rearrange("(k p) n -> p k n", p=128), writes=[Bwi])
                    xr = Rot(S, al1, "xc", 1, [128, 8, 512], F32)
                    hTs = [al1(f"hTc{i}", [128, 8, 512], BF16) for i in range(2)]
                    Bhs2 = [S.bufs(f"hc{i}_", 8) for i in range(2)]
                    zl = al1("zl", [128, 5, 512]); Bzl = S.bufs("zl", 5)
                    rp = Rot(S, al1, "rp", 2, [128, 2, 512], F32)
                    sqr = Rot(S, al1, "sqc", 2, [128, 512], F32)
                    rst = Rot(S, al1, "rsc", 4, [128, 512], F32)
                    tmr = Rot(S, al1, "tmc", 6, [128, 512], F32)
                    tmps = (sqr, rst, tmr)

                    lblocks = [(cs, Bcs, 0, TC, 1, 0, 0, False)] + [(xs, Bxs, tb, 512, 0, TC, 1, True) for tb in range(8)]

                    def lprep(i):
                        src, src_bufs, tb, nb, col, kbase, kb0, is_x = lblocks[i]
                        v = dview(src)
                        xb, xbb = xr.next()
                        S.dma("sync", xb[:, :, :nb], v[:, :, tb * nb:(tb + 1) * nb], reads=[src_bufs[tb]], writes=[xbb])
                        norm_mod(xb, xbb, nb, 1, 0, col, hTs[i % 2], Bhs2[i % 2], tmps)

                    lprep(0)
                    for li in range(len(lblocks)):
                        src, src_bufs, tb, nb, col, kbase, kb0, is_x = lblocks[li]
                        hT, Bh = hTs[li % 2], Bhs2[li % 2]
                        if li + 1 < len(lblocks):
                            lprep(li + 1)
                        if True:
                            kc0 = kbase + tb * nb
                            for c in (range(5) if is_x else range(3, 5)):
                                pz, pzb = bank()
                                for k in range(8):
                                    mm(pz[:, :nb], wi[:, k, c * 128:(c + 1) * 128], hT[:, k, :nb], k == 0, k == 7, [Bwi, Bh[k]], pzb)
                                act(zl[:, c, :nb], pz[:, :nb], AF.Copy, [pzb], [Bzl[c]])
                            if is_x:
                                r, rb = rms_stat(lambda j: zl[:, j, :nb], lambda j: Bzl[j], nb, (sqr, rst), 3, 1.0 / 384)
                                for c in range(3):
                                    stt(qn[:, c, tb * nb:(tb + 1) * nb], zl[:, c, :nb], vv("gq", c), r[:, :nb], ALU.mult, ALU.mult,
                                        [Bzl[c], Bvecs, rb], [Bqn[tb]])
                            r, rb = rms_stat(lambda j: zl[:, 3 + j, :nb], lambda j: Bzl[3 + j], nb, (sqr, rst), 2, 1.0 / 256)
                            for c in range(2):
                                stt(kvn[:, c, kc0:kc0 + nb], zl[:, 3 + c, :nb], vv("gkv", c), r[:, :nb], ALU.mult, ALU.mult,
                                    [Bzl[3 + c], Bvecs, rb], [Bkvn[kb0 + tb]])
                            pk, pkb = bank()
                            for k in range(8):
                                mm(pk[:, :nb], wi[:, k, 640:768], hT[:, k, :nb], k == 0, k == 7, [Bwi, Bh[k]], pkb)
                            if not is_x:
                                act(kpe[:, kc0:kc0 + nb], pk[:, :nb], AF.Copy, [pkb], [Bkpe[kb0 + tb]])
                            else:
                                pw, pwb = bank()
                                for k in range(8):
                                    mm(pw[:, :nb], wi[:, k, 768:896], hT[:, k, :nb], k == 0, k == 7, [Bwi, Bh[k]], pwb)
                                rt, rtb = rp.next()
                                S.dma("sync", rt[:, :, :nb], rope_in[:, :, tb * nb:(tb + 1) * nb], writes=[rtb])
                                t1, t1b = tmr.next()
                                tt(t1[:, :nb], pk[:, :nb], rt[:, 0, :nb], ALU.mult, [pkb, rtb], [t1b])
                                t2, t2b = tmr.next()
                                tt(t2[:, :nb], pw[:, :nb], rt[:, 1, :nb], ALU.mult, [pwb, rtb], [t2b])
                                tt(kpe[:, kc0:kc0 + nb], t1[:, :nb], t2[:, :nb], ALU.add, [t1b, t2b], [Bkpe[kb0 + tb]], eng="gpsimd")
                                for hp in range(4):
                                    pq, pqb = bank()
                                    pq2, pq2b = bank()
                                    for k in range(3):
                                        mm(pq[:, :nb], wuqr[:, k, hp * 128:(hp + 1) * 128], qn[:, k, tb * nb:(tb + 1) * nb],
                                           k == 0, k == 2, [Bwuqr, Bqn[tb]], pqb)
                                    for k in range(3):
                                        mm(pq2[:, :nb], wuqr[:, k, 512 + hp * 128:512 + (hp + 1) * 128], qn[:, k, tb * nb:(tb + 1) * nb],
                                           k == 0, k == 2, [Bwuqr, Bqn[tb]], pq2b)
                                    t1, t1b = tmr.next()
                                    tt(t1[:, :nb], pq[:, :nb], rt[:, 0, :nb], ALU.mult, [pqb, rtb], [t1b])
                                    t2, t2b = tmr.next()
                                    tt(t2[:, :nb], pq2[:, :nb], rt[:, 1, :nb], ALU.mult, [pq2b, rtb], [t2b])
                                    tt(qrp[:, hp, tb * nb:(tb + 1) * nb], t1[:, :nb], t2[:, :nb], ALU.add, [t1b, t2b], [Bqrp[tb]], eng="gpsimd")
                S.barrier()
                att = alloc("att", [128, 8, T], BF16)
                with ExitStack() as p2:
                    al2 = mk_alloc(p2)
                    KT = al2("KT", [128, NK], BF16); BKT = S.buf("KT")
                    Vh = al2("Vh", [128, 34, 128], BF16); BVh = S.buf("Vh")
                    Qh = al2("Qh", [128, T], BF16); BQh = S.buf("Qh")
                    pr = Rot(S, al2, "pT", 4, [128, 512], BF16)
                    rr = Rot(S, al2, "rr", 2, [128, 512], F32)
                    sb_banks = (0, 1, 2, 3)
                    acc_o = (4, 5)
                    acc_s = (6, 7)
                    allkv = Bkvn
                    for h in range(8):
                        hh, hp = h % 2, h // 2
                        for cb in range(0, NK, 512):
                            w = min(512, NK - cb)
                            pk, pkb = bank(sb_banks)
                            for k in range(2):
                                mm(pk[:, :w], wukv[:, k, h * 128:(h + 1) * 128], kvn[:, k, cb:cb + w], k == 0, k == 1, [Bwukv] + allkv, pkb)
                            act(KT[:, cb:cb + w], pk[:, :w], AF.Copy, [pkb], [BKT])
                        for kt0 in range(0, 34, 4):
                            nt = min(4, 34 - kt0)
                            pv, pvb = bank(sb_banks)
                            for i4 in range(nt):
                                kt = kt0 + i4
                                for k in range(2):
                                    mm(pv[:, i4 * 128:(i4 + 1) * 128], kvn[:, k, kt * 128:(kt + 1) * 128],
                                       wukv[:, k, 1024 + h * 128:1024 + (h + 1) * 128], k == 0, k == 1, [Bwukv] + allkv, pvb)
                            vcopy(Vh[:, kt0:kt0 + nt, :], pv[:, :nt * 128].rearrange("p (a b) -> p a b", a=nt), [pvb], [BVh])
                        for qb in range(8):
                            pq, pqb = bank(sb_banks)
                            for k in range(3):
                                mm(pq[:, :], wuq[:, k, h * 128:(h + 1) * 128], qn[:, k, qb * 512:(qb + 1) * 512], k == 0, k == 2,
                                   [Bwuq, Bqn[qb]], pqb)
                            act(Qh[:, qb * 512:(qb + 1) * 512], pq[:, :], AF.Copy, [pqb], [BQh])
                        for qb in range(8):
                            po, pob = bank(acc_o)
                            pS, pSb = bank(acc_s)
                            LAG = 2
                            pts = {}
                            for kt in range(34 + LAG):
                                if kt < 34:
                                    ps_, psb = bank(sb_banks)
                                    mm(ps_[:, :], KT[:, kt * 128:(kt + 1) * 128], Qh[:, qb * 512:(qb + 1) * 512], True, False, [BKT, BQh], psb)
                                    mm(ps_[:, :], kpe[hh * 64:(hh + 1) * 64, kt * 128:(kt + 1) * 128],
                                       qrp[hh * 64:(hh + 1) * 64, hp, qb * 512:(qb + 1) * 512], False, True, Bkpe + [Bqrp[qb]], psb)
                                    pT, pTb = pr.next()
                                    act(pT[:, :], ps_[:, :], AF.Exp, [psb], [pTb], scale=SCALE)
                                    pts[kt] = (pT, pTb)
                                if kt >= LAG:
                                    k2 = kt - LAG
                                    pT, pTb = pts.pop(k2)
                                    mm(po[:, :], Vh[:, k2, :], pT[:, :], k2 == 0, k2 == 33, [BVh, pTb], pob)
                                    mm(pS[:, :], onesb[:], pT[:, :], k2 == 0, k2 == 33, [Bonesb, pTb], pSb)
                            rc, rcb = rr.next()
                            recip(rc[:, :], pS[:, :], [pSb], [rcb])
                            tt(att[:, h, qb * 512:(qb + 1) * 512], po[:, :], rc[:, :], ALU.mult, [pob, rcb], [Batt[h][qb]])
                S.barrier()
                with ExitStack() as p3:
                    al3 = mk_alloc(p3)
                    wo = al3("wo", [128, 8, 1024], BF16); Bwo = S.buf("wo")
                    S.dma("gpsimd", wo[:], o_w_o.rearrange("(k p) n -> p k n", p=128), writes=[Bwo])
                    xr = Rot(S, al3, "xc3", 1, [128, 8, 512], F32)
                    v = dview(xs)
                    for tb in range(8):
                        xb, xbb = xr.next()
                        S.dma("sync", xb[:], v[:, :, tb * 512:(tb + 1) * 512], reads=[Bxs[tb]], writes=[xbb])
                        xn, xnb = xb, xbb
                        for dm in range(8):
                            po, pob = bank()
                            for h in range(8):
                                mm(po[:, :], wo[:, h, dm * 128:(dm + 1) * 128], att[:, h, tb * 512:(tb + 1) * 512], h == 0, h == 7,
                                   [Bwo, Batt[h][tb]], pob)
                            stt(xn[:, dm, :], po[:, :], mod(1, 2, dm, 0), xb[:, dm, :], ALU.mult, ALU.add, [pob, Bmod, xbb], [xnb])
                        S.dma("sync", v[:, :, tb * 512:(tb + 1) * 512], xn[:], reads=[xnb], writes=[Bxs[tb]])

        def phase_D():
            with ExitStack() as px:
                alloc = mk_alloc(px)
                st = ffn_engine(alloc)
                rt32 = alloc("rt32", [128, 8, 8]); Brt = S.buf("rt32")
                selt = alloc("selt", [8, 8, 128]); Bsel = S.buf("sel")
                S.dma("sync", rt32[:], o_router.rearrange("(k p) e -> p k e", p=128), writes=[Brt])
                S.dma("sync", selt[:], sel_in, writes=[Bsel])
                xr = Rot(S, alloc, "xd", 2, [128, 8, 512], F32)
                hT = alloc("hTd", [128, 8, 512], BF16); Bh = S.bufs("hd", 8)
                h32 = alloc("h32", [128, 8, 512]); Bh32 = S.bufs("h32_", 8)
                yacc = alloc("yaccd", [128, 8, 512]); By = S.bufs("yd", 8)
                gbt = alloc("gbt", [128, 8, 512]); Bgb = S.bufs("gb", 8)
                gT = alloc("gT", [8, 512]); BgT = S.buf("gT")
                sqr = Rot(S, alloc, "sqd", 2, [128, 512], F32)
                rst = Rot(S, alloc, "rsd", 2, [128, 512], F32)
                tmr = Rot(S, alloc, "tmd", 3, [128, 512], F32)
                sm = Rot(S, alloc, "smd", 12, [128, 8], F32)
                tmps = (sqr, rst, tmr)
                v = dview(xs)
                ov = dview(outT)
                experts = []
                for tb in range(8):
                    xb, xbb = xr.next()
                    S.dma("sync", xb[:], v[:, :, tb * 512:(tb + 1) * 512], reads=[Bxs[tb]], writes=[xbb])
                    norm_mod(xb, xbb, 512, 1, 1, 0, hT, Bh, tmps, h32=h32, Bh32=Bh32)
                    for t4 in range(4):
                        pl, plb = bank()
                        for k in range(8):
                            mm(pl[:, 0:8], h32[:, k, t4 * 128:(t4 + 1) * 128], rt32[:, k, :], k == 0, k == 7, [Bh32[k], Brt], plb)
                        lg, lgb = sm.next()
                        vcopy(lg[:, :], pl[:, 0:8], [plb], [lgb])
                        mx8, mxb = sm.next()
                        S.op("vector", lambda e, mx8=mx8, lg=lg: e.max(out=mx8[:, :], in_=lg[:, :]), reads=[lgb], writes=[mxb])
                        nm, nmb = sm.next()
                        ts(nm[:, 0:1], mx8[:, 0:1], -1.0, None, ALU.mult, None, [mxb], [nmb])
                        ex, exb = sm.next()
                        act(ex[:, :], lg[:, :], AF.Exp, [lgb, nmb], [exb], bias=nm[:, 0:1], scale=1.0)
                        mk, mkb = sm.next()
                        ts(mk[:, :], lg[:, :], mx8[:, 1:2], None, ALU.is_ge, None, [lgb, mxb], [mkb])
                        me, meb = sm.next()
                        tt(me[:, :], mk[:, :], ex[:, :], ALU.mult, [mkb, exb], [meb])
                        dn, dnb = sm.next()
                        S.op("vector", lambda e, dn=dn, me=me: e.tensor_reduce(out=dn[:, 0:1], in_=me[:, :], axis=mybir.AxisListType.X, op=ALU.add),
                             reads=[meb], writes=[dnb])
                        rd, rdb = sm.next()
                        recip(rd[:, 0:1], dn[:, 0:1], [dnb], [rdb])
                        gt, gtb = sm.next()
                        ts(gt[:, :], me[:, :], rd[:, 0:1], None, ALU.mult, None, [meb, rdb], [gtb])
                        ptr, ptrb = bank()
                        S.op("tensor", lambda e, ptr=ptr, gt=gt: e.transpose(out=ptr[0:8, 0:128], in_=gt[:, :], identity=ident[:]),
                             reads=[gtb, Bident], writes=[ptrb])
                        vcopy(gT[:, t4 * 128:(t4 + 1) * 128], ptr[0:8, 0:128], [ptrb], [BgT])
                    for e_ in range(8):
                        pg, pgb = bank()
                        mm(pg[:, :], selt[:, e_, :], gT[:, :], True, True, [Bsel, BgT], pgb)
                        act(gbt[:, e_, :], pg[:, :], AF.Copy, [pgb], [Bgb[e_]])
                    ffn_run(st, hT, Bh, 512, experts, 28, lambda ei: (gbt[:, ei, :], Bgb[ei]), yacc, By)
                    xn, xnb = xb, xbb
                    for dm in range(8):
                        stt(xn[:, dm, :], yacc[:, dm, :], mod(1, 5, dm, 0), xb[:, dm, :], ALU.mult, ALU.add, [By[dm], Bmod, xbb], [xnb])
                    r, rb = rms_stat(lambda j: xn[:, j, :], xnb, 512, (sqr, rst), 8, 1.0 / D)
                    for dm in range(8):
                        stt(yacc[:, dm, :], xn[:, dm, :], vv("fg", dm), r[:, :], ALU.mult, ALU.mult, [xnb, Bvecs, rb], [By[dm]])
                    S.dma("sync", ov[:, :, tb * 512:(tb + 1) * 512], yacc[:], reads=By, writes=[Bout[tb]])


        I32 = mybir.dt.int32
        JT = 24

        def phase_D2():
            with ExitStack() as px:
                alloc = mk_alloc(px)
                rt32 = alloc("rt32", [128, 8, 8]); Brt = S.buf("rt32")
                utri = alloc("utri", [128, 128]); Butri = S.buf("utri")
                ibase = alloc("ibase", [128, 7]); Bib = S.buf("ibase")
                identb = alloc("identb", [128, 128], BF16); Bidb = S.buf("identb")
                S.dma("sync", rt32[:], o_router.rearrange("(k p) e -> p k e", p=128), writes=[Brt])
                S.dma("sync", utri[:], utri_in, writes=[Butri])
                S.dma("sync", ibase[:], ibase_in, writes=[Bib])
                vcopy(identb[:], ident[:], [Bident], [Bidb])
                M_all = alloc("M_all", [128, 32, 8]); BM = S.bufs("Mall", 32)
                G_all = alloc("G_all", [128, 32, 8]); BGa = S.bufs("Gall", 32)
                R_all = alloc("R_all", [128, 32, 8]); BR = S.buf("Rall")
                P12f = alloc("P12f", [128, 64]); BP12f = S.bufs("P12f", 32)
                P12i = alloc("P12i", [128, 64], I32); BP12i = S.buf("P12i")
                G12 = alloc("G12", [128, 64]); BG12 = S.bufs("G12", 32)
                idxf = alloc("idxf", [128, JT * 7]); Bidxf = S.buf("idxf")
                idxi = alloc("idxi", [128, JT * 7], I32); Bidxi = S.buf("idxi")
                Bhz = S.buf("hz"); Bhs = S.buf("hs"); Bys = S.buf("ys")
                Bs2z = S.buf("s2z"); Bs2t = S.buf("s2t"); Bytok = S.buf("ytok")
                tokid = alloc("tokid", [128, 64, 16], I32); Btokid = S.buf("tokid")
                S.dma("sync", tokid[:], tokid_in, writes=[Btokid])
                with ExitStack() as p1:
                    al1 = mk_alloc(p1)
                    h_tm = al1("h_tm", [128, 32, 1024], BF16); Bhtm = S.bufs("htm", 32)
                    zt = al1("zt", [128, 8, 1024], BF16); Bzt = S.buf("zt")
                    S.op("gpsimd", lambda e: e.memset(zt[:], 0.0), writes=[Bzt])
                    for r in range(NS // 1024):
                        S.dma("sync", hsort[r * 1024:(r + 1) * 1024, :].rearrange("(r p) d -> p r d", p=128), zt[:],
                              reads=[Bzt], writes=[Bhz], join=True)
                    oobt = al1("oobt", [128, 96 * 16], I32); Boob = S.buf("oobt")
                    S.dma("sync", oobt[:], oobfill_in, writes=[Boob])
                    S.dma("sync", slot2tok.rearrange("(p r) o -> p (r o)", p=128), oobt[:], reads=[Boob], writes=[Bs2z])
                    xr = Rot(S, al1, "xd", 2, [128, 8, 512], F32)
                    h32s = [al1(f"h32_{i}", [128, 8, 512]) for i in range(2)]
                    Bh32s = [S.bufs(f"h32_{i}_", 8) for i in range(2)]
                    sqr = Rot(S, al1, "sqd", 2, [128, 512], F32)
                    rst = Rot(S, al1, "rsd", 2, [128, 512], F32)
                    tmr = Rot(S, al1, "tmd", 3, [128, 512], F32)
                    sm = Rot(S, al1, "smd", 48, [128, 8], F32)
                    tmps = (sqr, rst, tmr)
                    v = dview(xs)
                    for tb in range(8):
                        xb, xbb = xr.next()
                        S.dma("sync", xb[:], v[:, :, tb * 512:(tb + 1) * 512], reads=[Bxs[tb]], writes=[xbb])
                        h32, Bh32 = h32s[tb % 2], Bh32s[tb % 2]
                        norm_mod(xb, xbb, 512, 1, 1, 0, None, None, tmps, h32=h32, Bh32=Bh32)
                        for t4 in range(4):
                            c = tb * 4 + t4
                            pl, plb = bank()
                            for k in range(8):
                                mm(pl[:, 0:8], h32[:, k, t4 * 128:(t4 + 1) * 128], rt32[:, k, :], k == 0, k == 7, [Bh32[k], Brt], plb)
                            lg, lgb = sm.next()
                            vcopy(lg[:, :], pl[:, 0:8], [plb], [lgb])
                            mx8, mxb = sm.next()
                            S.op("vector", lambda e, mx8=mx8, lg=lg: e.max(out=mx8[:, :], in_=lg[:, :]), reads=[lgb], writes=[mxb])
                            nm, nmb = sm.next()
                            ts(nm[:, 0:1], mx8[:, 0:1], -1.0, None, ALU.mult, None, [mxb], [nmb])
                            ex, exb = sm.next()
                            act(ex[:, :], lg[:, :], AF.Exp, [lgb, nmb], [exb], bias=nm[:, 0:1], scale=1.0)
                            ts(M_all[:, c, :], lg[:, :], mx8[:, 1:2], None, ALU.is_ge, None, [lgb, mxb], [BM[c]])
                            me, meb = sm.next()
                            tt(me[:, :], M_all[:, c, :], ex[:, :], ALU.mult, [BM[c], exb], [meb])
                            dn, dnb = sm.next()
                            S.op("vector", lambda e, dn=dn, me=me: e.tensor_reduce(out=dn[:, 0:1], in_=me[:, :], axis=mybir.AxisListType.X, op=ALU.add),
                                 reads=[meb], writes=[dnb])
                            rd, rdb = sm.next()
                            recip(rd[:, 0:1], dn[:, 0:1], [dnb], [rdb])
                            ts(G_all[:, c, :], me[:, :], rd[:, 0:1], None, ALU.mult, None, [meb, rdb], [BGa[c]])
                            for half in range(2):
                                pt, ptb = bank()
                                for q in range(4):
                                    dk = half * 4 + q
                                    S.op("tensor", lambda e, pt=pt, q=q, dk=dk, t4=t4, h32=h32: e.transpose(
                                        out=pt[:, q * 128:(q + 1) * 128], in_=h32[:, dk, t4 * 128:(t4 + 1) * 128], identity=ident[:]),
                                        reads=[Bh32[dk], Bident], writes=[ptb])
                                if half == 0:
                                    act(h_tm[:, c, 0:512], pt[:, :], AF.Copy, [ptb], [Bhtm[c]])
                                else:
                                    vcopy(h_tm[:, c, 512:1024], pt[:, :], [ptb, Bhtm[c]], [Bhtm[c]])
                    sm2 = Rot(S, al1, "sm2", 24, [128, 8], F32)
                    pc_, pcb = bank()
                    for c in range(32):
                        mm(pc_[:, 0:8], ones[:], M_all[:, c, :], c == 0, c == 31, [Bones, BM[c]], pcb)
                    keep = Rot(S, al1, "keep", 5, [128, 8], F32)
                    cnt, cntb = keep.next()
                    vcopy(cnt[:, :], pc_[:, 0:8], [pcb], [cntb])
                    prk, prkb = bank()
                    for c in range(32):
                        mm(prk[:, c * 8:(c + 1) * 8], utri[:], M_all[:, c, :], True, c == 0, [Butri, BM[c]], prkb)
                        for c2 in range(c):
                            mm(prk[:, c * 8:(c + 1) * 8], ones[:], M_all[:, c2, :], False, c2 == c - 1, [Bones, BM[c2]], prkb)
                    vcopy(R_all[:].rearrange("p a b -> p (a b)"), prk[:, 0:256], [prkb], [BR])
                    tl, tlb = keep.next()
                    ts(tl[:, :], cnt[:, :], 0.0, None, ALU.is_gt, None, [cntb], [tlb])
                    for m in range(1, 8):
                        stt(tl[:, :], cnt[:, :], 512.0 * m, tl[:, :], ALU.is_gt, ALU.add, [cntb, tlb], [tlb])
                    pcv, pcvb = keep.next()
                    ts(pcv[:, :], tl[:, :], 512.0, None, ALU.mult, None, [tlb], [pcvb])
                    off, offb = keep.next()
                    memset(off[:, 0:1], 0.0, [offb])
                    for e_ in range(1, 8):
                        tt(off[:, e_:e_ + 1], off[:, e_ - 1:e_], pcv[:, e_ - 1:e_], ALU.add, [offb, pcvb], [offb])
                    endv, endb = keep.next()
                    tt(endv[:, :], off[:, :], pcv[:, :], ALU.add, [offb, pcvb], [endb])
                    ej = al1("ej", [128, JT]); Bej = S.buf("ej")
                    for j in range(JT):
                        cm, cmb = sm2.next()
                        ts(cm[:, :], endv[:, :], float(j * 512), None, ALU.is_le, None, [endb], [cmb])
                        S.op("vector", lambda e, cm=cm, j=j: e.tensor_reduce(out=ej[:, j:j + 1], in_=cm[:, :], axis=mybir.AxisListType.X, op=ALU.add),
                             reads=[cmb], writes=[Bej])
                    ej2 = al1("ej2", [128, JT]); Bej2 = S.buf("ej2")
                    ts(ej2[:, :], ej[:, :], 7.0, 1792.0, ALU.min, ALU.mult, [Bej], [Bej2])
                    for j in range(JT):
                        ts(idxf[:, j * 7:(j + 1) * 7], ibase[:, :], ej2[:, j:j + 1], None, ALU.add, None, [Bib, Bej2, Bidxf], [Bidxf])
                    vcopy(idxi[:, :], idxf[:, :], [Bidxf], [Bidxi])
                    for c in range(32):
                        a1, a1b = sm2.next()
                        stt(a1[:, :], R_all[:, c, :], 1.0, off[:, :], ALU.add, ALU.add, [BR, offb], [a1b])
                        a2, a2b = sm2.next()
                        tt(a2[:, :], a1[:, :], M_all[:, c, :], ALU.mult, [a1b, BM[c]], [a2b])
                        pm, pmb = sm2.next()
                        ts(pm[:, :], a2[:, :], -1.0, None, ALU.add, None, [a2b], [pmb])
                        mx, mxb = sm2.next()
                        S.op("vector", lambda e, mx=mx, pm=pm: e.max(out=mx[:, :], in_=pm[:, :]), reads=[pmb], writes=[mxb])
                        vcopy(P12f[:, c * 2:c * 2 + 2], mx[:, 0:2], [mxb], [BP12f[c]])
                        for k2 in range(2):
                            eq, eqb = sm2.next()
                            ts(eq[:, :], pm[:, :], mx[:, k2:k2 + 1], None, ALU.is_equal, None, [pmb, mxb], [eqb])
                            eg, egb = sm2.next()
                            tt(eg[:, :], eq[:, :], G_all[:, c, :], ALU.mult, [eqb, BGa[c]], [egb])
                            S.op("vector", lambda e, eg=eg, c=c, k2=k2: e.tensor_reduce(out=G12[:, c * 2 + k2:c * 2 + k2 + 1], in_=eg[:, :],
                                                                                         axis=mybir.AxisListType.X, op=ALU.add),
                                 reads=[egb, BG12[c]], writes=[BG12[c]])
                    vcopy(P12i[:, :], P12f[:, :], BP12f, [BP12i])
                    dbg_dump("cnt", cnt[:, :], [128, 8], [cntb])
                    dbg_dump("P12f", P12f[:, :], [128, 64], BP12f)
                    dbg_dump("G12", G12[:, :], [128, 64], BG12)
                    dbg_dump("ej", ej[:, :], [128, JT], [Bej])
                    dbg_dump("off", off[:, :], [128, 8], [offb])
                    dbg_dump("idxf", idxf[:, :], [128, JT * 7], [Bidxf])
                    dbg_dump("M_all", M_all[:].rearrange("p a b -> p (a b)"), [128, 256], BM)
                    dbg_dump("R_all", R_all[:].rearrange("p a b -> p (a b)"), [128, 256], [BR])
                    dbg_dump("h_tm", h_tm[:].rearrange("p a b -> p (a b)"), [128, 32 * 1024], Bhtm, dt=BF16)
                    for c in range(32):
                        for k2 in range(2):
                            def sc(e, c=c, k2=k2):
                                return e.indirect_dma_start(out=hsort, out_offset=bass.IndirectOffsetOnAxis(ap=P12i[:, c * 2 + k2:c * 2 + k2 + 1], axis=0),
                                                            in_=h_tm[:, c, :], in_offset=None)
                            S.dma_fn("gpsimd", sc, reads=[BP12i, Bhtm[c], Bhz], writes=[Bhs], join=True)

                            def sci(e, c=c, k2=k2):
                                return e.indirect_dma_start(out=slot2tok, out_offset=bass.IndirectOffsetOnAxis(ap=P12i[:, c * 2 + k2:c * 2 + k2 + 1], axis=0),
                                                            in_=tokid[:, c * 2 + k2, :], in_offset=None)
                            S.dma_fn("gpsimd", sci, reads=[BP12i, Btokid, Bs2z], writes=[Bs2t], join=True)
                S.barrier()
                with ExitStack() as p4:
                    al4 = mk_alloc(p4)
                    st = ffn_engine(al4)
                    hsr = Rot(S, al4, "hs_tm", 2, [128, 4, 1024], BF16)
                    hsTr = [al4(f"hsT{i}", [128, 8, 512], BF16) for i in range(2)]
                    BhsT = [S.bufs(f"hsT{i}_", 8) for i in range(2)]
                    yaccr = [al4(f"yaccs{i}", [128, 8, 512]) for i in range(2)]
                    Byr = [S.bufs(f"ys{i}_", 8) for i in range(2)]
                    ysr = Rot(S, al4, "ys_tm", 2, [128, 4, 1024], F32)
                    idr = Rot(S, al4, "idt", 2, [128, 64], I32)
                    Bscat = S.bufs("scat", 2)

                    def dyn_loader(j):
                        def load(f0, gs, w1t, w1b, w3t, w3b, w2t, w2b):
                            fg = f0 // 4
                            col = j * 7 + fg
                            for (tab, wt_, wb_) in ((x_w1, w1t, w1b), (x_w3, w3t, w3b), (x_w2, w2t, w2b)):
                                flat = wt_[:].rearrange("p a b -> p (a b)")
                                for hf in range(2):
                                    def g(e, tab=tab, flat=flat, hf=hf, col=col):
                                        return e.indirect_dma_start(out=flat[:, hf * 2048:(hf + 1) * 2048], out_offset=None, in_=tab,
                                                                    in_offset=bass.IndirectOffsetOnAxis(ap=idxi[:, col:col + 1], axis=0),
                                                                    element_offset=hf * 2048)
                                    S.dma_fn("gpsimd", g, reads=[Bidxi], writes=[wb_], join=(hf == 1))
                        return load

                    def prologue(j):
                        hs_t, hs_b = hsr.next()
                        S.dma("sync", hs_t[:], hsort[j * 512:(j + 1) * 512, :].rearrange("(r p) d -> p r d", p=128), reads=[Bhz, Bhs], writes=[hs_b])
                        hsT, Bh_ = hsTr[j % 2], BhsT[j % 2]
                        for dk in range(8):
                            pt, ptb = bank()
                            for r in range(4):
                                mm(pt[:, r * 128:(r + 1) * 128], hs_t[:, r, dk * 128:(dk + 1) * 128], identb[:], True, True, [hs_b, Bidb], ptb)
                            if dk % 2 == 0:
                                act(hsT[:, dk, :], pt[:, :], AF.Copy, [ptb], [Bh_[dk]])
                            else:
                                vcopy(hsT[:, dk, :], pt[:, :], [ptb], [Bh_[dk]])

                    def epilogue(j):
                        yacc, By = yaccr[j % 2], Byr[j % 2]
                        ys_t, ys_b = ysr.next()
                        for r in range(4):
                            for half in range(2):
                                pt, ptb = bank()
                                for q in range(4):
                                    dm = half * 4 + q
                                    S.op("tensor", lambda e, pt=pt, q=q, dm=dm, r=r, yacc=yacc: e.transpose(
                                        out=pt[:, q * 128:(q + 1) * 128], in_=yacc[:, dm, r * 128:(r + 1) * 128], identity=ident[:]),
                                        reads=[By[dm], Bident], writes=[ptb])
                                if half == 0:
                                    act(ys_t[:, r, 0:512], pt[:, :], AF.Copy, [ptb, ys_b], [ys_b])
                                else:
                                    vcopy(ys_t[:, r, 512:1024], pt[:, :], [ptb, ys_b], [ys_b])
                        idt, idb = idr.next()
                        S.dma("sync", idt[:].rearrange("p (r o) -> p r o", r=4), slot2tok[j * 512:(j + 1) * 512, :].rearrange("(r p) o -> p r o", p=128), reads=[Bs2z, Bs2t], writes=[idb])
                        for r in range(4):
                            def scy(e, r=r, idt=idt, ys_t=ys_t):
                                return e.indirect_dma_start(out=ytok, out_offset=bass.IndirectOffsetOnAxis(ap=idt[:, r * 16:r * 16 + 1], axis=0),
                                                            in_=ys_t[:, r, :], in_offset=None)
                            S.dma_fn("gpsimd", scy, reads=[idb, ys_b], writes=[Bscat[j % 2]], join=True)

                    prologue(0)
                    for j in range(JT):
                        if j + 1 < JT:
                            prologue(j + 1)
                        ffn_run(st, hsTr[j % 2], BhsT[j % 2], 512, [dyn_loader(j)], 28, None, yaccr[j % 2], Byr[j % 2],
                                next_loader=dyn_loader(j + 1) if j + 1 < JT else None)
                        if j >= 1:
                            epilogue(j - 1)
                    epilogue(JT - 1)
                S.barrier()
                if DEBUG:
                    tq = nc.dram_tensor("dbg_hsort", [NS, D], BF16, kind="ExternalOutput").ap()
                    S.dma("sync", tq, hsort, reads=[Bhz, Bhs]); DBG_NAMES.append("dbg_hsort")
                    tq2 = nc.dram_tensor("dbg_ysort", [NS, D], F32, kind="ExternalOutput").ap()
                    S.dma("sync", tq2, ysort, reads=[Bys]); DBG_NAMES.append("dbg_ysort")
                with ExitStack() as p5:
                    al5 = mk_alloc(p5)
                    xr = Rot(S, al5, "xd5", 2, [128, 8, 512], F32)
                    y12r = Rot(S, al5, "y12", 6, [128, 2, 1024], F32)
                    ytr = Rot(S, al5, "yt", 3, [128, 1024], F32)
                    oo = Rot(S, al5, "oo", 2, [128, 8, 512], F32)
                    sqr = Rot(S, al5, "sq5", 2, [128, 512], F32)
                    rst = Rot(S, al5, "rs5", 2, [128, 512], F32)
                    v = dview(xs)
                    ov = dview(outT)
                    for tb in range(8):
                        xb, xbb = xr.next()
                        S.dma("sync", xb[:], v[:, :, tb * 512:(tb + 1) * 512], reads=[Bxs[tb]], writes=[xbb])
                        for t4 in range(4):
                            c = tb * 4 + t4
                            y12, y12b = y12r.next()
                            S.dma("sync", y12[:], ytok[0:2 * T, :].rearrange("(t k) d -> t k d", k=2)[c * 128:(c + 1) * 128], reads=[Bytok], writes=[y12b])
                            y1, y1b, y2, y2b = y12[:, 0, :], y12b, y12[:, 1, :], y12b
                            yt, ytb = ytr.next()
                            ts(yt[:, :], y1, G12[:, c * 2:c * 2 + 1], None, ALU.mult, None, [y1b, BG12[c]], [ytb])
                            stt(yt[:, :], y2, G12[:, c * 2 + 1:c * 2 + 2], yt[:, :], ALU.mult, ALU.add, [y2b, BG12[c], ytb], [ytb])
                            for dm in range(8):
                                S.op("tensor", lambda e, dm=dm, t4=t4, yt=yt: e.transpose(
                                    out=banks[dm][:, t4 * 128:(t4 + 1) * 128], in_=yt[:, dm * 128:(dm + 1) * 128], identity=ident[:]),
                                    reads=[ytb, Bident], writes=[bank_b[dm]])
                        for dm in range(8):
                            stt(xb[:, dm, :], banks[dm][:, :], mod(1, 5, dm, 0), xb[:, dm, :], ALU.mult, ALU.add, [bank_b[dm], Bmod, xbb], [xbb])
                        r, rb = rms_stat(lambda j: xb[:, j, :], xbb, 512, (sqr, rst), 8, 1.0 / D)
                        ot, otb = oo.next()
                        for dm in range(8):
                            stt(ot[:, dm, :], xb[:, dm, :], vv("fg", dm), r[:, :], ALU.mult, ALU.mult, [xbb, Bvecs, rb], [otb])
                        S.dma("sync", ov[:, :, tb * 512:(tb + 1) * 512], ot[:], reads=[otb], writes=[Bout[tb]])

        Bxs = S.bufs("xs", 8)
        Bcs = S.bufs("cs", 1)
        Bout = S.bufs("out", 8)
        phases = [("A", phase_A), ("B", phase_B), ("C", phase_C), ("D", phase_D2)]
        for name, fn in phases:
            fn()
            S.barrier()
            if name == stop_after:
                break
        if stop_after != "D":
            for tb in range(8):
                S.dma("sync", outT[:, tb * 512:(tb + 1) * 512], xs[:, tb * 512:(tb + 1) * 512], reads=[Bxs[tb]], writes=[Bout[tb]])
        S.barrier()
        S.emit()
        print(f"[build] insts={S.n_inst} waits={S.n_wait} sems={S.nsem}", flush=True)
    return nc


def _rope_tables():
    rows = T // 64
    row = np.repeat(np.arange(rows), 64).astype(np.float32)
    col = np.tile(np.arange(64), rows).astype(np.float32)
    inv = (10000.0 ** (-np.arange(16, dtype=np.float32) / 16)).astype(np.float32)
    ang = np.stack([row[:, None] * inv, col[:, None] * inv], axis=1).astype(np.float32)
    cos, sin = np.cos(ang).astype(np.float32), np.sin(ang).astype(np.float32)
    C = np.zeros((64, T), np.float32)
    Sg = np.zeros((64, T), np.float32)
    for p in range(64):
        ax, half, f = p // 32, (p // 16) % 2, p % 16
        C[p] = cos[:, ax, f]
        Sg[p] = sin[:, ax, f] * (-1.0 if half == 0 else 1.0)
    tab = np.stack([np.concatenate([C, C], 0), np.concatenate([Sg, Sg], 0)], axis=1)
    return np.ascontiguousarray(tab)


def _prep_shared(inp):
    f = lambda a: np.ascontiguousarray(np.asarray(a, dtype=np.float32))
    sh = {}
    sh["ident"] = np.eye(128, dtype=np.float32)
    sh["lnv"] = f(np.stack([np.broadcast_to(inp["e_ln_v_g"][0], (128, 512)), np.broadcast_to(inp["e_ln_v_b"][0], (128, 512))], axis=1))
    bs = inp["e_b_s"][0]
    sh["bs"] = f(np.repeat(bs.reshape(4, 2, 1, 128), 64, axis=2).reshape(4, 128, 128).transpose(1, 0, 2))
    sh["wsT"] = f(inp["e_w_s"][0].transpose(2, 0, 1))
    sh["rope"] = _rope_tables()
    sel = np.zeros((8, 8, 128), np.float32)
    for e in range(8):
        sel[e, e, :] = 1.0
    sh["sel"] = sel
    sh["w_ada"] = f(inp["w_ada"])
    sh["e_w_in"] = f(inp["e_w_in"][0])
    sh["e_w_out"] = f(inp["e_w_out"][0])
    sh["e_ffn_w1"] = f(inp["e_ffn_w1"][0])
    sh["e_ffn_w3"] = f(inp["e_ffn_w3"][0])
    sh["e_ffn_w2"] = f(inp["e_ffn_w2"][0])
    perm = np.arange(64) ^ 16
    wi = inp["o_w_in"][0]
    kp = wi[:, 640:704]
    sh["o_w_in_x"] = f(np.concatenate([wi[:, :640], kp, kp, kp[:, perm], kp[:, perm]], axis=1))
    wq = inp["o_w_uq"][0].reshape(384, 8, 192)
    nope = wq[:, :, :128].reshape(384, 1024)
    rp = wq[:, :, 128:]
    sh["o_wuq"] = f(np.concatenate([nope, rp.reshape(384, 512), rp[:, :, perm].reshape(384, 512)], axis=1))
    wkv = inp["o_w_ukv"][0].reshape(256, 8, 256)
    sh["o_wukv"] = f(np.concatenate([wkv[:, :, :128].reshape(256, 1024), wkv[:, :, 128:].reshape(256, 1024)], axis=1))
    sh["o_w_o"] = f(inp["o_w_o"][0])
    sh["o_router"] = f(inp["o_router"][0])
    w1 = np.asarray(inp["o_exp_w1"][0], np.float32).reshape(8, 8, 128, 7, 512)
    sh["xw1r"] = np.ascontiguousarray(w1.transpose(0, 3, 2, 1, 4)).reshape(8 * 7 * 128 * 2, 2048)
    w3 = np.asarray(inp["o_exp_w3"][0], np.float32).reshape(8, 8, 128, 7, 512)
    sh["xw3r"] = np.ascontiguousarray(w3.transpose(0, 3, 2, 1, 4)).reshape(8 * 7 * 128 * 2, 2048)
    w2 = np.asarray(inp["o_exp_w2"][0], np.float32).reshape(8, 7, 4, 128, 1024)
    sh["xw2r"] = np.ascontiguousarray(w2.transpose(0, 1, 3, 2, 4)).reshape(8 * 7 * 128 * 2, 2048)
    tid = ((np.arange(32)[None, :, None] * 128 + np.arange(128)[:, None, None]) * 2 + np.arange(2)[None, None, :]).reshape(128, 64)
    sh["tokid"] = np.ascontiguousarray(np.repeat(tid[:, :, None], 16, axis=2).astype(np.int32))
    sh["oobfill"] = np.ascontiguousarray(np.repeat((2 * T + np.arange(128 * 96, dtype=np.int32)).reshape(128, 96, 1), 16, axis=2).reshape(128, 96 * 16))
    sh["utri"] = np.triu(np.ones((128, 128), np.float32), 1)
    sh["ibase"] = (np.arange(7, dtype=np.float32)[None, :] * 256 + 2 * np.arange(128, dtype=np.float32)[:, None]).astype(np.float32)
    return sh


def _prep_vecs(inp, b):
    v = np.zeros((128, NV), np.float32)

    def put(name, arr):
        arr = np.asarray(arr, np.float32).reshape(128, -1)
        v[:, VOFF[name]:VOFF[name] + arr.shape[1]] = arr
    cp = lambda a: np.asarray(a, np.float32).reshape(-1, 128).T
    put("sc", np.stack([cp(inp["c"][b]), cp(inp["c_ctx"])], axis=-1))
    ba = np.asarray(inp["b_ada"], np.float32).reshape(2, 6, 8, 128).transpose(3, 0, 1, 2)
    put("bada", np.repeat(ba[..., None], 2, axis=-1))
    ng = np.asarray(inp["norm_g"], np.float32).reshape(2, 2, 8, 128).transpose(3, 0, 1, 2)
    put("ng", np.repeat(ng[..., None], 2, axis=-1))
    put("convw", np.asarray(inp["e_conv_w"][0], np.float32).reshape(31, 4, 128).transpose(2, 1, 0))
    put("convb", cp(inp["e_conv_b"][0]))
    put("lnag", cp(inp["e_ln_a_g"][0]))
    put("lnab", cp(inp["e_ln_a_b"][0]))
    put("gq", cp(inp["o_g_q"][0]))
    put("gkv", cp(inp["o_g_kv"][0]))
    put("fg", cp(inp["final_g"]))
    return v


def run(inputs, stop_after="D", cores=None):
    inp = {k: np.asarray(v) for k, v in inputs.items()}
    cores = list(range(NCORES)) if cores is None else cores
    sh = _prep_shared(inp)
    in_maps = []
    for b in cores:
        m = dict(sh)
        m["xT"] = np.ascontiguousarray(inp["x"][b].T.astype(np.float32))
        m["cT"] = np.ascontiguousarray(inp["ctx"][b].T.astype(np.float32))
        m["vecs"] = _prep_vecs(inp, b)
        in_maps.append(m)
    nc = build(stop_after)
    res = run_bass_kernel_spmd(nc, in_maps, core_ids=list(range(len(cores))))
    if DEBUG:
        for n in DBG_NAMES:
            DBG_OUT[n] = np.asarray(res.results[0][n])
    return np.stack([np.ascontiguousarray(r["outT"].T) for r in res.results], axis=0)


def kernel(**inputs):
    return run(inputs).astype(np.float32)
```

```python
import numpy as np
from contextlib import ExitStack
import concourse.bass as bass
import concourse.mybir as mybir
from concourse.bass_utils import run_bass_kernel_spmd

F32 = mybir.dt.float32
BF16 = mybir.dt.bfloat16
AF = mybir.ActivationFunctionType
ALU = mybir.AluOpType

T, TC, D, KD = 4096, 256, 1024, 8
EPS = 1e-6
NCORES = 8
SEM_LIMIT = 30000
DEBUG = False
DBG_NAMES = []
DBG_OUT = {}


class Buf:
    __slots__ = ("name", "w", "r", "dsem", "dcnt")

    def __init__(self, name):
        self.name = name
        self.w = {}
        self.r = {}
        self.dsem = None
        self.dcnt = 0


class Sched:
    def __init__(self, nc, ctx):
        self.nc = nc
        self.ctx = ctx
        self.prog = {e: [] for e in ("tensor", "vector", "scalar", "gpsimd", "sync")}
        self.esem = {}
        self.ecnt = {}
        self.nsem = 0
        self.allsems = {}
        for e in ("tensor", "vector", "scalar", "gpsimd"):
            self._new_esem(e)
        self.seen = {e: {} for e in self.prog}
        self.n_inst = 0
        self.n_wait = 0

    def _alloc_sem(self, name):
        self.nsem += 1
        s = self.ctx.enter_context(self.nc.semaphore(f"{name}_{self.nsem}"))
        self.allsems[id(s)] = [s, 0]
        return s

    def _new_esem(self, e):
        self.esem[e] = self._alloc_sem(f"e_{e}")
        self.ecnt[e] = 0

    def buf(self, name):
        return Buf(name)

    def bufs(self, name, n):
        return [Buf(f"{name}{i}") for i in range(n)]

    def _need(self, eng, toks):
        out = {}
        for (sem, val, teng) in toks:
            if teng == eng and eng == "tensor":
                continue
            k = id(sem)
            if self.seen[eng].get(k, 0) >= val:
                continue
            if k not in out or out[k][1] < val:
                out[k] = (sem, val)
        for k, (sem, val) in out.items():
            self.seen[eng][k] = val
        return list(out.values())

    def _deps(self, eng, reads, writes, join=False):
        toks = []
        for b in reads:
            toks += b.w.values()
        for b in writes:
            if not join:
                toks += b.w.values()
            toks += b.r.values()
        return self._need(eng, toks)

    def _record(self, tok, reads, writes, join=False):
        k = id(tok[0])
        self.allsems[k][1] = max(self.allsems[k][1], tok[1])
        for b in reads:
            b.r[k] = tok
        for b in writes:
            if join:
                b.w[k] = tok
            else:
                b.w = {k: tok}
                b.r = {}

    def op(self, eng, fn, reads=(), writes=()):
        reads = [b for b in reads if b is not None]
        writes = [b for b in writes if b is not None]
        waits = self._deps(eng, reads, writes)
        if self.ecnt[eng] >= SEM_LIMIT:
            self._new_esem(eng)
        sem = self.esem[eng]
        self.ecnt[eng] += 1
        tok = (sem, self.ecnt[eng], eng)
        self._record(tok, reads, writes)
        self.n_inst += 1
        self.n_wait += len(waits)

        def run(e, waits=waits, fn=fn, sem=sem):
            for (s, v) in waits:
                e.wait_ge(s, v)
            fn(e).then_inc(sem, 1)
        self.prog[eng].append(run)

    def dma(self, queue, out, in_, reads=(), writes=(), join=False, **kw):
        reads = [b for b in reads if b is not None]
        writes = [b for b in writes if b is not None]
        waits = self._deps(queue, reads, writes, join)
        sb = writes[0] if writes else reads[0]
        if sb.dsem is None:
            sb.dsem = self._alloc_sem("d")
        sb.dcnt += 16
        tok = (sb.dsem, sb.dcnt, "dma")
        self._record(tok, reads, writes, join)
        self.n_inst += 1
        self.n_wait += len(waits)

        def run(e, waits=waits, sem=sb.dsem):
            for (s, v) in waits:
                e.wait_ge(s, v)
            e.dma_start(out=out, in_=in_, **kw).then_inc(sem, 16)
        self.prog[queue].append(run)

    def dma_fn(self, queue, fn, reads=(), writes=(), join=False):
        reads = [b for b in reads if b is not None]
        writes = [b for b in writes if b is not None]
        waits = self._deps(queue, reads, writes, join)
        sb = writes[0] if writes else reads[0]
        if sb.dsem is None:
            sb.dsem = self._alloc_sem("d")
        sb.dcnt += 16
        tok = (sb.dsem, sb.dcnt, "dma")
        self._record(tok, reads, writes, join)
        self.n_inst += 1

        def run(e, waits=waits, sem=sb.dsem, fn=fn):
            for (s, v) in waits:
                e.wait_ge(s, v)
            fn(e).then_inc(sem, 16)
        self.prog[queue].append(run)

    def barrier(self):
        toks = [(s, v, "x") for (s, v) in self.allsems.values() if v > 0]
        for eng in self.prog:
            waits = self._need(eng, toks)

            def run(e, waits=waits):
                for (s, v) in waits:
                    e.wait_ge(s, v)
            self.prog[eng].append(run)

    def emit(self):
        with self.nc.Block() as block:
            @block.sync
            def _(e):
                for f in self.prog["sync"]:
                    f(e)

            @block.tensor
            def _(e):
                for f in self.prog["tensor"]:
                    f(e)

            @block.vector
            def _(e):
                for f in self.prog["vector"]:
                    f(e)

            @block.scalar
            def _(e):
                for f in self.prog["scalar"]:
                    f(e)

            @block.gpsimd
            def _(e):
                for f in self.prog["gpsimd"]:
                    f(e)


class Rot:
    def __init__(self, S, alloc, name, k, shape, dt):
        self.t = [alloc(f"{name}{i}", shape, dt) for i in range(k)]
        self.b = S.bufs(name, k)
        self.i = 0

    def next(self):
        i = self.i
        self.i = (i + 1) % len(self.t)
        return self.t[i], self.b[i]


VOFF = {}


def _vlayout():
    off = 0
    for name, n in [("sc", 16), ("bada", 192), ("ng", 64), ("convw", 124), ("convb", 4), ("lnag", 4),
                    ("lnab", 4), ("gq", 3), ("gkv", 2), ("fg", 8)]:
        VOFF[name] = off
        off += n
    return off


NV = _vlayout()


def build(stop_after="D"):
    nc = bass.Bass("TRN2", target_bir_lowering=False)

    def din(name, shape):
        return nc.dram_tensor(name, list(shape), F32, kind="ExternalInput").ap()

    xT_in = din("xT", [D, T])
    cT_in = din("cT", [D, TC])
    vecs_in = din("vecs", [128, NV])
    ident_in = din("ident", [128, 128])
    lnv_in = din("lnv", [128, 2, 512])
    bs_in = din("bs", [128, 4, 128])
    wsT_in = din("wsT", [128, 8, 128])
    rope_in = din("rope", [128, 2, T])
    sel_in = din("sel", [8, 8, 128])
    w_ada = din("w_ada", [2, D, 6 * D])
    e_w_in = din("e_w_in", [D, 2048])
    e_w_out = din("e_w_out", [D, D])
    f_w1 = din("e_ffn_w1", [D, 2816])
    f_w3 = din("e_ffn_w3", [D, 2816])
    f_w2 = din("e_ffn_w2", [2816, D])
    o_w_in = din("o_w_in_x", [D, 896])
    o_wuq = din("o_wuq", [384, 2048])
    o_wukv = din("o_wukv", [256, 2048])
    o_w_o = din("o_w_o", [D, D])
    o_router = din("o_router", [D, 8])
    NROW = 8 * 7 * 128 * 2
    x_w1 = din("xw1r", [NROW, 2048])
    x_w3 = din("xw3r", [NROW, 2048])
    x_w2 = din("xw2r", [NROW, 2048])
    utri_in = din("utri", [128, 128])
    tokid_in = nc.dram_tensor("tokid", [128, 64, 16], mybir.dt.int32, kind="ExternalInput").ap()
    oobfill_in = nc.dram_tensor("oobfill", [128, 96 * 16], mybir.dt.int32, kind="ExternalInput").ap()
    slot2tok = nc.dram_tensor("slot2tok", [24 * 512, 16], mybir.dt.int32).ap()
    ytok = nc.dram_tensor("ytok", [2 * T + 24 * 512, D], F32).ap()
    ibase_in = din("ibase", [128, 7])
    NS = 24 * 512
    hsort = nc.dram_tensor("hsort", [NS, D], BF16).ap()
    ysort = nc.dram_tensor("ysort", [NS, D], F32).ap()
    outT = nc.dram_tensor("outT", [D, T], F32, kind="ExternalOutput").ap()
    xs = nc.dram_tensor("xs", [D, T], F32).ap()
    cs = nc.dram_tensor("cs", [D, TC], F32).ap()

    def dview(ap):
        return ap.rearrange("(j p) n -> p j n", p=128)

    with ExitStack() as ctx:
        S = Sched(nc, ctx)

        uid = [0]

        def dbg_dump(name, ap, shape, reads, dt=F32):
            if not DEBUG:
                return
            t = nc.dram_tensor("dbg_" + name, list(shape), dt, kind="ExternalOutput").ap()
            S.dma("sync", t, ap, reads=reads)
            DBG_NAMES.append("dbg_" + name)

        def mk_alloc(stack):
            def alloc(name, shape, dt=F32):
                uid[0] += 1
                return stack.enter_context(nc.sbuf_tensor(f"sb{uid[0]}_{name}", list(shape), dt))
            return alloc
        galloc = mk_alloc(ctx)

        banks = [ctx.enter_context(nc.psum_tensor(f"pb{i}", [128, 512], F32)) for i in range(8)]
        bank_b = S.bufs("pb", 8)
        bstate = {"i": 0}

        def bank(subset=None):
            if subset is None:
                i = bstate["i"]
                bstate["i"] = (i + 1) % 8
            else:
                i = subset[bstate.setdefault(id(subset), 0) % len(subset)]
                bstate[id(subset)] += 1
            return banks[i], bank_b[i]

        def mm(out, lhsT, rhs, start, stop, reads, wb):
            S.op("tensor", lambda e: e.matmul(out, lhsT=lhsT, rhs=rhs, start=start, stop=stop), reads=reads, writes=[wb])

        def act(out, in_, func, reads, writes, **kw):
            S.op("scalar", lambda e: e.activation(out=out, in_=in_, func=func, **kw), reads=reads, writes=writes)

        def tt(out, a, b, op, reads, writes, eng="vector"):
            S.op(eng, lambda e: e.tensor_tensor(out=out, in0=a, in1=b, op=op), reads=reads, writes=writes)

        def stt(out, in0, scalar, in1, op0, op1, reads, writes, eng="vector"):
            S.op(eng, lambda e: e.scalar_tensor_tensor(out=out, in0=in0, scalar=scalar, in1=in1, op0=op0, op1=op1),
                 reads=reads, writes=writes)

        def ts(out, in0, s1, s2, op0, op1, reads, writes, eng="vector"):
            if s2 is None:
                S.op(eng, lambda e: e.tensor_scalar(out=out, in0=in0, scalar1=s1, scalar2=None, op0=op0), reads=reads, writes=writes)
            else:
                S.op(eng, lambda e: e.tensor_scalar(out=out, in0=in0, scalar1=s1, scalar2=s2, op0=op0, op1=op1),
                     reads=reads, writes=writes)

        def vcopy(out, in_, reads, writes, eng="vector"):
            S.op(eng, lambda e: e.tensor_copy(out=out, in_=in_), reads=reads, writes=writes)

        def recip(out, in_, reads, writes):
            S.op("vector", lambda e: e.reciprocal(out=out, in_=in_), reads=reads, writes=writes)

        def memset(ap, val, writes):
            S.op("vector", lambda e: e.memset(ap, val), writes=writes)

        vecs = galloc("vecs", [128, NV]); Bvecs = S.buf("vecs")
        ident = galloc("ident", [128, 128]); Bident = S.buf("ident")
        ones = galloc("ones", [128, 128]); Bones = S.buf("ones")
        onesb = galloc("onesb", [128, 128], BF16); Bonesb = S.buf("onesb")
        epsc = galloc("epsc", [128, 1]); Beps = S.buf("eps")
        modT = galloc("modT", [128, 2 * 6 * 8 * 2]); Bmod = S.buf("modT")
        Gt = galloc("Gt", [128, 2 * 2 * 8 * 2]); BG = S.buf("Gt")
        scs = galloc("scs", [128, 16]); Bscs = S.buf("scs")
        S.dma("sync", vecs[:], vecs_in, writes=[Bvecs])
        S.dma("sync", ident[:], ident_in, writes=[Bident])
        memset(ones[:], 1.0, [Bones])
        memset(onesb[:], 1.0, [Bonesb])
        memset(epsc[:], EPS, [Beps])

        def vv(name, idx, n=1):
            o = VOFF[name] + idx
            return vecs[:, o:o + n]

        def mod(i, v, j, col):
            o = ((i * 6 + v) * 8 + j) * 2 + col
            return modT[:, o:o + 1]

        def Gs(i, n, j, col):
            o = ((i * 2 + n) * 8 + j) * 2 + col
            return Gt[:, o:o + 1]

        with ExitStack() as px:
            alloc = mk_alloc(px)
            wa = Rot(S, alloc, "wa", 2, [128, 8, 1024], F32)
            act(scs[:], vv("sc", 0, 16), AF.Silu, [Bvecs], [Bscs])
            for i in range(2):
                wv = w_ada[i].rearrange("(k p) n -> p k n", p=128)
                for v in range(6):
                    wt, wb = wa.next()
                    S.dma("sync", wt[:], wv[:, :, v * 1024:(v + 1) * 1024], writes=[wb])
                    pb, pbb = bank()
                    for j in range(8):
                        for k in range(8):
                            mm(pb[:, j * 2:j * 2 + 2], wt[:, k, j * 128:(j + 1) * 128], scs[:, k * 2:k * 2 + 2],
                               k == 0, k == 7, [wb, Bscs], pbb)
                    o = (i * 6 + v) * 16
                    tt(modT[:, o:o + 16], pb[:, 0:16], vecs[:, VOFF["bada"] + o:VOFF["bada"] + o + 16], ALU.add,
                       [pbb, Bvecs], [Bmod])
            for i in range(2):
                for n in range(2):
                    vs = 1 if n == 0 else 4
                    o = (i * 2 + n) * 16
                    om = (i * 6 + vs) * 16
                    stt(Gt[:, o:o + 16], modT[:, om:om + 16], 1.0, vecs[:, VOFF["ng"] + o:VOFF["ng"] + o + 16],
                        ALU.add, ALU.mult, [Bmod, Bvecs], [BG])
        S.barrier()

        def rms_stat(xt, Bx, n, tmps, nchunks, inv_d):
            sqr, rst = tmps
            pb, pbb = bank()
            for j in range(nchunks):
                sq, sqb = sqr.next()
                act(sq[:, :n], xt(j), AF.Square, [Bx(j) if callable(Bx) else Bx], [sqb])
                mm(pb[:, :n], ones[:], sq[:, :n], j == 0, j == nchunks - 1, [Bones, sqb], pbb)
            r1, r1b = rst.next()
            act(r1[:, :n], pb[:, :n], AF.Sqrt, [pbb, Beps], [r1b], bias=epsc[:], scale=inv_d)
            r2, r2b = rst.next()
            recip(r2[:, :n], r1[:, :n], [r1b], [r2b])
            return r2, r2b

        def norm_mod(xblk, Bx, n, i, nn, col, hT, Bh, tmps, h32=None, Bh32=None):
            sqr, rst, tmr = tmps
            r, rb = rms_stat(lambda j: xblk[:, j, :n], Bx, n, (sqr, rst), 8, 1.0 / D)
            vsh = 0 if nn == 0 else 3
            for j in range(8):
                tm, tmb = tmr.next()
                stt(tm[:, :n], xblk[:, j, :n], Gs(i, nn, j, col), r[:, :n], ALU.mult, ALU.mult, [Bx, BG, rb], [tmb])
                if h32 is not None:
                    act(h32[:, j, :n], tm[:, :n], AF.Identity, [tmb, Bmod], [Bh32[j]], bias=mod(i, vsh, j, col), scale=1.0)
                    if hT is not None:
                        vcopy(hT[:, j, :n], h32[:, j, :n], [Bh32[j]], [Bh[j]], eng="gpsimd")
                else:
                    act(hT[:, j, :n], tm[:, :n], AF.Identity, [tmb, Bmod], [Bh[j]], bias=mod(i, vsh, j, col), scale=1.0)

        def load_w_bf(dst, dst_b, src_view, nparts=1, axis=1):
            S.dma("gpsimd", dst, src_view, writes=[dst_b])

        def phase_A():
            with ExitStack() as px:
                alloc = mk_alloc(px)
                win = alloc("win", [128, 8, 2048], BF16); Bwin = S.buf("win")
                wout = alloc("wout", [128, 8, 1024], BF16); Bwout = S.buf("wout")
                wsT = alloc("wsT", [128, 8, 128], BF16); BwsT = S.buf("wsT")
                lnv = alloc("lnv", [128, 2, 512]); Blnv = S.buf("lnv")
                bsb = alloc("bsb", [128, 4, 128]); Bbs = S.buf("bs")
                a_pad = alloc("a_pad", [128, 4, T + 30], BF16); Bap = S.bufs("ap", 4)
                xr = Rot(S, alloc, "xa", 2, [128, 8, 512], F32)
                hT = alloc("hT", [128, 8, 512], BF16); Bh = S.bufs("h", 8)
                uT = alloc("uT", [128, 4, 512], BF16); Bu = S.bufs("u", 4)
                vtm = alloc("vtm", [128, 4, 512], BF16); Bv = S.bufs("v", 4)
                ac = alloc("ac", [128, 4, 512]); Bac = S.bufs("ac", 4)
                a2T = alloc("a2T", [128, 4, 512], BF16); Ba2 = S.bufs("a2", 4)
                boT = alloc("boT", [128, 4, 512], BF16); Bbo = S.bufs("bo", 4)
                sqr = Rot(S, alloc, "sqa", 2, [128, 512], F32)
                rst = Rot(S, alloc, "rsa", 4, [128, 512], F32)
                tmr = Rot(S, alloc, "tma", 3, [128, 512], F32)
                diag = alloc("diag", [128, 4, 31, 128], BF16); Bdiag = S.buf("diag")
                for c in range(4):
                    for k in range(31):
                        ts(diag[:, c, k, :], ident[:], vv("convw", c * 31 + k), None, ALU.mult, None, [Bident, Bvecs, Bdiag], [Bdiag])
                smr = Rot(S, alloc, "sma", 8, [128, 8], F32)
                tmps = (sqr, rst, tmr)
                S.dma("gpsimd", win[:], e_w_in.rearrange("(k p) n -> p k n", p=128), writes=[Bwin])
                S.dma("gpsimd", wout[:], e_w_out.rearrange("(k p) n -> p k n", p=128), writes=[Bwout])
                S.dma("gpsimd", wsT[:], wsT_in, writes=[BwsT])
                S.dma("sync", lnv[:], lnv_in, writes=[Blnv])
                S.dma("sync", bsb[:], bs_in, writes=[Bbs])

                def seq(src, dst, dst_bufs, ntok, col):
                    nb = min(512, ntok)
                    nblk = ntok // nb
                    sv, dv = dview(src), dview(dst)
                    for c in range(4):
                        memset(a_pad[:, c, :], 0.0, [Bap[c]])
                    xq = {}

                    def xload(tb):
                        if tb < nblk:
                            xb_, xbb_ = xr.next()
                            S.dma("sync", xb_[:, :, :nb], sv[:, :, tb * nb:(tb + 1) * nb], writes=[xbb_])
                            xq[tb] = (xb_, xbb_)
                    xload(0)
                    for tb in range(nblk):
                        xload(tb + 1)
                        xb, xbb = xq.pop(tb)
                        norm_mod(xb, xbb, nb, 0, 0, col, hT, Bh, tmps)
                        for c in range(4):
                            pv, pvb = bank()
                            pg, pgb = bank()
                            for k in range(8):
                                mm(pv[:, :nb], win[:, k, c * 128:(c + 1) * 128], hT[:, k, :nb], k == 0, k == 7, [Bwin, Bh[k]], pvb)
                            for k in range(8):
                                mm(pg[:, :nb], win[:, k, 512 + c * 128:512 + (c + 1) * 128], hT[:, k, :nb], k == 0, k == 7,
                                   [Bwin, Bh[k]], pgb)
                            sg, sgb = tmr.next()
                            act(sg[:, :nb], pg[:, :nb], AF.Sigmoid, [pgb], [sgb])
                            tt(a_pad[:, c, 15 + tb * nb:15 + (tb + 1) * nb], pv[:, :nb], sg[:, :nb], ALU.mult, [pvb, sgb], [Bap[c]])
                    xload(0)
                    for tb in range(nblk):
                        xload(tb + 1)
                        xb, xbb = xq.pop(tb)
                        norm_mod(xb, xbb, nb, 0, 0, col, hT, Bh, tmps)
                        for c in range(4):
                            pu, pub = bank()
                            for k in range(8):
                                mm(pu[:, :nb], win[:, k, 1024 + c * 128:1024 + (c + 1) * 128], hT[:, k, :nb], k == 0, k == 7,
                                   [Bwin, Bh[k]], pub)
                            act(uT[:, c, :nb], pu[:, :nb], AF.Gelu_apprx_tanh, [pub], [Bu[c]])
                        for t4 in range(nb // 128):
                            pv, pvb = bank()
                            for k in range(8):
                                mm(pv[:, :], hT[:, k, t4 * 128:(t4 + 1) * 128], win[:, k, 1536:2048], k == 0, k == 7, [Bh[k], Bwin], pvb)
                            gv, gvb = tmr.next()
                            act(gv[:, :], pv[:, :], AF.Gelu_apprx_tanh, [pvb], [gvb])
                            st, stb = smr.next()
                            S.op("vector", lambda e, st=st, gv=gv: e.bn_stats(out=st[:, 0:6], in_=gv[:, :]), reads=[gvb], writes=[stb])
                            mv, mvb = smr.next()
                            S.op("vector", lambda e, st=st, mv=mv: e.bn_aggr(out=mv[:, 0:2], in_=st[:, 0:6]), reads=[stb], writes=[mvb])
                            sd, sdb = smr.next()
                            act(sd[:, 0:1], mv[:, 1:2], AF.Sqrt, [mvb, Beps], [sdb], bias=epsc[:], scale=1.0)
                            rs, rsb = smr.next()
                            recip(rs[:, 0:1], sd[:, 0:1], [sdb], [rsb])
                            g2, g2b = tmr.next()
                            ts(g2[:, :], gv[:, :], mv[:, 0:1], rs[:, 0:1], ALU.subtract, ALU.mult, [gvb, mvb, rsb], [g2b])
                            g3, g3b = tmr.next()
                            tt(g3[:, :], g2[:, :], lnv[:, 0, :], ALU.mult, [g2b, Blnv], [g3b], eng="gpsimd")
                            tt(vtm[:, t4, :], g3[:, :], lnv[:, 1, :], ALU.add, [g3b, Blnv], [Bv[t4]], eng="gpsimd")
                        for c in range(4):
                            base = tb * nb
                            pcv_, pcvb_ = bank()
                            for k in range(31):
                                mm(pcv_[:, :nb], diag[:, c, k, :], a_pad[:, c, base + k:base + k + nb], k == 0, k == 30, [Bdiag, Bap[c]], pcvb_)
                            act(ac[:, c, :nb], pcv_[:, :nb], AF.Identity, [pcvb_, Bvecs], [Bac[c]], bias=vv("convb", c), scale=1.0)
                        p1, p1b = bank()
                        p2, p2b = bank()
                        for c in range(4):
                            mm(p1[:, :nb], ones[:], ac[:, c, :nb], c == 0, c == 3, [Bones, Bac[c]], p1b)
                        for c in range(4):
                            sq, sqb = sqr.next()
                            act(sq[:, :nb], ac[:, c, :nb], AF.Square, [Bac[c]], [sqb])
                            mm(p2[:, :nb], ones[:], sq[:, :nb], c == 0, c == 3, [Bones, sqb], p2b)
                        mean, meanb = rst.next()
                        act(mean[:, :nb], p1[:, :nb], AF.Copy, [p1b], [meanb], scale=1.0 / 512)
                        msq, msqb = rst.next()
                        tt(msq[:, :nb], mean[:, :nb], mean[:, :nb], ALU.mult, [meanb], [msqb])
                        var, varb = rst.next()
                        stt(var[:, :nb], p2[:, :nb], 1.0 / 512, msq[:, :nb], ALU.mult, ALU.subtract, [p2b, msqb], [varb])
                        sd, sdb = tmr.next()
                        act(sd[:, :nb], var[:, :nb], AF.Sqrt, [varb, Beps], [sdb], bias=epsc[:], scale=1.0)
                        rs, rsb = rst.next()
                        recip(rs[:, :nb], sd[:, :nb], [sdb], [rsb])
                        for c in range(4):
                            y1, y1b = tmr.next()
                            tt(y1[:, :nb], ac[:, c, :nb], mean[:, :nb], ALU.subtract, [Bac[c], meanb], [y1b])
                            y2, y2b = tmr.next()
                            tt(y2[:, :nb], y1[:, :nb], rs[:, :nb], ALU.mult, [y1b, rsb], [y2b])
                            act(a2T[:, c, :nb], y2[:, :nb], AF.Silu, [y2b, Bvecs], [Ba2[c]], bias=vv("lnab", c), scale=vv("lnag", c))
                        for t4 in range(nb // 128):
                            ps_, psb = bank()
                            for jp in range(4):
                                for hh in range(2):
                                    h = 2 * jp + hh
                                    mm(ps_[hh * 64:(hh + 1) * 64, jp * 128:(jp + 1) * 128], vtm[:, t4, h * 64:(h + 1) * 64], wsT[:, h, :],
                                       True, True, [Bv[t4], BwsT], psb)
                            sv1, sv1b = tmr.next()
                            tt(sv1[:, :], ps_[:, :], bsb[:].rearrange("p a b -> p (a b)"), ALU.add, [psb, Bbs], [sv1b])
                            tt(boT[:, :, t4 * 128:(t4 + 1) * 128], sv1[:, :].rearrange("p (a b) -> p a b", a=4),
                               uT[:, :, t4 * 128:(t4 + 1) * 128], ALU.mult, [sv1b] + Bu, Bbo)
                        xn, xnb = xb, xbb
                        for dm in range(8):
                            po, pob = bank()
                            for c in range(4):
                                mm(po[:, :nb], wout[:, c, dm * 128:(dm + 1) * 128], a2T[:, c, :nb], c == 0, False, [Bwout, Ba2[c]], pob)
                            for c in range(4):
                                mm(po[:, :nb], wout[:, 4 + c, dm * 128:(dm + 1) * 128], boT[:, c, :nb], False, c == 3, [Bwout, Bbo[c]], pob)
                            stt(xn[:, dm, :nb], po[:, :nb], mod(0, 2, dm, col), xb[:, dm, :nb], ALU.mult, ALU.add,
                                [pob, Bmod, xbb], [xnb])
                        S.dma("sync", dv[:, :, tb * nb:(tb + 1) * nb], xn[:, :, :nb], reads=[xnb], writes=[dst_bufs[tb]])

                seq(cT_in, cs, Bcs, TC, 1)
                seq(xT_in, xs, Bxs, T, 0)

        def ffn_engine(alloc):
            st = {}
            st["w1"] = Rot(S, alloc, "w1g", 3, [128, 8, 512], BF16)
            st["w3"] = Rot(S, alloc, "w3g", 3, [128, 8, 512], BF16)
            st["w2"] = Rot(S, alloc, "w2g", 3, [128, 4, 1024], BF16)
            st["g"] = Rot(S, alloc, "gg", 8, [128, 512], BF16)
            st["s1"] = Rot(S, alloc, "s1", 3, [128, 512], F32)
            st["t2"] = Rot(S, alloc, "t2", 3, [128, 512], F32)
            return st

        def static_loader(w1, w3, w2):
            w1v = w1.rearrange("(k p) f -> p k f", p=128)
            w3v = w3.rearrange("(k p) f -> p k f", p=128)

            def load(f0, gs, w1t, w1b, w3t, w3b, w2t, w2b):
                S.dma("gpsimd", w1t[:, :, :gs * 128], w1v[:, :, f0 * 128:(f0 + gs) * 128], writes=[w1b])
                S.dma("gpsimd", w3t[:, :, :gs * 128], w3v[:, :, f0 * 128:(f0 + gs) * 128], writes=[w3b])
                S.dma("gpsimd", w2t[:, :gs, :], w2[f0 * 128:(f0 + gs) * 128, :].rearrange("(f p) d -> p f d", p=128), writes=[w2b])
            return load

        def ffn_run(st, hT, Bh, n, loaders, nchunks, gb, yacc, By, next_loader=None):
            groups = []
            f0 = 0
            while f0 < nchunks:
                gs = min(4, nchunks - f0)
                groups.append((f0, gs))
                f0 += gs
            items = [(ei, ld, f0, gs) for ei, ld in enumerate(loaders) for (f0, gs) in groups]
            nI = len(items)
            wt = {}

            def issue_load(i):
                ei, ld, f0, gs = items[i]
                w1t, w1b = st["w1"].next()
                w3t, w3b = st["w3"].next()
                w2t, w2b = st["w2"].next()
                ld(f0, gs, w1t, w1b, w3t, w3b, w2t, w2b)
                wt[i] = (w1t, w1b, w3t, w3b, w2t, w2b)

            def up(i):
                ei, ld, f0, gs = items[i]
                w1t, w1b, w3t, w3b, w2t, w2b = wt[i]
                gts = []
                for fi in range(gs):
                    pa, pab = bank()
                    pb_, pbb = bank()
                    for k in range(8):
                        mm(pa[:, :n], w1t[:, k, fi * 128:(fi + 1) * 128], hT[:, k, :n], k == 0, k == 7, [w1b, Bh[k]], pab)
                    for k in range(8):
                        mm(pb_[:, :n], w3t[:, k, fi * 128:(fi + 1) * 128], hT[:, k, :n], k == 0, k == 7, [w3b, Bh[k]], pbb)
                    s1, s1b = st["s1"].next()
                    act(s1[:, :n], pa[:, :n], AF.Silu, [pab], [s1b])
                    g, gbuf = st["g"].next()
                    if gb is None:
                        tt(g[:, :n], s1[:, :n], pb_[:, :n], ALU.mult, [s1b, pbb], [gbuf])
                    else:
                        t2, t2b = st["t2"].next()
                        tt(t2[:, :n], s1[:, :n], pb_[:, :n], ALU.mult, [s1b, pbb], [t2b])
                        gap, gapb = gb(ei)
                        tt(g[:, :n], t2[:, :n], gap, ALU.mult, [t2b, gapb], [gbuf], eng="gpsimd")
                    gts.append((g, gbuf))
                return gts

            def down(i, gts):
                ei, ld, f0, gs = items[i]
                w1t, w1b, w3t, w3b, w2t, w2b = wt.pop(i)
                for dm in range(8):
                    pd, pdb = bank()
                    for fi in range(gs):
                        mm(pd[:, :n], w2t[:, fi, dm * 128:(dm + 1) * 128], gts[fi][0][:, :n], fi == 0, fi == gs - 1,
                           [w2b, gts[fi][1]], pdb)
                    if i == 0:
                        act(yacc[:, dm, :n], pd[:, :n], AF.Copy, [pdb], [By[dm]])
                    else:
                        tt(yacc[:, dm, :n], yacc[:, dm, :n], pd[:, :n], ALU.add, [By[dm], pdb], [By[dm]])

            pre = st.pop("pre", None)
            if pre is not None:
                wt.update(pre)
            else:
                issue_load(0)
                issue_load(1)
            nxt = {}
            prev = None
            for i in range(nI + 1):
                cur = up(i) if i < nI else None
                if prev is not None:
                    down(i - 1, prev)
                if i + 2 < nI:
                    issue_load(i + 2)
                elif next_loader is not None and i + 2 - nI < 2:
                    kk = i + 2 - nI
                    f0n, gsn = groups[kk]
                    w1t, w1b = st["w1"].next()
                    w3t, w3b = st["w3"].next()
                    w2t, w2b = st["w2"].next()
                    next_loader(f0n, gsn, w1t, w1b, w3t, w3b, w2t, w2b)
                    nxt[kk] = (w1t, w1b, w3t, w3b, w2t, w2b)
                prev = cur
            if next_loader is not None:
                st["pre"] = nxt

        def phase_B():
            with ExitStack() as px:
                alloc = mk_alloc(px)
                st = ffn_engine(alloc)
                xr = Rot(S, alloc, "xb", 3, [128, 8, 512], F32)
                hTs = [alloc(f"hTb{i}", [128, 8, 512], BF16) for i in range(2)]
                Bhs_ = [S.bufs(f"hb{i}_", 8) for i in range(2)]
                yacc = alloc("yaccb", [128, 8, 512]); By = S.bufs("yb", 8)
                sqr = Rot(S, alloc, "sqb", 2, [128, 512], F32)
                rst = Rot(S, alloc, "rsb", 2, [128, 512], F32)
                tmr = Rot(S, alloc, "tmb", 3, [128, 512], F32)
                tmps = (sqr, rst, tmr)

                fl = static_loader(f_w1, f_w3, f_w2)
                blocks = [(cs, Bcs, 0, TC, 1)] + [(xs, Bxs, tb, 512, 0) for tb in range(8)]

                xl = {}

                def load(i):
                    if i >= len(blocks):
                        return
                    buf_ap, bufs, tb, nb, col = blocks[i]
                    v = dview(buf_ap)
                    xb, xbb = xr.next()
                    S.dma("sync", xb[:, :, :nb], v[:, :, tb * nb:(tb + 1) * nb], reads=[bufs[tb]], writes=[xbb])
                    xl[i] = (xb, xbb)

                def prep(i):
                    buf_ap, bufs, tb, nb, col = blocks[i]
                    xb, xbb = xl[i]
                    norm_mod(xb, xbb, nb, 0, 1, col, hTs[i % 2], Bhs_[i % 2], tmps)
                    return xb, xbb

                load(0)
                load(1)
                cur = prep(0)
                for i in range(len(blocks)):
                    buf_ap, bufs, tb, nb, col = blocks[i]
                    v = dview(buf_ap)
                    load(i + 2)
                    nxt = prep(i + 1) if i + 1 < len(blocks) else None
                    xb, xbb = cur
                    ffn_run(st, hTs[i % 2], Bhs_[i % 2], nb, [fl], 22, None, yacc, By, next_loader=fl if i + 1 < len(blocks) else None)
                    for dm in range(8):
                        stt(xb[:, dm, :nb], yacc[:, dm, :nb], mod(0, 5, dm, col), xb[:, dm, :nb], ALU.mult, ALU.add,
                            [By[dm], Bmod, xbb], [xbb])
                    S.dma("sync", v[:, :, tb * nb:(tb + 1) * nb], xb[:, :, :nb], reads=[xbb], writes=[bufs[tb]])
                    cur = nxt

        NK = TC + T
        SCALE = 192.0 ** -0.5

        def phase_C():
            with ExitStack() as px:
                alloc = mk_alloc(px)
                qn = alloc("qn", [128, 3, T], BF16); Bqn = S.bufs("qn", 8)
                kvn = alloc("kvn", [128, 2, NK], BF16); Bkvn = S.bufs("kvn", 9)
                kpe2 = alloc("kpe2", [128, 2, NK], BF16); Bkpe = S.bufs("kpe", 9); Bkz = S.buf("kpez")
                S.op("gpsimd", lambda e: e.memset(kpe2[:], 0.0), writes=[Bkz])
                qrp = alloc("qrp", [128, 4, T], BF16); Bqrp = S.bufs("qrp", 8)
                Batt = [S.bufs(f"att{h}_", 8) for h in range(8)]
                wuq = alloc("wuq", [128, 3, 1024], BF16); Bwuq = S.buf("wuq")
                wukv = alloc("wukv", [128, 2, 2048], BF16); Bwukv = S.buf("wukv")
                S.dma("gpsimd", wuq[:], o_wuq.rearrange("(k p) n -> p k n", p=128)[:, :, 0:1024], writes=[Bwuq])
                S.dma("gpsimd", wukv[:], o_wukv.rearrange("(k p) n -> p k n", p=128), writes=[Bwukv])
                with ExitStack() as p1:
                    al1 = mk_alloc(p1)
                    wuqr = al1("wuqr", [128, 3, 1024], BF16); Bwuqr = S.buf("wuqr")
                    S.dma("gpsimd", wuqr[:], o_wuq.rearrange("(k p) n -> p k n", p=128)[:, :, 1024:2048], writes=[Bwuqr])
                    wi = al1("wi", [128, 8, 896], BF16); Bwi = S.buf("wi")
                    S.dma("gpsimd", wi[:], o_w_in.rearrange("(k p) n -> p k n", p=128), writes=[Bwi])
                    xr = Rot(S, al1, "xc", 1, [128, 8, 512], F32)
                    hTs = [al1(f"hTc{i}", [128, 8, 512], BF16) for i in range(2)]
                    Bhs2 = [S.bufs(f"hc{i}_", 8) for i in range(2)]
                    zl = al1("zl", [128, 5, 512]); Bzl = S.bufs("zl", 5)
                    rp = Rot(S, al1, "rp", 1, [128, 2, 512], F32)
                    sqr = Rot(S, al1, "sqc", 2, [128, 512], F32)
                    rst = Rot(S, al1, "rsc", 4, [128, 512], F32)
                    tmr = Rot(S, al1, "tmc", 4, [128, 512], F32)
                    tmps = (sqr, rst, tmr)

                    lblocks = [(cs, Bcs, 0, TC, 1, 0, 0, False)] + [(xs, Bxs, tb, 512, 0, TC, 1, True) for tb in range(8)]

                    def lprep(i):
                        src, src_bufs, tb, nb, col, kbase, kb0, is_x = lblocks[i]
                        v = dview(src)
                        xb, xbb = xr.next()
                        S.dma("sync", xb[:, :, :nb], v[:, :, tb * nb:(tb + 1) * nb], reads=[src_bufs[tb]], writes=[xbb])
                        norm_mod(xb, xbb, nb, 1, 0, col, hTs[i % 2], Bhs2[i % 2], tmps)

                    lprep(0)
                    for li in range(len(lblocks)):
                        src, src_bufs, tb, nb, col, kbase, kb0, is_x = lblocks[li]
                        hT, Bh = hTs[li % 2], Bhs2[li % 2]
                        if li + 1 < len(lblocks):
                            lprep(li + 1)
                        if True:
                            kc0 = kbase + tb * nb
                            for c in (range(5) if is_x else range(3, 5)):
                                pz, pzb = bank()
                                for k in range(8):
                                    mm(pz[:, :nb], wi[:, k, c * 128:(c + 1) * 128], hT[:, k, :nb], k == 0, k == 7, [Bwi, Bh[k]], pzb)
                                act(zl[:, c, :nb], pz[:, :nb], AF.Copy, [pzb], [Bzl[c]])
                            if is_x:
                                r, rb = rms_stat(lambda j: zl[:, j, :nb], lambda j: Bzl[j], nb, (sqr, rst), 3, 1.0 / 384)
                                for c in range(3):
                                    stt(qn[:, c, tb * nb:(tb + 1) * nb], zl[:, c, :nb], vv("gq", c), r[:, :nb], ALU.mult, ALU.mult,
                                        [Bzl[c], Bvecs, rb], [Bqn[tb]])
                            r, rb = rms_stat(lambda j: zl[:, 3 + j, :nb], lambda j: Bzl[3 + j], nb, (sqr, rst), 2, 1.0 / 256)
                            for c in range(2):
                                stt(kvn[:, c, kc0:kc0 + nb], zl[:, 3 + c, :nb], vv("gkv", c), r[:, :nb], ALU.mult, ALU.mult,
                                    [Bzl[3 + c], Bvecs, rb], [Bkvn[kb0 + tb]])
                            pk, pkb = bank()
                            for k in range(8):
                                mm(pk[:, :nb], wi[:, k, 640:768], hT[:, k, :nb], k == 0, k == 7, [Bwi, Bh[k]], pkb)
                            if not is_x:
                                act(kpe2[0:64, 0, kc0:kc0 + nb], pk[0:64, :nb], AF.Copy, [pkb, Bkz], [Bkpe[kb0 + tb]])
                                vcopy(kpe2[64:128, 1, kc0:kc0 + nb], pk[64:128, :nb], [pkb, Bkz, Bkpe[kb0 + tb]], [Bkpe[kb0 + tb]])
                            else:
                                pw, pwb = bank()
                                for k in range(8):
                                    mm(pw[:, :nb], wi[:, k, 768:896], hT[:, k, :nb], k == 0, k == 7, [Bwi, Bh[k]], pwb)
                                rt, rtb = rp.next()
                                S.dma("sync", rt[:, :, :nb], rope_in[:, :, tb * nb:(tb + 1) * nb], writes=[rtb])
                                t1, t1b = tmr.next()
                                tt(t1[:, :nb], pk[:, :nb], rt[:, 0, :nb], ALU.mult, [pkb, rtb], [t1b])
                                t2, t2b = tmr.next()
                                tt(t2[:, :nb], pw[:, :nb], rt[:, 1, :nb], ALU.mult, [pwb, rtb], [t2b])
                                tt(kpe2[0:64, 0, kc0:kc0 + nb], t1[0:64, :nb], t2[0:64, :nb], ALU.add, [t1b, t2b, Bkz], [Bkpe[kb0 + tb]], eng="gpsimd")
                                tt(kpe2[64:128, 1, kc0:kc0 + nb], t1[64:128, :nb], t2[64:128, :nb], ALU.add, [t1b, t2b, Bkz, Bkpe[kb0 + tb]],
                                   [Bkpe[kb0 + tb]], eng="gpsimd")
                                for hp in range(4):
                                    pq, pqb = bank()
                                    pq2, pq2b = bank()
                                    for k in range(3):
                                        mm(pq[:, :nb], wuqr[:, k, hp * 128:(hp + 1) * 128], qn[:, k, tb * nb:(tb + 1) * nb],
                                           k == 0, k == 2, [Bwuqr, Bqn[tb]], pqb)
                                    for k in range(3):
                                        mm(pq2[:, :nb], wuqr[:, k, 512 + hp * 128:512 + (hp + 1) * 128], qn[:, k, tb * nb:(tb + 1) * nb],
                                           k == 0, k == 2, [Bwuqr, Bqn[tb]], pq2b)
                                    t1, t1b = tmr.next()
                                    tt(t1[:, :nb], pq[:, :nb], rt[:, 0, :nb], ALU.mult, [pqb, rtb], [t1b])
                                    t2, t2b = tmr.next()
                                    tt(t2[:, :nb], pq2[:, :nb], rt[:, 1, :nb], ALU.mult, [pq2b, rtb], [t2b])
                                    tt(qrp[:, hp, tb * nb:(tb + 1) * nb], t1[:, :nb], t2[:, :nb], ALU.add, [t1b, t2b], [Bqrp[tb]], eng="gpsimd")
                S.barrier()
                att = alloc("att", [128, 8, T], BF16)
                with ExitStack() as p2:
                    al2 = mk_alloc(p2)
                    KT = al2("KT", [128, NK], BF16); BKT = S.buf("KT")
                    Vh = al2("Vh", [128, 34, 128], BF16); BVh = S.buf("Vh")
                    Qh = al2("Qh", [128, T], BF16); BQh = S.buf("Qh")
                    pr = Rot(S, al2, "pT", 4, [128, 512], BF16)
                    rr = Rot(S, al2, "rr", 1, [128, 512], F32)
                    sb_banks = (0, 1, 2, 3)
                    acc_o = (4, 5)
                    acc_s = (6, 7)
                    allkv = Bkvn
                    for h in range(8):
                        hh, hp = h % 2, h // 2
                        for cb in range(0, NK, 512):
                            w = min(512, NK - cb)
                            pk, pkb = bank(sb_banks)
                            for k in range(2):
                                mm(pk[:, :w], wukv[:, k, h * 128:(h + 1) * 128], kvn[:, k, cb:cb + w], k == 0, k == 1, [Bwukv] + allkv, pkb)
                            act(KT[:, cb:cb + w], pk[:, :w], AF.Copy, [pkb], [BKT])
                        for kt0 in range(0, 34, 4):
                            nt = min(4, 34 - kt0)
                            pv, pvb = bank(sb_banks)
                            for i4 in range(nt):
                                kt = kt0 + i4
                                for k in range(2):
                                    mm(pv[:, i4 * 128:(i4 + 1) * 128], kvn[:, k, kt * 128:(kt + 1) * 128],
                                       wukv[:, k, 1024 + h * 128:1024 + (h + 1) * 128], k == 0, k == 1, [Bwukv] + allkv, pvb)
                            vcopy(Vh[:, kt0:kt0 + nt, :], pv[:, :nt * 128].rearrange("p (a b) -> p a b", a=nt), [pvb], [BVh])
                        for qb in range(8):
                            pq, pqb = bank(sb_banks)
                            for k in range(3):
                                mm(pq[:, :], wuq[:, k, h * 128:(h + 1) * 128], qn[:, k, qb * 512:(qb + 1) * 512], k == 0, k == 2,
                                   [Bwuq, Bqn[qb]], pqb)
                            act(Qh[:, qb * 512:(qb + 1) * 512], pq[:, :], AF.Copy, [pqb], [BQh])
                        for qb in range(8):
                            po, pob = bank(acc_o)
                            pS, pSb = bank(acc_s)
                            LAG = 2
                            pts = {}
                            for kt in range(34 + LAG):
                                if kt < 34:
                                    ps_, psb = bank(sb_banks)
                                    mm(ps_[:, :], KT[:, kt * 128:(kt + 1) * 128], Qh[:, qb * 512:(qb + 1) * 512], True, False, [BKT, BQh], psb)
                                    mm(ps_[:, :], kpe2[:, hh, kt * 128:(kt + 1) * 128],
                                       qrp[:, hp, qb * 512:(qb + 1) * 512], False, True, Bkpe + [Bqrp[qb]], psb)
                                    pT, pTb = pr.next()
                                    act(pT[:, :], ps_[:, :], AF.Exp, [psb], [pTb], scale=SCALE)
                                    pts[kt] = (pT, pTb)
                                if kt >= LAG:
                                    k2 = kt - LAG
                                    pT, pTb = pts.pop(k2)
                                    mm(po[:, :], Vh[:, k2, :], pT[:, :], k2 == 0, k2 == 33, [BVh, pTb], pob)
                                    mm(pS[:, :], onesb[:], pT[:, :], k2 == 0, k2 == 33, [Bonesb, pTb], pSb)
                            rc, rcb = rr.next()
                            recip(rc[:, :], pS[:, :], [pSb], [rcb])
                            tt(att[:, h, qb * 512:(qb + 1) * 512], po[:, :], rc[:, :], ALU.mult, [pob, rcb], [Batt[h][qb]])
                S.barrier()
                with ExitStack() as p3:
                    al3 = mk_alloc(p3)
                    wo = al3("wo", [128, 8, 1024], BF16); Bwo = S.buf("wo")
                    S.dma("gpsimd", wo[:], o_w_o.rearrange("(k p) n -> p k n", p=128), writes=[Bwo])
                    xr = Rot(S, al3, "xc3", 1, [128, 8, 512], F32)
                    v = dview(xs)
                    for tb in range(8):
                        xb, xbb = xr.next()
                        S.dma("sync", xb[:], v[:, :, tb * 512:(tb + 1) * 512], reads=[Bxs[tb]], writes=[xbb])
                        xn, xnb = xb, xbb
                        for dm in range(8):
                            po, pob = bank()
                            for h in range(8):
                                mm(po[:, :], wo[:, h, dm * 128:(dm + 1) * 128], att[:, h, tb * 512:(tb + 1) * 512], h == 0, h == 7,
                                   [Bwo, Batt[h][tb]], pob)
                            stt(xn[:, dm, :], po[:, :], mod(1, 2, dm, 0), xb[:, dm, :], ALU.mult, ALU.add, [pob, Bmod, xbb], [xnb])
                        S.dma("sync", v[:, :, tb * 512:(tb + 1) * 512], xn[:], reads=[xnb], writes=[Bxs[tb]])

        def phase_D():
            with ExitStack() as px:
                alloc = mk_alloc(px)
                st = ffn_engine(alloc)
                rt32 = alloc("rt32", [128, 8, 8]); Brt = S.buf("rt32")
                selt = alloc("selt", [8, 8, 128]); Bsel = S.buf("sel")
                S.dma("sync", rt32[:], o_router.rearrange("(k p) e -> p k e", p=128), writes=[Brt])
                S.dma("sync", selt[:], sel_in, writes=[Bsel])
                xr = Rot(S, alloc, "xd", 2, [128, 8, 512], F32)
                hT = alloc("hTd", [128, 8, 512], BF16); Bh = S.bufs("hd", 8)
                h32 = alloc("h32", [128, 8, 512]); Bh32 = S.bufs("h32_", 8)
                yacc = alloc("yaccd", [128, 8, 512]); By = S.bufs("yd", 8)
                gbt = alloc("gbt", [128, 8, 512]); Bgb = S.bufs("gb", 8)
                gT = alloc("gT", [8, 512]); BgT = S.buf("gT")
                sqr = Rot(S, alloc, "sqd", 2, [128, 512], F32)
                rst = Rot(S, alloc, "rsd", 2, [128, 512], F32)
                tmr = Rot(S, alloc, "tmd", 3, [128, 512], F32)
                sm = Rot(S, alloc, "smd", 12, [128, 8], F32)
                tmps = (sqr, rst, tmr)
                v = dview(xs)
                ov = dview(outT)
                experts = []
                for tb in range(8):
                    xb, xbb = xr.next()
                    S.dma("sync", xb[:], v[:, :, tb * 512:(tb + 1) * 512], reads=[Bxs[tb]], writes=[xbb])
                    norm_mod(xb, xbb, 512, 1, 1, 0, hT, Bh, tmps, h32=h32, Bh32=Bh32)
                    for t4 in range(4):
                        pl, plb = bank()
                        for k in range(8):
                            mm(pl[:, 0:8], h32[:, k, t4 * 128:(t4 + 1) * 128], rt32[:, k, :], k == 0, k == 7, [Bh32[k], Brt], plb)
                        lg, lgb = sm.next()
                        vcopy(lg[:, :], pl[:, 0:8], [plb], [lgb])
                        mx8, mxb = sm.next()
                        S.op("vector", lambda e, mx8=mx8, lg=lg: e.max(out=mx8[:, :], in_=lg[:, :]), reads=[lgb], writes=[mxb])
                        nm, nmb = sm.next()
                        ts(nm[:, 0:1], mx8[:, 0:1], -1.0, None, ALU.mult, None, [mxb], [nmb])
                        ex, exb = sm.next()
                        act(ex[:, :], lg[:, :], AF.Exp, [lgb, nmb], [exb], bias=nm[:, 0:1], scale=1.0)
                        mk, mkb = sm.next()
                        ts(mk[:, :], lg[:, :], mx8[:, 1:2], None, ALU.is_ge, None, [lgb, mxb], [mkb])
                        me, meb = sm.next()
                        tt(me[:, :], mk[:, :], ex[:, :], ALU.mult, [mkb, exb], [meb])
                        dn, dnb = sm.next()
                        S.op("vector", lambda e, dn=dn, me=me: e.tensor_reduce(out=dn[:, 0:1], in_=me[:, :], axis=mybir.AxisListType.X, op=ALU.add),
                             reads=[meb], writes=[dnb])
                        rd, rdb = sm.next()
                        recip(rd[:, 0:1], dn[:, 0:1], [dnb], [rdb])
                        gt, gtb = sm.next()
                        ts(gt[:, :], me[:, :], rd[:, 0:1], None, ALU.mult, None, [meb, rdb], [gtb])
                        ptr, ptrb = bank()
                        S.op("tensor", lambda e, ptr=ptr, gt=gt: e.transpose(out=ptr[0:8, 0:128], in_=gt[:, :], identity=ident[:]),
                             reads=[gtb, Bident], writes=[ptrb])
                        vcopy(gT[:, t4 * 128:(t4 + 1) * 128], ptr[0:8, 0:128], [ptrb], [BgT])
                    for e_ in range(8):
                        pg, pgb = bank()
                        mm(pg[:, :], selt[:, e_, :], gT[:, :], True, True, [Bsel, BgT], pgb)
                        act(gbt[:, e_, :], pg[:, :], AF.Copy, [pgb], [Bgb[e_]])
                    ffn_run(st, hT, Bh, 512, experts, 28, lambda ei: (gbt[:, ei, :], Bgb[ei]), yacc, By)
                    xn, xnb = xb, xbb
                    for dm in range(8):
                        stt(xn[:, dm, :], yacc[:, dm, :], mod(1, 5, dm, 0), xb[:, dm, :], ALU.mult, ALU.add, [By[dm], Bmod, xbb], [xnb])
                    r, rb = rms_stat(lambda j: xn[:, j, :], xnb, 512, (sqr, rst), 8, 1.0 / D)
                    for dm in range(8):
                        stt(yacc[:, dm, :], xn[:, dm, :], vv("fg", dm), r[:, :], ALU.mult, ALU.mult, [xnb, Bvecs, rb], [By[dm]])
                    S.dma("sync", ov[:, :, tb * 512:(tb + 1) * 512], yacc[:], reads=By, writes=[Bout[tb]])


        I32 = mybir.dt.int32
        JT = 24

        def phase_D2():
            with ExitStack() as px:
                alloc = mk_alloc(px)
                rt32 = alloc("rt32", [128, 8, 8]); Brt = S.buf("rt32")
                utri = alloc("utri", [128, 128]); Butri = S.buf("utri")
                ibase = alloc("ibase", [128, 7]); Bib = S.buf("ibase")
                identb = alloc("identb", [128, 128], BF16); Bidb = S.buf("identb")
                S.dma("sync", rt32[:], o_router.rearrange("(k p) e -> p k e", p=128), writes=[Brt])
                S.dma("sync", utri[:], utri_in, writes=[Butri])
                S.dma("sync", ibase[:], ibase_in, writes=[Bib])
                vcopy(identb[:], ident[:], [Bident], [Bidb])
                M_all = alloc("M_all", [128, 32, 8]); BM = S.bufs("Mall", 32)
                G_all = alloc("G_all", [128, 32, 8]); BGa = S.bufs("Gall", 32)
                R_all = alloc("R_all", [128, 32, 8]); BR = S.buf("Rall")
                P12f = alloc("P12f", [128, 64]); BP12f = S.bufs("P12f", 32)
                P12i = alloc("P12i", [128, 64], I32); BP12i = S.buf("P12i")
                G12 = alloc("G12", [128, 64]); BG12 = S.bufs("G12", 32)
                idxf = alloc("idxf", [128, JT * 7]); Bidxf = S.buf("idxf")
                idxi = alloc("idxi", [128, JT * 7], I32); Bidxi = S.buf("idxi")
                Bhz = S.buf("hz"); Bhs = S.buf("hs"); Bys = S.buf("ys")
                Bs2z = S.buf("s2z"); Bs2t = S.buf("s2t"); Bytok = S.buf("ytok")
                tokid = alloc("tokid", [128, 64, 16], I32); Btokid = S.buf("tokid")
                S.dma("sync", tokid[:], tokid_in, writes=[Btokid])
                with ExitStack() as p1:
                    al1 = mk_alloc(p1)
                    h_tm = al1("h_tm", [128, 32, 1024], BF16); Bhtm = S.bufs("htm", 32)
                    zt = al1("zt", [128, 8, 1024], BF16); Bzt = S.buf("zt")
                    S.op("gpsimd", lambda e: e.memset(zt[:], 0.0), writes=[Bzt])
                    for r in range(NS // 1024):
                        S.dma("sync", hsort[r * 1024:(r + 1) * 1024, :].rearrange("(r p) d -> p r d", p=128), zt[:],
                              reads=[Bzt], writes=[Bhz], join=True)
                    oobt = al1("oobt", [128, 96 * 16], I32); Boob = S.buf("oobt")
                    S.dma("sync", oobt[:], oobfill_in, writes=[Boob])
                    S.dma("sync", slot2tok.rearrange("(p r) o -> p (r o)", p=128), oobt[:], reads=[Boob], writes=[Bs2z])
                    xr = Rot(S, al1, "xd", 2, [128, 8, 512], F32)
                    h32s = [al1(f"h32_{i}", [128, 8, 512]) for i in range(2)]
                    Bh32s = [S.bufs(f"h32_{i}_", 8) for i in range(2)]
                    sqr = Rot(S, al1, "sqd", 2, [128, 512], F32)
                    rst = Rot(S, al1, "rsd", 2, [128, 512], F32)
                    tmr = Rot(S, al1, "tmd", 3, [128, 512], F32)
                    sm = Rot(S, al1, "smd", 48, [128, 8], F32)
                    tmps = (sqr, rst, tmr)
                    v = dview(xs)
                    for tb in range(8):
                        xb, xbb = xr.next()
                        S.dma("sync", xb[:], v[:, :, tb * 512:(tb + 1) * 512], reads=[Bxs[tb]], writes=[xbb])
                        h32, Bh32 = h32s[tb % 2], Bh32s[tb % 2]
                        norm_mod(xb, xbb, 512, 1, 1, 0, None, None, tmps, h32=h32, Bh32=Bh32)
                        for t4 in range(4):
                            c = tb * 4 + t4
                            pl, plb = bank()
                            for k in range(8):
                                mm(pl[:, 0:8], h32[:, k, t4 * 128:(t4 + 1) * 128], rt32[:, k, :], k == 0, k == 7, [Bh32[k], Brt], plb)
                            lg, lgb = sm.next()
                            vcopy(lg[:, :], pl[:, 0:8], [plb], [lgb])
                            mx8, mxb = sm.next()
                            S.op("vector", lambda e, mx8=mx8, lg=lg: e.max(out=mx8[:, :], in_=lg[:, :]), reads=[lgb], writes=[mxb])
                            nm, nmb = sm.next()
                            ts(nm[:, 0:1], mx8[:, 0:1], -1.0, None, ALU.mult, None, [mxb], [nmb])
                            ex, exb = sm.next()
                            act(ex[:, :], lg[:, :], AF.Exp, [lgb, nmb], [exb], bias=nm[:, 0:1], scale=1.0)
                            ts(M_all[:, c, :], lg[:, :], mx8[:, 1:2], None, ALU.is_ge, None, [lgb, mxb], [BM[c]])
                            me, meb = sm.next()
                            tt(me[:, :], M_all[:, c, :], ex[:, :], ALU.mult, [BM[c], exb], [meb])
                            dn, dnb = sm.next()
                            S.op("vector", lambda e, dn=dn, me=me: e.tensor_reduce(out=dn[:, 0:1], in_=me[:, :], axis=mybir.AxisListType.X, op=ALU.add),
                                 reads=[meb], writes=[dnb])
                            rd, rdb = sm.next()
                            recip(rd[:, 0:1], dn[:, 0:1], [dnb], [rdb])
                            ts(G_all[:, c, :], me[:, :], rd[:, 0:1], None, ALU.mult, None, [meb, rdb], [BGa[c]])
                            for half in range(2):
                                pt, ptb = bank()
                                for q in range(4):
                                    dk = half * 4 + q
                                    S.op("tensor", lambda e, pt=pt, q=q, dk=dk, t4=t4, h32=h32: e.transpose(
                                        out=pt[:, q * 128:(q + 1) * 128], in_=h32[:, dk, t4 * 128:(t4 + 1) * 128], identity=ident[:]),
                                        reads=[Bh32[dk], Bident], writes=[ptb])
                                if half == 0:
                                    act(h_tm[:, c, 0:512], pt[:, :], AF.Copy, [ptb], [Bhtm[c]])
                                else:
                                    vcopy(h_tm[:, c, 512:1024], pt[:, :], [ptb, Bhtm[c]], [Bhtm[c]])
                    sm2 = Rot(S, al1, "sm2", 24, [128, 8], F32)
                    pc_, pcb = bank()
                    for c in range(32):
                        mm(pc_[:, 0:8], ones[:], M_all[:, c, :], c == 0, c == 31, [Bones, BM[c]], pcb)
                    keep = Rot(S, al1, "keep", 5, [128, 8], F32)
                    cnt, cntb = keep.next()
                    vcopy(cnt[:, :], pc_[:, 0:8], [pcb], [cntb])
                    prk, prkb = bank()
                    for c in range(32):
                        mm(prk[:, c * 8:(c + 1) * 8], utri[:], M_all[:, c, :], True, c == 0, [Butri, BM[c]], prkb)
                        for c2 in range(c):
                            mm(prk[:, c * 8:(c + 1) * 8], ones[:], M_all[:, c2, :], False, c2 == c - 1, [Bones, BM[c2]], prkb)
                    vcopy(R_all[:].rearrange("p a b -> p (a b)"), prk[:, 0:256], [prkb], [BR])
                    tl, tlb = keep.next()
                    ts(tl[:, :], cnt[:, :], 0.0, None, ALU.is_gt, None, [cntb], [tlb])
                    for m in range(1, 8):
                        stt(tl[:, :], cnt[:, :], 512.0 * m, tl[:, :], ALU.is_gt, ALU.add, [cntb, tlb], [tlb])
                    pcv, pcvb = keep.next()
                    ts(pcv[:, :], tl[:, :], 512.0, None, ALU.mult, None, [tlb], [pcvb])
                    off, offb = keep.next()
                    memset(off[:, 0:1], 0.0, [offb])
                    for e_ in range(1, 8):
                        tt(off[:, e_:e_ + 1], off[:, e_ - 1:e_], pcv[:, e_ - 1:e_], ALU.add, [offb, pcvb], [offb])
                    endv, endb = keep.next()
                    tt(endv[:, :], off[:, :], pcv[:, :], ALU.add, [offb, pcvb], [endb])
                    ej = al1("ej", [128, JT]); Bej = S.buf("ej")
                    for j in range(JT):
                        cm, cmb = sm2.next()
                        ts(cm[:, :], endv[:, :], float(j * 512), None, ALU.is_le, None, [endb], [cmb])
                        S.op("vector", lambda e, cm=cm, j=j: e.tensor_reduce(out=ej[:, j:j + 1], in_=cm[:, :], axis=mybir.AxisListType.X, op=ALU.add),
                             reads=[cmb], writes=[Bej])
                    ej2 = al1("ej2", [128, JT]); Bej2 = S.buf("ej2")
                    ts(ej2[:, :], ej[:, :], 7.0, 1792.0, ALU.min, ALU.mult, [Bej], [Bej2])
                    for j in range(JT):
                        ts(idxf[:, j * 7:(j + 1) * 7], ibase[:, :], ej2[:, j:j + 1], None, ALU.add, None, [Bib, Bej2, Bidxf], [Bidxf])
                    vcopy(idxi[:, :], idxf[:, :], [Bidxf], [Bidxi])
                    for c in range(32):
                        a1, a1b = sm2.next()
                        stt(a1[:, :], R_all[:, c, :], 1.0, off[:, :], ALU.add, ALU.add, [BR, offb], [a1b])
                        a2, a2b = sm2.next()
                        tt(a2[:, :], a1[:, :], M_all[:, c, :], ALU.mult, [a1b, BM[c]], [a2b])
                        pm, pmb = sm2.next()
                        ts(pm[:, :], a2[:, :], -1.0, None, ALU.add, None, [a2b], [pmb])
                        mx, mxb = sm2.next()
                        S.op("vector", lambda e, mx=mx, pm=pm: e.max(out=mx[:, :], in_=pm[:, :]), reads=[pmb], writes=[mxb])
                        vcopy(P12f[:, c * 2:c * 2 + 2], mx[:, 0:2], [mxb], [BP12f[c]])
                        for k2 in range(2):
                            eq, eqb = sm2.next()
                            ts(eq[:, :], pm[:, :], mx[:, k2:k2 + 1], None, ALU.is_equal, None, [pmb, mxb], [eqb])
                            eg, egb = sm2.next()
                            tt(eg[:, :], eq[:, :], G_all[:, c, :], ALU.mult, [eqb, BGa[c]], [egb])
                            S.op("vector", lambda e, eg=eg, c=c, k2=k2: e.tensor_reduce(out=G12[:, c * 2 + k2:c * 2 + k2 + 1], in_=eg[:, :],
                                                                                         axis=mybir.AxisListType.X, op=ALU.add),
                                 reads=[egb, BG12[c]], writes=[BG12[c]])
                    vcopy(P12i[:, :], P12f[:, :], BP12f, [BP12i])
                    dbg_dump("cnt", cnt[:, :], [128, 8], [cntb])
                    dbg_dump("P12f", P12f[:, :], [128, 64], BP12f)
                    dbg_dump("G12", G12[:, :], [128, 64], BG12)
                    dbg_dump("ej", ej[:, :], [128, JT], [Bej])
                    dbg_dump("off", off[:, :], [128, 8], [offb])
                    dbg_dump("idxf", idxf[:, :], [128, JT * 7], [Bidxf])
                    dbg_dump("M_all", M_all[:].rearrange("p a b -> p (a b)"), [128, 256], BM)
                    dbg_dump("R_all", R_all[:].rearrange("p a b -> p (a b)"), [128, 256], [BR])
                    dbg_dump("h_tm", h_tm[:].rearrange("p a b -> p (a b)"), [128, 32 * 1024], Bhtm, dt=BF16)
                    for c in range(32):
                        for k2 in range(2):
                            def sc(e, c=c, k2=k2):
                                return e.indirect_dma_start(out=hsort, out_offset=bass.IndirectOffsetOnAxis(ap=P12i[:, c * 2 + k2:c * 2 + k2 + 1], axis=0),
                                                            in_=h_tm[:, c, :], in_offset=None)
                            S.dma_fn("gpsimd", sc, reads=[BP12i, Bhtm[c], Bhz], writes=[Bhs], join=True)

                            def sci(e, c=c, k2=k2):
                                return e.indirect_dma_start(out=slot2tok, out_offset=bass.IndirectOffsetOnAxis(ap=P12i[:, c * 2 + k2:c * 2 + k2 + 1], axis=0),
                                                            in_=tokid[:, c * 2 + k2, :], in_offset=None)
                            S.dma_fn("gpsimd", sci, reads=[BP12i, Btokid, Bs2z], writes=[Bs2t], join=True)
                S.barrier()
                with ExitStack() as p4:
                    al4 = mk_alloc(p4)
                    st = ffn_engine(al4)
                    hsr = Rot(S, al4, "hs_tm", 2, [128, 4, 1024], BF16)
                    hsTr = [al4(f"hsT{i}", [128, 8, 512], BF16) for i in range(2)]
                    BhsT = [S.bufs(f"hsT{i}_", 8) for i in range(2)]
                    yaccr = [al4(f"yaccs{i}", [128, 8, 512]) for i in range(2)]
                    Byr = [S.bufs(f"ys{i}_", 8) for i in range(2)]
                    ysr = Rot(S, al4, "ys_tm", 2, [128, 4, 1024], F32)
                    idr = Rot(S, al4, "idt", 2, [128, 64], I32)
                    Bscat = S.bufs("scat", 2)

                    def dyn_loader(j):
                        def load(f0, gs, w1t, w1b, w3t, w3b, w2t, w2b):
                            fg = f0 // 4
                            col = j * 7 + fg
                            for (tab, wt_, wb_) in ((x_w1, w1t, w1b), (x_w3, w3t, w3b), (x_w2, w2t, w2b)):
                                flat = wt_[:].rearrange("p a b -> p (a b)")
                                for hf in range(2):
                                    def g(e, tab=tab, flat=flat, hf=hf, col=col):
                                        return e.indirect_dma_start(out=flat[:, hf * 2048:(hf + 1) * 2048], out_offset=None, in_=tab,
                                                                    in_offset=bass.IndirectOffsetOnAxis(ap=idxi[:, col:col + 1], axis=0),
                                                                    element_offset=hf * 2048)
                                    S.dma_fn("gpsimd", g, reads=[Bidxi], writes=[wb_], join=(hf == 1))
                        return load

                    def prologue(j):
                        hs_t, hs_b = hsr.next()
                        S.dma("sync", hs_t[:], hsort[j * 512:(j + 1) * 512, :].rearrange("(r p) d -> p r d", p=128), reads=[Bhz, Bhs], writes=[hs_b])
                        hsT, Bh_ = hsTr[j % 2], BhsT[j % 2]
                        for dk in range(8):
                            pt, ptb = bank()
                            for r in range(4):
                                mm(pt[:, r * 128:(r + 1) * 128], hs_t[:, r, dk * 128:(dk + 1) * 128], identb[:], True, True, [hs_b, Bidb], ptb)
                            if dk % 2 == 0:
                                act(hsT[:, dk, :], pt[:, :], AF.Copy, [ptb], [Bh_[dk]])
                            else:
                                vcopy(hsT[:, dk, :], pt[:, :], [ptb], [Bh_[dk]])

                    def epilogue(j):
                        yacc, By = yaccr[j % 2], Byr[j % 2]
                        ys_t, ys_b = ysr.next()
                        for r in range(4):
                            for half in range(2):
                                pt, ptb = bank()
                                for q in range(4):
                                    dm = half * 4 + q
                                    S.op("tensor", lambda e, pt=pt, q=q, dm=dm, r=r, yacc=yacc: e.transpose(
                                        out=pt[:, q * 128:(q + 1) * 128], in_=yacc[:, dm, r * 128:(r + 1) * 128], identity=ident[:]),
                                        reads=[By[dm], Bident], writes=[ptb])
                                if half == 0:
                                    act(ys_t[:, r, 0:512], pt[:, :], AF.Copy, [ptb, ys_b], [ys_b])
                                else:
                                    vcopy(ys_t[:, r, 512:1024], pt[:, :], [ptb, ys_b], [ys_b])
                        idt, idb = idr.next()
                        S.dma("sync", idt[:].rearrange("p (r o) -> p r o", r=4), slot2tok[j * 512:(j + 1) * 512, :].rearrange("(r p) o -> p r o", p=128), reads=[Bs2z, Bs2t], writes=[idb])
                        for r in range(4):
                            def scy(e, r=r, idt=idt, ys_t=ys_t):
                                return e.indirect_dma_start(out=ytok, out_offset=bass.IndirectOffsetOnAxis(ap=idt[:, r * 16:r * 16 + 1], axis=0),
                                                            in_=ys_t[:, r, :], in_offset=None)
                            S.dma_fn("gpsimd", scy, reads=[idb, ys_b], writes=[Bscat[j % 2]], join=True)

                    prologue(0)
                    for j in range(JT):
                        if j + 1 < JT:
                            prologue(j + 1)
                        ffn_run(st, hsTr[j % 2], BhsT[j % 2], 512, [dyn_loader(j)], 28, None, yaccr[j % 2], Byr[j % 2],
                                next_loader=dyn_loader(j + 1) if j + 1 < JT else None)
                        if j >= 1:
                            epilogue(j - 1)
                    epilogue(JT - 1)
                S.barrier()
                if DEBUG:
                    tq = nc.dram_tensor("dbg_hsort", [NS, D], BF16, kind="ExternalOutput").ap()
                    S.dma("sync", tq, hsort, reads=[Bhz, Bhs]); DBG_NAMES.append("dbg_hsort")
                    tq2 = nc.dram_tensor("dbg_ysort", [NS, D], F32, kind="ExternalOutput").ap()
                    S.dma("sync", tq2, ysort, reads=[Bys]); DBG_NAMES.append("dbg_ysort")
                with ExitStack() as p5:
                    al5 = mk_alloc(p5)
                    xr = Rot(S, al5, "xd5", 2, [128, 8, 512], F32)
                    y12r = Rot(S, al5, "y12", 6, [128, 2, 1024], F32)
                    ytr = Rot(S, al5, "yt", 3, [128, 1024], F32)
                    oo = Rot(S, al5, "oo", 2, [128, 8, 512], F32)
                    sqr = Rot(S, al5, "sq5", 2, [128, 512], F32)
                    rst = Rot(S, al5, "rs5", 2, [128, 512], F32)
                    v = dview(xs)
                    ov = dview(outT)
                    for tb in range(8):
                        xb, xbb = xr.next()
                        S.dma("sync", xb[:], v[:, :, tb * 512:(tb + 1) * 512], reads=[Bxs[tb]], writes=[xbb])
                        for t4 in range(4):
                            c = tb * 4 + t4
                            y12, y12b = y12r.next()
                            S.dma("sync", y12[:], ytok[0:2 * T, :].rearrange("(t k) d -> t k d", k=2)[c * 128:(c + 1) * 128], reads=[Bytok], writes=[y12b])
                            y1, y1b, y2, y2b = y12[:, 0, :], y12b, y12[:, 1, :], y12b
                            yt, ytb = ytr.next()
                            ts(yt[:, :], y1, G12[:, c * 2:c * 2 + 1], None, ALU.mult, None, [y1b, BG12[c]], [ytb])
                            stt(yt[:, :], y2, G12[:, c * 2 + 1:c * 2 + 2], yt[:, :], ALU.mult, ALU.add, [y2b, BG12[c], ytb], [ytb])
                            for dm in range(8):
                                S.op("tensor", lambda e, dm=dm, t4=t4, yt=yt: e.transpose(
                                    out=banks[dm][:, t4 * 128:(t4 + 1) * 128], in_=yt[:, dm * 128:(dm + 1) * 128], identity=ident[:]),
                                    reads=[ytb, Bident], writes=[bank_b[dm]])
                        for dm in range(8):
                            stt(xb[:, dm, :], banks[dm][:, :], mod(1, 5, dm, 0), xb[:, dm, :], ALU.mult, ALU.add, [bank_b[dm], Bmod, xbb], [xbb])
                        r, rb = rms_stat(lambda j: xb[:, j, :], xbb, 512, (sqr, rst), 8, 1.0 / D)
                        ot, otb = oo.next()
                        for dm in range(8):
                            stt(ot[:, dm, :], xb[:, dm, :], vv("fg", dm), r[:, :], ALU.mult, ALU.mult, [xbb, Bvecs, rb], [otb])
                        S.dma("sync", ov[:, :, tb * 512:(tb + 1) * 512], ot[:], reads=[otb], writes=[Bout[tb]])

        Bxs = S.bufs("xs", 8)
        Bcs = S.bufs("cs", 1)
        Bout = S.bufs("out", 8)
        phases = [("A", phase_A), ("B", phase_B), ("C", phase_C), ("D", phase_D2)]
        for name, fn in phases:
            fn()
            S.barrier()
            if name == stop_after:
                break
        if stop_after != "D":
            for tb in range(8):
                S.dma("sync", outT[:, tb * 512:(tb + 1) * 512], xs[:, tb * 512:(tb + 1) * 512], reads=[Bxs[tb]], writes=[Bout[tb]])
        S.barrier()
        S.emit()
        print(f"[build] insts={S.n_inst} waits={S.n_wait} sems={S.nsem}", flush=True)
    return nc


def _rope_tables():
    rows = T // 64
    row = np.repeat(np.arange(rows), 64).astype(np.float32)
    col = np.tile(np.arange(64), rows).astype(np.float32)
    inv = (10000.0 ** (-np.arange(16, dtype=np.float32) / 16)).astype(np.float32)
    ang = np.stack([row[:, None] * inv, col[:, None] * inv], axis=1).astype(np.float32)
    cos, sin = np.cos(ang).astype(np.float32), np.sin(ang).astype(np.float32)
    C = np.zeros((64, T), np.float32)
    Sg = np.zeros((64, T), np.float32)
    for p in range(64):
        ax, half, f = p // 32, (p // 16) % 2, p % 16
        C[p] = cos[:, ax, f]
        Sg[p] = sin[:, ax, f] * (-1.0 if half == 0 else 1.0)
    tab = np.stack([np.concatenate([C, C], 0), np.concatenate([Sg, Sg], 0)], axis=1)
    return np.ascontiguousarray(tab)


def _prep_shared(inp):
    f = lambda a: np.ascontiguousarray(np.asarray(a, dtype=np.float32))
    sh = {}
    sh["ident"] = np.eye(128, dtype=np.float32)
    sh["lnv"] = f(np.stack([np.broadcast_to(inp["e_ln_v_g"][0], (128, 512)), np.broadcast_to(inp["e_ln_v_b"][0], (128, 512))], axis=1))
    bs = inp["e_b_s"][0]
    sh["bs"] = f(np.repeat(bs.reshape(4, 2, 1, 128), 64, axis=2).reshape(4, 128, 128).transpose(1, 0, 2))
    sh["wsT"] = f(inp["e_w_s"][0].transpose(2, 0, 1))
    sh["rope"] = _rope_tables()
    sel = np.zeros((8, 8, 128), np.float32)
    for e in range(8):
        sel[e, e, :] = 1.0
    sh["sel"] = sel
    sh["w_ada"] = f(inp["w_ada"])
    sh["e_w_in"] = f(inp["e_w_in"][0])
    sh["e_w_out"] = f(inp["e_w_out"][0])
    sh["e_ffn_w1"] = f(inp["e_ffn_w1"][0])
    sh["e_ffn_w3"] = f(inp["e_ffn_w3"][0])
    sh["e_ffn_w2"] = f(inp["e_ffn_w2"][0])
    perm = np.arange(64) ^ 16
    wi = inp["o_w_in"][0]
    kp = wi[:, 640:704]
    sh["o_w_in_x"] = f(np.concatenate([wi[:, :640], kp, kp, kp[:, perm], kp[:, perm]], axis=1))
    wq = inp["o_w_uq"][0].reshape(384, 8, 192)
    nope = wq[:, :, :128].reshape(384, 1024)
    rp = wq[:, :, 128:]
    sh["o_wuq"] = f(np.concatenate([nope, rp.reshape(384, 512), rp[:, :, perm].reshape(384, 512)], axis=1))
    wkv = inp["o_w_ukv"][0].reshape(256, 8, 256)
    sh["o_wukv"] = f(np.concatenate([wkv[:, :, :128].reshape(256, 1024), wkv[:, :, 128:].reshape(256, 1024)], axis=1))
    sh["o_w_o"] = f(inp["o_w_o"][0])
    sh["o_router"] = f(inp["o_router"][0])
    w1 = np.asarray(inp["o_exp_w1"][0], np.float32).reshape(8, 8, 128, 7, 512)
    sh["xw1r"] = np.ascontiguousarray(w1.transpose(0, 3, 2, 1, 4)).reshape(8 * 7 * 128 * 2, 2048)
    w3 = np.asarray(inp["o_exp_w3"][0], np.float32).reshape(8, 8, 128, 7, 512)
    sh["xw3r"] = np.ascontiguousarray(w3.transpose(0, 3, 2, 1, 4)).reshape(8 * 7 * 128 * 2, 2048)
    w2 = np.asarray(inp["o_exp_w2"][0], np.float32).reshape(8, 7, 4, 128, 1024)
    sh["xw2r"] = np.ascontiguousarray(w2.transpose(0, 1, 3, 2, 4)).reshape(8 * 7 * 128 * 2, 2048)
    tid = ((np.arange(32)[None, :, None] * 128 + np.arange(128)[:, None, None]) * 2 + np.arange(2)[None, None, :]).reshape(128, 64)
    sh["tokid"] = np.ascontiguousarray(np.repeat(tid[:, :, None], 16, axis=2).astype(np.int32))
    sh["oobfill"] = np.ascontiguousarray(np.repeat((2 * T + np.arange(128 * 96, dtype=np.int32)).reshape(128, 96, 1), 16, axis=2).reshape(128, 96 * 16))
    sh["utri"] = np.triu(np.ones((128, 128), np.float32), 1)
    sh["ibase"] = (np.arange(7, dtype=np.float32)[None, :] * 256 + 2 * np.arange(128, dtype=np.float32)[:, None]).astype(np.float32)
    return sh


def _prep_vecs(inp, b):
    v = np.zeros((128, NV), np.float32)

    def put(name, arr):
        arr = np.asarray(arr, np.float32).reshape(128, -1)
        v[:, VOFF[name]:VOFF[name] + arr.shape[1]] = arr
    cp = lambda a: np.asarray(a, np.float32).reshape(-1, 128).T
    put("sc", np.stack([cp(inp["c"][b]), cp(inp["c_ctx"])], axis=-1))
    ba = np.asarray(inp["b_ada"], np.float32).reshape(2, 6, 8, 128).transpose(3, 0, 1, 2)
    put("bada", np.repeat(ba[..., None], 2, axis=-1))
    ng = np.asarray(inp["norm_g"], np.float32).reshape(2, 2, 8, 128).transpose(3, 0, 1, 2)
    put("ng", np.repeat(ng[..., None], 2, axis=-1))
    put("convw", np.asarray(inp["e_conv_w"][0], np.float32).reshape(31, 4, 128).transpose(2, 1, 0))
    put("convb", cp(inp["e_conv_b"][0]))
    put("lnag", cp(inp["e_ln_a_g"][0]))
    put("lnab", cp(inp["e_ln_a_b"][0]))
    put("gq", cp(inp["o_g_q"][0]))
    put("gkv", cp(inp["o_g_kv"][0]))
    put("fg", cp(inp["final_g"]))
    return v


def run(inputs, stop_after="D", cores=None):
    inp = {k: np.asarray(v) for k, v in inputs.items()}
    cores = list(range(NCORES)) if cores is None else cores
    sh = _prep_shared(inp)
    in_maps = []
    for b in cores:
        m = dict(sh)
        m["xT"] = np.ascontiguousarray(inp["x"][b].T.astype(np.float32))
        m["cT"] = np.ascontiguousarray(inp["ctx"][b].T.astype(np.float32))
        m["vecs"] = _prep_vecs(inp, b)
        in_maps.append(m)
    nc = build(stop_after)
    res = run_bass_kernel_spmd(nc, in_maps, core_ids=list(range(len(cores))))
    if DEBUG:
        for n in DBG_NAMES:
            DBG_OUT[n] = np.asarray(res.results[0][n])
    return np.stack([np.ascontiguousarray(r["outT"].T) for r in res.results], axis=0)


def kernel(**inputs):
    return run(inputs).astype(np.float32)
```

```python
import numpy as np
from contextlib import ExitStack
import concourse.bass as bass
import concourse.mybir as mybir
from concourse.bass_utils import run_bass_kernel_spmd

F32 = mybir.dt.float32
BF16 = mybir.dt.bfloat16
AF = mybir.ActivationFunctionType
ALU = mybir.AluOpType

T, TC, D, KD = 4096, 256, 1024, 8
EPS = 1e-6
NCORES = 8
SEM_LIMIT = 30000
DEBUG = False
DBG_NAMES = []
DBG_OUT = {}


class Buf:
    __slots__ = ("name", "w", "r", "dsem", "dcnt")

    def __init__(self, name):
        self.name = name
        self.w = {}
        self.r = {}
        self.dsem = None
        self.dcnt = 0


class Sched:
    def __init__(self, nc, ctx):
        self.nc = nc
        self.ctx = ctx
        self.prog = {e: [] for e in ("tensor", "vector", "scalar", "gpsimd", "sync")}
        self.esem = {}
        self.ecnt = {}
        self.nsem = 0
        self.allsems = {}
        for e in ("tensor", "vector", "scalar", "gpsimd"):
            self._new_esem(e)
        self.seen = {e: {} for e in self.prog}
        self.n_inst = 0
        self.n_wait = 0

    def _alloc_sem(self, name):
        self.nsem += 1
        s = self.ctx.enter_context(self.nc.semaphore(f"{name}_{self.nsem}"))
        self.allsems[id(s)] = [s, 0]
        return s

    def _new_esem(self, e):
        self.esem[e] = self._alloc_sem(f"e_{e}")
        self.ecnt[e] = 0

    def buf(self, name):
        return Buf(name)

    def bufs(self, name, n):
        return [Buf(f"{name}{i}") for i in range(n)]

    def _need(self, eng, toks):
        out = {}
        for (sem, val, teng) in toks:
            if teng == eng and eng == "tensor":
                continue
            k = id(sem)
            if self.seen[eng].get(k, 0) >= val:
                continue
            if k not in out or out[k][1] < val:
                out[k] = (sem, val)
        for k, (sem, val) in out.items():
            self.seen[eng][k] = val
        return list(out.values())

    def _deps(self, eng, reads, writes, join=False):
        toks = []
        for b in reads:
            toks += b.w.values()
        for b in writes:
            if not join:
                toks += b.w.values()
            toks += b.r.values()
        return self._need(eng, toks)

    def _record(self, tok, reads, writes, join=False):
        k = id(tok[0])
        self.allsems[k][1] = max(self.allsems[k][1], tok[1])
        for b in reads:
            b.r[k] = tok
        for b in writes:
            if join:
                b.w[k] = tok
            else:
                b.w = {k: tok}
                b.r = {}

    def op(self, eng, fn, reads=(), writes=()):
        reads = [b for b in reads if b is not None]
        writes = [b for b in writes if b is not None]
        waits = self._deps(eng, reads, writes)
        if self.ecnt[eng] >= SEM_LIMIT:
            self._new_esem(eng)
        sem = self.esem[eng]
        self.ecnt[eng] += 1
        tok = (sem, self.ecnt[eng], eng)
        self._record(tok, reads, writes)
        self.n_inst += 1
        self.n_wait += len(waits)

        def run(e, waits=waits, fn=fn, sem=sem):
            for (s, v) in waits:
                e.wait_ge(s, v)
            fn(e).then_inc(sem, 1)
        self.prog[eng].append(run)

    def dma(self, queue, out, in_, reads=(), writes=(), join=False, **kw):
        reads = [b for b in reads if b is not None]
        writes = [b for b in writes if b is not None]
        waits = self._deps(queue, reads, writes, join)
        sb = writes[0] if writes else reads[0]
        if sb.dsem is None:
            sb.dsem = self._alloc_sem("d")
        sb.dcnt += 16
        tok = (sb.dsem, sb.dcnt, "dma")
        self._record(tok, reads, writes, join)
        self.n_inst += 1
        self.n_wait += len(waits)

        def run(e, waits=waits, sem=sb.dsem):
            for (s, v) in waits:
                e.wait_ge(s, v)
            e.dma_start(out=out, in_=in_, **kw).then_inc(sem, 16)
        self.prog[queue].append(run)

    def dma_fn(self, queue, fn, reads=(), writes=(), join=False):
        reads = [b for b in reads if b is not None]
        writes = [b for b in writes if b is not None]
        waits = self._deps(queue, reads, writes, join)
        sb = writes[0] if writes else reads[0]
        if sb.dsem is None:
            sb.dsem = self._alloc_sem("d")
        sb.dcnt += 16
        tok = (sb.dsem, sb.dcnt, "dma")
        self._record(tok, reads, writes, join)
        self.n_inst += 1

        def run(e, waits=waits, sem=sb.dsem, fn=fn):
            for (s, v) in waits:
                e.wait_ge(s, v)
            fn(e).then_inc(sem, 16)
        self.prog[queue].append(run)

    def barrier(self):
        toks = [(s, v, "x") for (s, v) in self.allsems.values() if v > 0]
        for eng in self.prog:
            waits = self._need(eng, toks)

            def run(e, waits=waits):
                for (s, v) in waits:
                    e.wait_ge(s, v)
            self.prog[eng].append(run)

    def emit(self):
        with self.nc.Block() as block:
            @block.sync
            def _(e):
                for f in self.prog["sync"]:
                    f(e)

            @block.tensor
            def _(e):
                for f in self.prog["tensor"]:
                    f(e)

            @block.vector
            def _(e):
                for f in self.prog["vector"]:
                    f(e)

            @block.scalar
            def _(e):
                for f in self.prog["scalar"]:
                    f(e)

            @block.gpsimd
            def _(e):
                for f in self.prog["gpsimd"]:
                    f(e)


class Rot:
    def __init__(self, S, alloc, name, k, shape, dt):
        self.t = [alloc(f"{name}{i}", shape, dt) for i in range(k)]
        self.b = S.bufs(name, k)
        self.i = 0

    def next(self):
        i = self.i
        self.i = (i + 1) % len(self.t)
        return self.t[i], self.b[i]


VOFF = {}


def _vlayout():
    off = 0
    for name, n in [("sc", 16), ("bada", 192), ("ng", 64), ("convw", 124), ("convb", 4), ("lnag", 4),
                    ("lnab", 4), ("gq", 3), ("gkv", 2), ("fg", 8)]:
        VOFF[name] = off
        off += n
    return off


NV = _vlayout()


def build(stop_after="D"):
    nc = bass.Bass("TRN2", target_bir_lowering=False)

    def din(name, shape):
        return nc.dram_tensor(name, list(shape), F32, kind="ExternalInput").ap()

    xT_in = din("xT", [D, T])
    cT_in = din("cT", [D, TC])
    vecs_in = din("vecs", [128, NV])
    ident_in = din("ident", [128, 128])
    lnv_in = din("lnv", [128, 2, 512])
    bs_in = din("bs", [128, 4, 128])
    wsT_in = din("wsT", [128, 8, 128])
    rope_in = din("rope", [128, 2, T])
    sel_in = din("sel", [8, 8, 128])
    w_ada = din("w_ada", [2, D, 6 * D])
    e_w_in = din("e_w_in", [D, 2048])
    e_w_out = din("e_w_out", [D, D])
    f_w1 = din("e_ffn_w1", [D, 2816])
    f_w3 = din("e_ffn_w3", [D, 2816])
    f_w2 = din("e_ffn_w2", [2816, D])
    o_w_in = din("o_w_in_x", [D, 896])
    o_wuq = din("o_wuq", [384, 2048])
    o_wukv = din("o_wukv", [256, 2048])
    o_w_o = din("o_w_o", [D, D])
    o_router = din("o_router", [D, 8])
    NROW = 8 * 7 * 128 * 2
    x_w1 = din("xw1r", [NROW, 2048])
    x_w3 = din("xw3r", [NROW, 2048])
    x_w2 = din("xw2r", [NROW, 2048])
    utri_in = din("utri", [128, 128])
    tokid_in = nc.dram_tensor("tokid", [128, 64, 16], mybir.dt.int32, kind="ExternalInput").ap()
    oobfill_in = nc.dram_tensor("oobfill", [128, 96 * 16], mybir.dt.int32, kind="ExternalInput").ap()
    slot2tok = nc.dram_tensor("slot2tok", [24 * 512, 16], mybir.dt.int32).ap()
    ytok = nc.dram_tensor("ytok", [2 * T + 24 * 512, D], F32).ap()
    ibase_in = din("ibase", [128, 7])
    NS = 24 * 512
    hsort = nc.dram_tensor("hsort", [NS, D], BF16).ap()
    ysort = nc.dram_tensor("ysort", [NS, D], F32).ap()
    outT = nc.dram_tensor("outT", [D, T], F32, kind="ExternalOutput").ap()
    xs = nc.dram_tensor("xs", [D, T], F32).ap()
    cs = nc.dram_tensor("cs", [D, TC], F32).ap()

    def dview(ap):
        return ap.rearrange("(j p) n -> p j n", p=128)

    with ExitStack() as ctx:
        S = Sched(nc, ctx)

        uid = [0]

        def dbg_dump(name, ap, shape, reads, dt=F32):
            if not DEBUG:
                return
            t = nc.dram_tensor("dbg_" + name, list(shape), dt, kind="ExternalOutput").ap()
            S.dma("sync", t, ap, reads=reads)
            DBG_NAMES.append("dbg_" + name)

        def mk_alloc(stack):
            def alloc(name, shape, dt=F32):
                uid[0] += 1
                return stack.enter_context(nc.sbuf_tensor(f"sb{uid[0]}_{name}", list(shape), dt))
            return alloc
        galloc = mk_alloc(ctx)

        banks = [ctx.enter_context(nc.psum_tensor(f"pb{i}", [128, 512], F32)) for i in range(8)]
        bank_b = S.bufs("pb", 8)
        bstate = {"i": 0}

        def bank(subset=None):
            if subset is None:
                i = bstate["i"]
                bstate["i"] = (i + 1) % 8
            else:
                i = subset[bstate.setdefault(id(subset), 0) % len(subset)]
                bstate[id(subset)] += 1
            return banks[i], bank_b[i]

        def mm(out, lhsT, rhs, start, stop, reads, wb):
            S.op("tensor", lambda e: e.matmul(out, lhsT=lhsT, rhs=rhs, start=start, stop=stop), reads=reads, writes=[wb])

        def act(out, in_, func, reads, writes, **kw):
            S.op("scalar", lambda e: e.activation(out=out, in_=in_, func=func, **kw), reads=reads, writes=writes)

        def tt(out, a, b, op, reads, writes, eng="vector"):
            S.op(eng, lambda e: e.tensor_tensor(out=out, in0=a, in1=b, op=op), reads=reads, writes=writes)

        def stt(out, in0, scalar, in1, op0, op1, reads, writes, eng="vector"):
            S.op(eng, lambda e: e.scalar_tensor_tensor(out=out, in0=in0, scalar=scalar, in1=in1, op0=op0, op1=op1),
                 reads=reads, writes=writes)

        def ts(out, in0, s1, s2, op0, op1, reads, writes, eng="vector"):
            if s2 is None:
                S.op(eng, lambda e: e.tensor_scalar(out=out, in0=in0, scalar1=s1, scalar2=None, op0=op0), reads=reads, writes=writes)
            else:
                S.op(eng, lambda e: e.tensor_scalar(out=out, in0=in0, scalar1=s1, scalar2=s2, op0=op0, op1=op1),
                     reads=reads, writes=writes)

        def vcopy(out, in_, reads, writes, eng="vector"):
            S.op(eng, lambda e: e.tensor_copy(out=out, in_=in_), reads=reads, writes=writes)

        def recip(out, in_, reads, writes):
            S.op("vector", lambda e: e.reciprocal(out=out, in_=in_), reads=reads, writes=writes)

        def memset(ap, val, writes):
            S.op("vector", lambda e: e.memset(ap, val), writes=writes)

        vecs = galloc("vecs", [128, NV]); Bvecs = S.buf("vecs")
        ident = galloc("ident", [128, 128]); Bident = S.buf("ident")
        ones = galloc("ones", [128, 128]); Bones = S.buf("ones")
        onesb = galloc("onesb", [128, 128], BF16); Bonesb = S.buf("onesb")
        epsc = galloc("epsc", [128, 1]); Beps = S.buf("eps")
        modT = galloc("modT", [128, 2 * 6 * 8 * 2]); Bmod = S.buf("modT")
        Gt = galloc("Gt", [128, 2 * 2 * 8 * 2]); BG = S.buf("Gt")
        scs = galloc("scs", [128, 16]); Bscs = S.buf("scs")
        S.dma("sync", vecs[:], vecs_in, writes=[Bvecs])
        S.dma("sync", ident[:], ident_in, writes=[Bident])
        memset(ones[:], 1.0, [Bones])
        memset(onesb[:], 1.0, [Bonesb])
        memset(epsc[:], EPS, [Beps])

        def vv(name, idx, n=1):
            o = VOFF[name] + idx
            return vecs[:, o:o + n]

        def mod(i, v, j, col):
            o = ((i * 6 + v) * 8 + j) * 2 + col
            return modT[:, o:o + 1]

        def Gs(i, n, j, col):
            o = ((i * 2 + n) * 8 + j) * 2 + col
            return Gt[:, o:o + 1]

        with ExitStack() as px:
            alloc = mk_alloc(px)
            wa = Rot(S, alloc, "wa", 2, [128, 8, 1024], F32)
            act(scs[:], vv("sc", 0, 16), AF.Silu, [Bvecs], [Bscs])
            for i in range(2):
                wv = w_ada[i].rearrange("(k p) n -> p k n", p=128)
                for v in range(6):
                    wt, wb = wa.next()
                    S.dma("sync", wt[:], wv[:, :, v * 1024:(v + 1) * 1024], writes=[wb])
                    pb, pbb = bank()
                    for j in range(8):
                        for k in range(8):
                            mm(pb[:, j * 2:j * 2 + 2], wt[:, k, j * 128:(j + 1) * 128], scs[:, k * 2:k * 2 + 2],
                               k == 0, k == 7, [wb, Bscs], pbb)
                    o = (i * 6 + v) * 16
                    tt(modT[:, o:o + 16], pb[:, 0:16], vecs[:, VOFF["bada"] + o:VOFF["bada"] + o + 16], ALU.add,
                       [pbb, Bvecs], [Bmod])
            for i in range(2):
                for n in range(2):
                    vs = 1 if n == 0 else 4
                    o = (i * 2 + n) * 16
                    om = (i * 6 + vs) * 16
                    stt(Gt[:, o:o + 16], modT[:, om:om + 16], 1.0, vecs[:, VOFF["ng"] + o:VOFF["ng"] + o + 16],
                        ALU.add, ALU.mult, [Bmod, Bvecs], [BG])
        S.barrier()

        def rms_stat(xt, Bx, n, tmps, nchunks, inv_d):
            sqr, rst = tmps
            pb, pbb = bank()
            for j in range(nchunks):
                sq, sqb = sqr.next()
                act(sq[:, :n], xt(j), AF.Square, [Bx(j) if callable(Bx) else Bx], [sqb])
                mm(pb[:, :n], ones[:], sq[:, :n], j == 0, j == nchunks - 1, [Bones, sqb], pbb)
            r1, r1b = rst.next()
            act(r1[:, :n], pb[:, :n], AF.Sqrt, [pbb, Beps], [r1b], bias=epsc[:], scale=inv_d)
            r2, r2b = rst.next()
            recip(r2[:, :n], r1[:, :n], [r1b], [r2b])
            return r2, r2b

        def norm_mod(xblk, Bx, n, i, nn, col, hT, Bh, tmps, h32=None, Bh32=None):
            sqr, rst, tmr = tmps
            r, rb = rms_stat(lambda j: xblk[:, j, :n], Bx, n, (sqr, rst), 8, 1.0 / D)
            vsh = 0 if nn == 0 else 3
            for j in range(8):
                tm, tmb = tmr.next()
                stt(tm[:, :n], xblk[:, j, :n], Gs(i, nn, j, col), r[:, :n], ALU.mult, ALU.mult, [Bx, BG, rb], [tmb])
                if h32 is not None:
                    act(h32[:, j, :n], tm[:, :n], AF.Identity, [tmb, Bmod], [Bh32[j]], bias=mod(i, vsh, j, col), scale=1.0)
                    if hT is not None:
                        vcopy(hT[:, j, :n], h32[:, j, :n], [Bh32[j]], [Bh[j]], eng="gpsimd")
                else:
                    act(hT[:, j, :n], tm[:, :n], AF.Identity, [tmb, Bmod], [Bh[j]], bias=mod(i, vsh, j, col), scale=1.0)

        def load_w_bf(dst, dst_b, src_view, nparts=1, axis=1):
            S.dma("gpsimd", dst, src_view, writes=[dst_b])

        def phase_A():
            with ExitStack() as px:
                alloc = mk_alloc(px)
                win = alloc("win", [128, 8, 2048], BF16); Bwin = S.buf("win")
                wout = alloc("wout", [128, 8, 1024], BF16); Bwout = S.buf("wout")
                wsT = alloc("wsT", [128, 8, 128], BF16); BwsT = S.buf("wsT")
                lnv = alloc("lnv", [128, 2, 512]); Blnv = S.buf("lnv")
                bsb = alloc("bsb", [128, 4, 128]); Bbs = S.buf("bs")
                a_pad = alloc("a_pad", [128, 4, T + 30], BF16); Bap = S.bufs("ap", 4)
                xr = Rot(S, alloc, "xa", 2, [128, 8, 512], F32)
                hT = alloc("hT", [128, 8, 512], BF16); Bh = S.bufs("h", 8)
                uT = alloc("uT", [128, 4, 512], BF16); Bu = S.bufs("u", 4)
                vtm = alloc("vtm", [128, 4, 512], BF16); Bv = S.bufs("v", 4)
                ac = alloc("ac", [128, 4, 512]); Bac = S.bufs("ac", 4)
                a2T = alloc("a2T", [128, 4, 512], BF16); Ba2 = S.bufs("a2", 4)
                boT = alloc("boT", [128, 4, 512], BF16); Bbo = S.bufs("bo", 4)
                sqr = Rot(S, alloc, "sqa", 2, [128, 512], F32)
                rst = Rot(S, alloc, "rsa", 4, [128, 512], F32)
                tmr = Rot(S, alloc, "tma", 3, [128, 512], F32)
                diag = alloc("diag", [128, 4, 31, 128], BF16); Bdiag = S.buf("diag")
                for c in range(4):
                    for k in range(31):
                        ts(diag[:, c, k, :], ident[:], vv("convw", c * 31 + k), None, ALU.mult, None, [Bident, Bvecs, Bdiag], [Bdiag])
                smr = Rot(S, alloc, "sma", 8, [128, 8], F32)
                tmps = (sqr, rst, tmr)
                S.dma("gpsimd", win[:], e_w_in.rearrange("(k p) n -> p k n", p=128), writes=[Bwin])
                S.dma("gpsimd", wout[:], e_w_out.rearrange("(k p) n -> p k n", p=128), writes=[Bwout])
                S.dma("gpsimd", wsT[:], wsT_in, writes=[BwsT])
                S.dma("sync", lnv[:], lnv_in, writes=[Blnv])
                S.dma("sync", bsb[:], bs_in, writes=[Bbs])

                def seq(src, dst, dst_bufs, ntok, col):
                    nb = min(512, ntok)
                    nblk = ntok // nb
                    sv, dv = dview(src), dview(dst)
                    for c in range(4):
                        memset(a_pad[:, c, :], 0.0, [Bap[c]])
                    xq = {}

                    def xload(tb):
                        if tb < nblk:
                            xb_, xbb_ = xr.next()
                            S.dma("sync", xb_[:, :, :nb], sv[:, :, tb * nb:(tb + 1) * nb], writes=[xbb_])
                            xq[tb] = (xb_, xbb_)
                    xload(0)
                    for tb in range(nblk):
                        xload(tb + 1)
                        xb, xbb = xq.pop(tb)
                        norm_mod(xb, xbb, nb, 0, 0, col, hT, Bh, tmps)
                        for c in range(4):
                            pv, pvb = bank()
                            pg, pgb = bank()
                            for k in range(8):
                                mm(pv[:, :nb], win[:, k, c * 128:(c + 1) * 128], hT[:, k, :nb], k == 0, k == 7, [Bwin, Bh[k]], pvb)
                            for k in range(8):
                                mm(pg[:, :nb], win[:, k, 512 + c * 128:512 + (c + 1) * 128], hT[:, k, :nb], k == 0, k == 7,
                                   [Bwin, Bh[k]], pgb)
                            sg, sgb = tmr.next()
                            act(sg[:, :nb], pg[:, :nb], AF.Sigmoid, [pgb], [sgb])
                            tt(a_pad[:, c, 15 + tb * nb:15 + (tb + 1) * nb], pv[:, :nb], sg[:, :nb], ALU.mult, [pvb, sgb], [Bap[c]])
                    xload(0)
                    for tb in range(nblk):
                        xload(tb + 1)
                        xb, xbb = xq.pop(tb)
                        norm_mod(xb, xbb, nb, 0, 0, col, hT, Bh, tmps)
                        for c in range(4):
                            pu, pub = bank()
                            for k in range(8):
                                mm(pu[:, :nb], win[:, k, 1024 + c * 128:1024 + (c + 1) * 128], hT[:, k, :nb], k == 0, k == 7,
                                   [Bwin, Bh[k]], pub)
                            act(uT[:, c, :nb], pu[:, :nb], AF.Gelu_apprx_tanh, [pub], [Bu[c]])
                        for t4 in range(nb // 128):
                            pv, pvb = bank()
                            for k in range(8):
                                mm(pv[:, :], hT[:, k, t4 * 128:(t4 + 1) * 128], win[:, k, 1536:2048], k == 0, k == 7, [Bh[k], Bwin], pvb)
                            gv, gvb = tmr.next()
                            act(gv[:, :], pv[:, :], AF.Gelu_apprx_tanh, [pvb], [gvb])
                            st, stb = smr.next()
                            S.op("vector", lambda e, st=st, gv=gv: e.bn_stats(out=st[:, 0:6], in_=gv[:, :]), reads=[gvb], writes=[stb])
                            mv, mvb = smr.next()
                            S.op("vector", lambda e, st=st, mv=mv: e.bn_aggr(out=mv[:, 0:2], in_=st[:, 0:6]), reads=[stb], writes=[mvb])
                            sd, sdb = smr.next()
                            act(sd[:, 0:1], mv[:, 1:2], AF.Sqrt, [mvb, Beps], [sdb], bias=epsc[:], scale=1.0)
                            rs, rsb = smr.next()
                            recip(rs[:, 0:1], sd[:, 0:1], [sdb], [rsb])
                            g2, g2b = tmr.next()
                            ts(g2[:, :], gv[:, :], mv[:, 0:1], rs[:, 0:1], ALU.subtract, ALU.mult, [gvb, mvb, rsb], [g2b])
                            g3, g3b = tmr.next()
                            tt(g3[:, :], g2[:, :], lnv[:, 0, :], ALU.mult, [g2b, Blnv], [g3b], eng="gpsimd")
                            tt(vtm[:, t4, :], g3[:, :], lnv[:, 1, :], ALU.add, [g3b, Blnv], [Bv[t4]], eng="gpsimd")
                        for c in range(4):
                            base = tb * nb
                            pcv_, pcvb_ = bank()
                            for k in range(31):
                                mm(pcv_[:, :nb], diag[:, c, k, :], a_pad[:, c, base + k:base + k + nb], k == 0, k == 30, [Bdiag, Bap[c]], pcvb_)
                            act(ac[:, c, :nb], pcv_[:, :nb], AF.Identity, [pcvb_, Bvecs], [Bac[c]], bias=vv("convb", c), scale=1.0)
                        p1, p1b = bank()
                        p2, p2b = bank()
                        for c in range(4):
                            mm(p1[:, :nb], ones[:], ac[:, c, :nb], c == 0, c == 3, [Bones, Bac[c]], p1b)
                        for c in range(4):
                            sq, sqb = sqr.next()
                            act(sq[:, :nb], ac[:, c, :nb], AF.Square, [Bac[c]], [sqb])
                            mm(p2[:, :nb], ones[:], sq[:, :nb], c == 0, c == 3, [Bones, sqb], p2b)
                        mean, meanb = rst.next()
                        act(mean[:, :nb], p1[:, :nb], AF.Copy, [p1b], [meanb], scale=1.0 / 512)
                        msq, msqb = rst.next()
                        tt(msq[:, :nb], mean[:, :nb], mean[:, :nb], ALU.mult, [meanb], [msqb])
                        var, varb = rst.next()
                        stt(var[:, :nb], p2[:, :nb], 1.0 / 512, msq[:, :nb], ALU.mult, ALU.subtract, [p2b, msqb], [varb])
                        sd, sdb = tmr.next()
                        act(sd[:, :nb], var[:, :nb], AF.Sqrt, [varb, Beps], [sdb], bias=epsc[:], scale=1.0)
                        rs, rsb = rst.next()
                        recip(rs[:, :nb], sd[:, :nb], [sdb], [rsb])
                        for c in range(4):
                            y1, y1b = tmr.next()
                            tt(y1[:, :nb], ac[:, c, :nb], mean[:, :nb], ALU.subtract, [Bac[c], meanb], [y1b])
                            y2, y2b = tmr.next()
                            tt(y2[:, :nb], y1[:, :nb], rs[:, :nb], ALU.mult, [y1b, rsb], [y2b])
                            act(a2T[:, c, :nb], y2[:, :nb], AF.Silu, [y2b, Bvecs], [Ba2[c]], bias=vv("lnab", c), scale=vv("lnag", c))
                        for t4 in range(nb // 128):
                            ps_, psb = bank()
                            for jp in range(4):
                                for hh in range(2):
                                    h = 2 * jp + hh
                                    mm(ps_[hh * 64:(hh + 1) * 64, jp * 128:(jp + 1) * 128], vtm[:, t4, h * 64:(h + 1) * 64], wsT[:, h, :],
                                       True, True, [Bv[t4], BwsT], psb)
                            sv1, sv1b = tmr.next()
                            tt(sv1[:, :], ps_[:, :], bsb[:].rearrange("p a b -> p (a b)"), ALU.add, [psb, Bbs], [sv1b])
                            tt(boT[:, :, t4 * 128:(t4 + 1) * 128], sv1[:, :].rearrange("p (a b) -> p a b", a=4),
                               uT[:, :, t4 * 128:(t4 + 1) * 128], ALU.mult, [sv1b] + Bu, Bbo)
                        xn, xnb = xb, xbb
                        for dm in range(8):
                            po, pob = bank()
                            for c in range(4):
                                mm(po[:, :nb], wout[:, c, dm * 128:(dm + 1) * 128], a2T[:, c, :nb], c == 0, False, [Bwout, Ba2[c]], pob)
                            for c in range(4):
                                mm(po[:, :nb], wout[:, 4 + c, dm * 128:(dm + 1) * 128], boT[:, c, :nb], False, c == 3, [Bwout, Bbo[c]], pob)
                            stt(xn[:, dm, :nb], po[:, :nb], mod(0, 2, dm, col), xb[:, dm, :nb], ALU.mult, ALU.add,
                                [pob, Bmod, xbb], [xnb])
                        S.dma("sync", dv[:, :, tb * nb:(tb + 1) * nb], xn[:, :, :nb], reads=[xnb], writes=[dst_bufs[tb]])

                seq(cT_in, cs, Bcs, TC, 1)
                seq(xT_in, xs, Bxs, T, 0)

        def ffn_engine(alloc):
            st = {}
            st["w1"] = Rot(S, alloc, "w1g", 3, [128, 8, 512], BF16)
            st["w3"] = Rot(S, alloc, "w3g", 3, [128, 8, 512], BF16)
            st["w2"] = Rot(S, alloc, "w2g", 3, [128, 4, 1024], BF16)
            st["g"] = Rot(S, alloc, "gg", 8, [128, 512], BF16)
            st["s1"] = Rot(S, alloc, "s1", 3, [128, 512], F32)
            st["t2"] = Rot(S, alloc, "t2", 3, [128, 512], F32)
            return st

        def static_loader(w1, w3, w2):
            w1v = w1.rearrange("(k p) f -> p k f", p=128)
            w3v = w3.rearrange("(k p) f -> p k f", p=128)

            def load(f0, gs, w1t, w1b, w3t, w3b, w2t, w2b):
                S.dma("gpsimd", w1t[:, :, :gs * 128], w1v[:, :, f0 * 128:(f0 + gs) * 128], writes=[w1b])
                S.dma("gpsimd", w3t[:, :, :gs * 128], w3v[:, :, f0 * 128:(f0 + gs) * 128], writes=[w3b])
                S.dma("gpsimd", w2t[:, :gs, :], w2[f0 * 128:(f0 + gs) * 128, :].rearrange("(f p) d -> p f d", p=128), writes=[w2b])
            return load

        def ffn_run(st, hT, Bh, n, loaders, nchunks, gb, yacc, By, next_loader=None):
            groups = []
            f0 = 0
            while f0 < nchunks:
                gs = min(4, nchunks - f0)
                groups.append((f0, gs))
                f0 += gs
            items = [(ei, ld, f0, gs) for ei, ld in enumerate(loaders) for (f0, gs) in groups]
            nI = len(items)
            wt = {}

            def issue_load(i):
                ei, ld, f0, gs = items[i]
                w1t, w1b = st["w1"].next()
                w3t, w3b = st["w3"].next()
                w2t, w2b = st["w2"].next()
                ld(f0, gs, w1t, w1b, w3t, w3b, w2t, w2b)
                wt[i] = (w1t, w1b, w3t, w3b, w2t, w2b)

            def up(i):
                ei, ld, f0, gs = items[i]
                w1t, w1b, w3t, w3b, w2t, w2b = wt[i]
                gts = []
                for fi in range(gs):
                    pa, pab = bank()
                    pb_, pbb = bank()
                    for k in range(8):
                        mm(pa[:, :n], w1t[:, k, fi * 128:(fi + 1) * 128], hT[:, k, :n], k == 0, k == 7, [w1b, Bh[k]], pab)
                    for k in range(8):
                        mm(pb_[:, :n], w3t[:, k, fi * 128:(fi + 1) * 128], hT[:, k, :n], k == 0, k == 7, [w3b, Bh[k]], pbb)
                    s1, s1b = st["s1"].next()
                    act(s1[:, :n], pa[:, :n], AF.Silu, [pab], [s1b])
                    g, gbuf = st["g"].next()
                    if gb is None:
                        tt(g[:, :n], s1[:, :n], pb_[:, :n], ALU.mult, [s1b, pbb], [gbuf])
                    else:
                        t2, t2b = st["t2"].next()
                        tt(t2[:, :n], s1[:, :n], pb_[:, :n], ALU.mult, [s1b, pbb], [t2b])
                        gap, gapb = gb(ei)
                        tt(g[:, :n], t2[:, :n], gap, ALU.mult, [t2b, gapb], [gbuf], eng="gpsimd")
                    gts.append((g, gbuf))
                return gts

            def down(i, gts):
                ei, ld, f0, gs = items[i]
                w1t, w1b, w3t, w3b, w2t, w2b = wt.pop(i)
                for dm in range(8):
                    pd, pdb = bank()
                    for fi in range(gs):
                        mm(pd[:, :n], w2t[:, fi, dm * 128:(dm + 1) * 128], gts[fi][0][:, :n], fi == 0, fi == gs - 1,
                           [w2b, gts[fi][1]], pdb)
                    if i == 0:
                        act(yacc[:, dm, :n], pd[:, :n], AF.Copy, [pdb], [By[dm]])
                    else:
                        tt(yacc[:, dm, :n], yacc[:, dm, :n], pd[:, :n], ALU.add, [By[dm], pdb], [By[dm]])

            pre = st.pop("pre", None)
            if pre is not None:
                wt.update(pre)
            else:
                issue_load(0)
                issue_load(1)
            nxt = {}
            prev = None
            for i in range(nI + 1):
                cur = up(i) if i < nI else None
                if prev is not None:
                    down(i - 1, prev)
                if i + 2 < nI:
                    issue_load(i + 2)
                elif next_loader is not None and i + 2 - nI < 2:
                    kk = i + 2 - nI
                    f0n, gsn = groups[kk]
                    w1t, w1b = st["w1"].next()
                    w3t, w3b = st["w3"].next()
                    w2t, w2b = st["w2"].next()
                    next_loader(f0n, gsn, w1t, w1b, w3t, w3b, w2t, w2b)
                    nxt[kk] = (w1t, w1b, w3t, w3b, w2t, w2b)
                prev = cur
            if next_loader is not None:
                st["pre"] = nxt

        def phase_B():
            with ExitStack() as px:
                alloc = mk_alloc(px)
                st = ffn_engine(alloc)
                xr = Rot(S, alloc, "xb", 3, [128, 8, 512], F32)
                hTs = [alloc(f"hTb{i}", [128, 8, 512], BF16) for i in range(2)]
                Bhs_ = [S.bufs(f"hb{i}_", 8) for i in range(2)]
                yacc = alloc("yaccb", [128, 8, 512]); By = S.bufs("yb", 8)
                sqr = Rot(S, alloc, "sqb", 2, [128, 512], F32)
                rst = Rot(S, alloc, "rsb", 2, [128, 512], F32)
                tmr = Rot(S, alloc, "tmb", 3, [128, 512], F32)
                tmps = (sqr, rst, tmr)

                fl = static_loader(f_w1, f_w3, f_w2)
                blocks = [(cs, Bcs, 0, TC, 1)] + [(xs, Bxs, tb, 512, 0) for tb in range(8)]

                xl = {}

                def load(i):
                    if i >= len(blocks):
                        return
                    buf_ap, bufs, tb, nb, col = blocks[i]
                    v = dview(buf_ap)
                    xb, xbb = xr.next()
                    S.dma("sync", xb[:, :, :nb], v[:, :, tb * nb:(tb + 1) * nb], reads=[bufs[tb]], writes=[xbb])
                    xl[i] = (xb, xbb)

                def prep(i):
                    buf_ap, bufs, tb, nb, col = blocks[i]
                    xb, xbb = xl[i]
                    norm_mod(xb, xbb, nb, 0, 1, col, hTs[i % 2], Bhs_[i % 2], tmps)
                    return xb, xbb

                load(0)
                load(1)
                cur = prep(0)
                for i in range(len(blocks)):
                    buf_ap, bufs, tb, nb, col = blocks[i]
                    v = dview(buf_ap)
                    load(i + 2)
                    nxt = prep(i + 1) if i + 1 < len(blocks) else None
                    xb, xbb = cur
                    ffn_run(st, hTs[i % 2], Bhs_[i % 2], nb, [fl], 22, None, yacc, By, next_loader=fl if i + 1 < len(blocks) else None)
                    for dm in range(8):
                        stt(xb[:, dm, :nb], yacc[:, dm, :nb], mod(0, 5, dm, col), xb[:, dm, :nb], ALU.mult, ALU.add,
                            [By[dm], Bmod, xbb], [xbb])
                    S.dma("sync", v[:, :, tb * nb:(tb + 1) * nb], xb[:, :, :nb], reads=[xbb], writes=[bufs[tb]])
                    cur = nxt

        NK = TC + T
        SCALE = 192.0 ** -0.5

        def phase_C():
            with ExitStack() as px:
                alloc = mk_alloc(px)
                qn = alloc("qn", [128, 3, T], BF16); Bqn = S.bufs("qn", 8)
                kvn = alloc("kvn", [128, 2, NK], BF16); Bkvn = S.bufs("kvn", 9)
                kpe2 = alloc("kpe2", [128, 2, NK], BF16); Bkpe = S.bufs("kpe", 9); Bkz = S.buf("kpez")
                S.op("gpsimd", lambda e: e.memset(kpe2[:], 0.0), writes=[Bkz])
                qrp = alloc("qrp", [128, 4, T], BF16); Bqrp = S.bufs("qrp", 8)
                Batt = [S.bufs(f"att{h}_", 8) for h in range(8)]
                wuq = alloc("wuq", [128, 3, 1024], BF16); Bwuq = S.buf("wuq")
                wukv = alloc("wukv", [128, 2, 2048], BF16); Bwukv = S.buf("wukv")
                S.dma("gpsimd", wuq[:], o_wuq.rearrange("(k p) n -> p k n", p=128)[:, :, 0:1024], writes=[Bwuq])
                S.dma("gpsimd", wukv[:], o_wukv.rearrange("(k p) n -> p k n", p=128), writes=[Bwukv])
                with ExitStack() as p1:
                    al1 = mk_alloc(p1)
                    wuqr = al1("wuqr", [128, 3, 1024], BF16); Bwuqr = S.buf("wuqr")
                    S.dma("gpsimd", wuqr[:], o_wuq.rearrange("(k p) n -> p k n", p=128)[:, :, 1024:2048], writes=[Bwuqr])
                    wi = al1("wi", [128, 8, 896], BF16); Bwi = S.buf("wi")
                    S.dma("gpsimd", wi[:], o_w_in.rearrange("(k p) n -> p k n", p=128), writes=[Bwi])
                    xr = Rot(S, al1, "xc", 1, [128, 8, 512], F32)
                    hTs = [al1(f"hTc{i}", [128, 8, 512], BF16) for i in range(2)]
                    Bhs2 = [S.bufs(f"hc{i}_", 8) for i in range(2)]
                    zl = al1("zl", [128, 5, 512]); Bzl = S.bufs("zl", 5)
                    rp = Rot(S, al1, "rp", 1, [128, 2, 512], F32)
                    sqr = Rot(S, al1, "sqc", 2, [128, 512], F32)
                    rst = Rot(S, al1, "rsc", 4, [128, 512], F32)
                    tmr = Rot(S, al1, "tmc", 4, [128, 512], F32)
                    tmps = (sqr, rst, tmr)

                    lblocks = [(cs, Bcs, 0, TC, 1, 0, 0, False)] + [(xs, Bxs, tb, 512, 0, TC, 1, True) for tb in range(8)]

                    def lprep(i):
                        src, src_bufs, tb, nb, col, kbase, kb0, is_x = lblocks[i]
                        v = dview(src)
                        xb, xbb = xr.next()
                        S.dma("sync", xb[:, :, :nb], v[:, :, tb * nb:(tb + 1) * nb], reads=[src_bufs[tb]], writes=[xbb])
                        norm_mod(xb, xbb, nb, 1, 0, col, hTs[i % 2], Bhs2[i % 2], tmps)

                    lprep(0)
                    for li in range(len(lblocks)):
                        src, src_bufs, tb, nb, col, kbase, kb0, is_x = lblocks[li]
                        hT, Bh = hTs[li % 2], Bhs2[li % 2]
                        if li + 1 < len(lblocks):
                            lprep(li + 1)
                        if True:
                            kc0 = kbase + tb * nb
                            for c in (range(5) if is_x else range(3, 5)):
                                pz, pzb = bank()
                                for k in range(8):
                                    mm(pz[:, :nb], wi[:, k, c * 128:(c + 1) * 128], hT[:, k, :nb], k == 0, k == 7, [Bwi, Bh[k]], pzb)
                                act(zl[:, c, :nb], pz[:, :nb], AF.Copy, [pzb], [Bzl[c]])
                            if is_x:
                                r, rb = rms_stat(lambda j: zl[:, j, :nb], lambda j: Bzl[j], nb, (sqr, rst), 3, 1.0 / 384)
                                for c in range(3):
                                    stt(qn[:, c, tb * nb:(tb + 1) * nb], zl[:, c, :nb], vv("gq", c), r[:, :nb], ALU.mult, ALU.mult,
                                        [Bzl[c], Bvecs, rb], [Bqn[tb]])
                            r, rb = rms_stat(lambda j: zl[:, 3 + j, :nb], lambda j: Bzl[3 + j], nb, (sqr, rst), 2, 1.0 / 256)
                            for c in range(2):
                                stt(kvn[:, c, kc0:kc0 + nb], zl[:, 3 + c, :nb], vv("gkv", c), r[:, :nb], ALU.mult, ALU.mult,
                                    [Bzl[3 + c], Bvecs, rb], [Bkvn[kb0 + tb]])
                            pk, pkb = bank()
                            for k in range(8):
                                mm(pk[:, :nb], wi[:, k, 640:768], hT[:, k, :nb], k == 0, k == 7, [Bwi, Bh[k]], pkb)
                            if not is_x:
                                act(kpe2[0:64, 0, kc0:kc0 + nb], pk[0:64, :nb], AF.Copy, [pkb, Bkz], [Bkpe[kb0 + tb]])
                                vcopy(kpe2[64:128, 1, kc0:kc0 + nb], pk[64:128, :nb], [pkb, Bkz, Bkpe[kb0 + tb]], [Bkpe[kb0 + tb]])
                            else:
                                pw, pwb = bank()
                                for k in range(8):
                                    mm(pw[:, :nb], wi[:, k, 768:896], hT[:, k, :nb], k == 0, k == 7, [Bwi, Bh[k]], pwb)
                                rt, rtb = rp.next()
                                S.dma("sync", rt[:, :, :nb], rope_in[:, :, tb * nb:(tb + 1) * nb], writes=[rtb])
                                t1, t1b = tmr.next()
                                tt(t1[:, :nb], pk[:, :nb], rt[:, 0, :nb], ALU.mult, [pkb, rtb], [t1b])
                                t2, t2b = tmr.next()
                                tt(t2[:, :nb], pw[:, :nb], rt[:, 1, :nb], ALU.mult, [pwb, rtb], [t2b])
                                tt(kpe2[0:64, 0, kc0:kc0 + nb], t1[0:64, :nb], t2[0:64, :nb], ALU.add, [t1b, t2b, Bkz], [Bkpe[kb0 + tb]], eng="gpsimd")
                                tt(kpe2[64:128, 1, kc0:kc0 + nb], t1[64:128, :nb], t2[64:128, :nb], ALU.add, [t1b, t2b, Bkz, Bkpe[kb0 + tb]],
                                   [Bkpe[kb0 + tb]], eng="gpsimd")
                                for hp in range(4):
                                    pq, pqb = bank()
                                    pq2, pq2b = bank()
                                    for k in range(3):
                                        mm(pq[:, :nb], wuqr[:, k, hp * 128:(hp + 1) * 128], qn[:, k, tb * nb:(tb + 1) * nb],
                                           k == 0, k == 2, [Bwuqr, Bqn[tb]], pqb)
                                    for k in range(3):
                                        mm(pq2[:, :nb], wuqr[:, k, 512 + hp * 128:512 + (hp + 1) * 128], qn[:, k, tb * nb:(tb + 1) * nb],
                                           k == 0, k == 2, [Bwuqr, Bqn[tb]], pq2b)
                                    t1, t1b = tmr.next()
                                    tt(t1[:, :nb], pq[:, :nb], rt[:, 0, :nb], ALU.mult, [pqb, rtb], [t1b])
                                    t2, t2b = tmr.next()
                                    tt(t2[:, :nb], pq2[:, :nb], rt[:, 1, :nb], ALU.mult, [pq2b, rtb], [t2b])
                                    tt(qrp[:, hp, tb * nb:(tb + 1) * nb], t1[:, :nb], t2[:, :nb], ALU.add, [t1b, t2b], [Bqrp[tb]], eng="gpsimd")
                S.barrier()
                att = alloc("att", [128, 8, T], BF16)
                with ExitStack() as p2:
                    al2 = mk_alloc(p2)
                    KT = al2("KT", [128, NK], BF16); BKT = S.buf("KT")
                    Vh = al2("Vh", [128, 34, 128], BF16); BVh = S.buf("Vh")
                    Qh = al2("Qh", [128, T], BF16); BQh = S.buf("Qh")
                    pr = Rot(S, al2, "pT", 4, [128, 512], BF16)
                    rr = Rot(S, al2, "rr", 1, [128, 512], F32)
                    sb_banks = (0, 1, 2, 3)
                    acc_o = (4, 5)
                    acc_s = (6, 7)
                    allkv = Bkvn
                    for h in range(8):
                        hh, hp = h % 2, h // 2
                        for cb in range(0, NK, 512):
                            w = min(512, NK - cb)
                            pk, pkb = bank(sb_banks)
                            for k in range(2):
                                mm(pk[:, :w], wukv[:, k, h * 128:(h + 1) * 128], kvn[:, k, cb:cb + w], k == 0, k == 1, [Bwukv] + allkv, pkb)
                            act(KT[:, cb:cb + w], pk[:, :w], AF.Copy, [pkb], [BKT])
                        for kt0 in range(0, 34, 4):
                            nt = min(4, 34 - kt0)
                            pv, pvb = bank(sb_banks)
                            for i4 in range(nt):
                                kt = kt0 + i4
                                for k in range(2):
                                    mm(pv[:, i4 * 128:(i4 + 1) * 128], kvn[:, k, kt * 128:(kt + 1) * 128],
                                       wukv[:, k, 1024 + h * 128:1024 + (h + 1) * 128], k == 0, k == 1, [Bwukv] + allkv, pvb)
                            vcopy(Vh[:, kt0:kt0 + nt, :], pv[:, :nt * 128].rearrange("p (a b) -> p a b", a=nt), [pvb], [BVh])
                        for qb in range(8):
                            pq, pqb = bank(sb_banks)
                            for k in range(3):
                                mm(pq[:, :], wuq[:, k, h * 128:(h + 1) * 128], qn[:, k, qb * 512:(qb + 1) * 512], k == 0, k == 2,
                                   [Bwuq, Bqn[qb]], pqb)
                            act(Qh[:, qb * 512:(qb + 1) * 512], pq[:, :], AF.Copy, [pqb], [BQh])
                        for qb in range(8):
                            po, pob = bank(acc_o)
                            pS, pSb = bank(acc_s)
                            LAG = 2
                            pts = {}
                            for kt in range(34 + LAG):
                                if kt < 34:
                                    ps_, psb = bank(sb_banks)
                                    mm(ps_[:, :], KT[:, kt * 128:(kt + 1) * 128], Qh[:, qb * 512:(qb + 1) * 512], True, False, [BKT, BQh], psb)
                                    mm(ps_[:, :], kpe2[:, hh, kt * 128:(kt + 1) * 128],
                                       qrp[:, hp, qb * 512:(qb + 1) * 512], False, True, Bkpe + [Bqrp[qb]], psb)
                                    pT, pTb = pr.next()
                                    act(pT[:, :], ps_[:, :], AF.Exp, [psb], [pTb], scale=SCALE)
                                    pts[kt] = (pT, pTb)
                                if kt >= LAG:
                                    k2 = kt - LAG
                                    pT, pTb = pts.pop(k2)
                                    mm(po[:, :], Vh[:, k2, :], pT[:, :], k2 == 0, k2 == 33, [BVh, pTb], pob)
                                    mm(pS[:, :], onesb[:], pT[:, :], k2 == 0, k2 == 33, [Bonesb, pTb], pSb)
                            rc, rcb = rr.next()
                            recip(rc[:, :], pS[:, :], [pSb], [rcb])
                            tt(att[:, h, qb * 512:(qb + 1) * 512], po[:, :], rc[:, :], ALU.mult, [pob, rcb], [Batt[h][qb]])
                S.barrier()
                with ExitStack() as p3:
                    al3 = mk_alloc(p3)
                    wo = al3("wo", [128, 8, 1024], BF16); Bwo = S.buf("wo")
                    S.dma("gpsimd", wo[:], o_w_o.rearrange("(k p) n -> p k n", p=128), writes=[Bwo])
                    xr = Rot(S, al3, "xc3", 1, [128, 8, 512], F32)
                    v = dview(xs)
                    for tb in range(8):
                        xb, xbb = xr.next()
                        S.dma("sync", xb[:], v[:, :, tb * 512:(tb + 1) * 512], reads=[Bxs[tb]], writes=[xbb])
                        xn, xnb = xb, xbb
                        for dm in range(8):
                            po, pob = bank()
                            for h in range(8):
                                mm(po[:, :], wo[:, h, dm * 128:(dm + 1) * 128], att[:, h, tb * 512:(tb + 1) * 512], h == 0, h == 7,
                                   [Bwo, Batt[h][tb]], pob)
                            stt(xn[:, dm, :], po[:, :], mod(1, 2, dm, 0), xb[:, dm, :], ALU.mult, ALU.add, [pob, Bmod, xbb], [xnb])
                        S.dma("sync", v[:, :, tb * 512:(tb + 1) * 512], xn[:], reads=[xnb], writes=[Bxs[tb]])

        def phase_D():
            with ExitStack() as px:
                alloc = mk_alloc(px)
                st = ffn_engine(alloc)
                rt32 = alloc("rt32", [128, 8, 8]); Brt = S.buf("rt32")
                selt = alloc("selt", [8, 8, 128]); Bsel = S.buf("sel")
                S.dma("sync", rt32[:], o_router.rearrange("(k p) e -> p k e", p=128), writes=[Brt])
                S.dma("sync", selt[:], sel_in, writes=[Bsel])
                xr = Rot(S, alloc, "xd", 2, [128, 8, 512], F32)
                hT = alloc("hTd", [128, 8, 512], BF16); Bh = S.bufs("hd", 8)
                h32 = alloc("h32", [128, 8, 512]); Bh32 = S.bufs("h32_", 8)
                yacc = alloc("yaccd", [128, 8, 512]); By = S.bufs("yd", 8)
                gbt = alloc("gbt", [128, 8, 512]); Bgb = S.bufs("gb", 8)
                gT = alloc("gT", [8, 512]); BgT = S.buf("gT")
                sqr = Rot(S, alloc, "sqd", 2, [128, 512], F32)
                rst = Rot(S, alloc, "rsd", 2, [128, 512], F32)
                tmr = Rot(S, alloc, "tmd", 3, [128, 512], F32)
                sm = Rot(S, alloc, "smd", 12, [128, 8], F32)
                tmps = (sqr, rst, tmr)
                v = dview(xs)
                ov = dview(outT)
                experts = []
                for tb in range(8):
                    xb, xbb = xr.next()
                    S.dma("sync", xb[:], v[:, :, tb * 512:(tb + 1) * 512], reads=[Bxs[tb]], writes=[xbb])
                    norm_mod(xb, xbb, 512, 1, 1, 0, hT, Bh, tmps, h32=h32, Bh32=Bh32)
                    for t4 in range(4):
                        pl, plb = bank()
                        for k in range(8):
                            mm(pl[:, 0:8], h32[:, k, t4 * 128:(t4 + 1) * 128], rt32[:, k, :], k == 0, k == 7, [Bh32[k], Brt], plb)
                        lg, lgb = sm.next()
                        vcopy(lg[:, :], pl[:, 0:8], [plb], [lgb])
                        mx8, mxb = sm.next()
                        S.op("vector", lambda e, mx8=mx8, lg=lg: e.max(out=mx8[:, :], in_=lg[:, :]), reads=[lgb], writes=[mxb])
                        nm, nmb = sm.next()
                        ts(nm[:, 0:1], mx8[:, 0:1], -1.0, None, ALU.mult, None, [mxb], [nmb])
                        ex, exb = sm.next()
                        act(ex[:, :], lg[:, :], AF.Exp, [lgb, nmb], [exb], bias=nm[:, 0:1], scale=1.0)
                        mk, mkb = sm.next()
                        ts(mk[:, :], lg[:, :], mx8[:, 1:2], None, ALU.is_ge, None, [lgb, mxb], [mkb])
                        me, meb = sm.next()
                        tt(me[:, :], mk[:, :], ex[:, :], ALU.mult, [mkb, exb], [meb])
                        dn, dnb = sm.next()
                        S.op("vector", lambda e, dn=dn, me=me: e.tensor_reduce(out=dn[:, 0:1], in_=me[:, :], axis=mybir.AxisListType.X, op=ALU.add),
                             reads=[meb], writes=[dnb])
                        rd, rdb = sm.next()
                        recip(rd[:, 0:1], dn[:, 0:1], [dnb], [rdb])
                        gt, gtb = sm.next()
                        ts(gt[:, :], me[:, :], rd[:, 0:1], None, ALU.mult, None, [meb, rdb], [gtb])
                        ptr, ptrb = bank()
                        S.op("tensor", lambda e, ptr=ptr, gt=gt: e.transpose(out=ptr[0:8, 0:128], in_=gt[:, :], identity=ident[:]),
                             reads=[gtb, Bident], writes=[ptrb])
                        vcopy(gT[:, t4 * 128:(t4 + 1) * 128], ptr[0:8, 0:128], [ptrb], [BgT])
                    for e_ in range(8):
                        pg, pgb = bank()
                        mm(pg[:, :], selt[:, e_, :], gT[:, :], True, True, [Bsel, BgT], pgb)
                        act(gbt[:, e_, :], pg[:, :], AF.Copy, [pgb], [Bgb[e_]])
                    ffn_run(st, hT, Bh, 512, experts, 28, lambda ei: (gbt[:, ei, :], Bgb[ei]), yacc, By)
                    xn, xnb = xb, xbb
                    for dm in range(8):
                        stt(xn[:, dm, :], yacc[:, dm, :], mod(1, 5, dm, 0), xb[:, dm, :], ALU.mult, ALU.add, [By[dm], Bmod, xbb], [xnb])
                    r, rb = rms_stat(lambda j: xn[:, j, :], xnb, 512, (sqr, rst), 8, 1.0 / D)
                    for dm in range(8):
                        stt(yacc[:, dm, :], xn[:, dm, :], vv("fg", dm), r[:, :], ALU.mult, ALU.mult, [xnb, Bvecs, rb], [By[dm]])
                    S.dma("sync", ov[:, :, tb * 512:(tb + 1) * 512], yacc[:], reads=By, writes=[Bout[tb]])


        I32 = mybir.dt.int32
        JT = 24

        def phase_D2():
            with ExitStack() as px:
                alloc = mk_alloc(px)
                rt32 = alloc("rt32", [128, 8, 8]); Brt = S.buf("rt32")
                utri = alloc("utri", [128, 128]); Butri = S.buf("utri")
                ibase = alloc("ibase", [128, 7]); Bib = S.buf("ibase")
                identb = alloc("identb", [128, 128], BF16); Bidb = S.buf("identb")
                S.dma("sync", rt32[:], o_router.rearrange("(k p) e -> p k e", p=128), writes=[Brt])
                S.dma("sync", utri[:], utri_in, writes=[Butri])
                S.dma("sync", ibase[:], ibase_in, writes=[Bib])
                vcopy(identb[:], ident[:], [Bident], [Bidb])
                M_all = alloc("M_all", [128, 32, 8]); BM = S.bufs("Mall", 32)
                G_all = alloc("G_all", [128, 32, 8]); BGa = S.bufs("Gall", 32)
                R_all = alloc("R_all", [128, 32, 8]); BR = S.buf("Rall")
                P12f = alloc("P12f", [128, 64]); BP12f = S.bufs("P12f", 32)
                P12i = alloc("P12i", [128, 64], I32); BP12i = S.buf("P12i")
                G12 = alloc("G12", [128, 64]); BG12 = S.bufs("G12", 32)
                idxf = alloc("idxf", [128, JT * 7]); Bidxf = S.buf("idxf")
                idxi = alloc("idxi", [128, JT * 7], I32); Bidxi = S.buf("idxi")
                Bhz = S.buf("hz"); Bhs = S.buf("hs"); Bys = S.buf("ys")
                Bs2z = S.buf("s2z"); Bs2t = S.buf("s2t"); Bytok = S.buf("ytok")
                tokid = alloc("tokid", [128, 64, 16], I32); Btokid = S.buf("tokid")
                S.dma("sync", tokid[:], tokid_in, writes=[Btokid])
                with ExitStack() as p1:
                    al1 = mk_alloc(p1)
                    h_tm = al1("h_tm", [128, 32, 1024], BF16); Bhtm = S.bufs("htm", 32)
                    zt = al1("zt", [128, 8, 1024], BF16); Bzt = S.buf("zt")
                    S.op("gpsimd", lambda e: e.memset(zt[:], 0.0), writes=[Bzt])
                    for r in range(NS // 1024):
                        S.dma("sync", hsort[r * 1024:(r + 1) * 1024, :].rearrange("(r p) d -> p r d", p=128), zt[:],
                              reads=[Bzt], writes=[Bhz], join=True)
                    oobt = al1("oobt", [128, 96 * 16], I32); Boob = S.buf("oobt")
                    S.dma("sync", oobt[:], oobfill_in, writes=[Boob])
                    S.dma("sync", slot2tok.rearrange("(p r) o -> p (r o)", p=128), oobt[:], reads=[Boob], writes=[Bs2z])
                    xr = Rot(S, al1, "xd", 2, [128, 8, 512], F32)
                    h32s = [al1(f"h32_{i}", [128, 8, 512]) for i in range(2)]
                    Bh32s = [S.bufs(f"h32_{i}_", 8) for i in range(2)]
                    sqr = Rot(S, al1, "sqd", 2, [128, 512], F32)
                    rst = Rot(S, al1, "rsd", 2, [128, 512], F32)
                    tmr = Rot(S, al1, "tmd", 3, [128, 512], F32)
                    sm = Rot(S, al1, "smd", 48, [128, 8], F32)
                    tmps = (sqr, rst, tmr)
                    v = dview(xs)
                    for tb in range(8):
                        xb, xbb = xr.next()
                        S.dma("sync", xb[:], v[:, :, tb * 512:(tb + 1) * 512], reads=[Bxs[tb]], writes=[xbb])
                        h32, Bh32 = h32s[tb % 2], Bh32s[tb % 2]
                        norm_mod(xb, xbb, 512, 1, 1, 0, None, None, tmps, h32=h32, Bh32=Bh32)
                        for t4 in range(4):
                            c = tb * 4 + t4
                            pl, plb = bank()
                            for k in range(8):
                                mm(pl[:, 0:8], h32[:, k, t4 * 128:(t4 + 1) * 128], rt32[:, k, :], k == 0, k == 7, [Bh32[k], Brt], plb)
                            lg, lgb = sm.next()
                            vcopy(lg[:, :], pl[:, 0:8], [plb], [lgb])
                            mx8, mxb = sm.next()
                            S.op("vector", lambda e, mx8=mx8, lg=lg: e.max(out=mx8[:, :], in_=lg[:, :]), reads=[lgb], writes=[mxb])
                            nm, nmb = sm.next()
                            ts(nm[:, 0:1], mx8[:, 0:1], -1.0, None, ALU.mult, None, [mxb], [nmb])
                            ex, exb = sm.next()
                            act(ex[:, :], lg[:, :], AF.Exp, [lgb, nmb], [exb], bias=nm[:, 0:1], scale=1.0)
                            ts(M_all[:, c, :], lg[:, :], mx8[:, 1:2], None, ALU.is_ge, None, [lgb, mxb], [BM[c]])
                            me, meb = sm.next()
                            tt(me[:, :], M_all[:, c, :], ex[:, :], ALU.mult, [BM[c], exb], [meb])
                            dn, dnb = sm.next()
                            S.op("vector", lambda e, dn=dn, me=me: e.tensor_reduce(out=dn[:, 0:1], in_=me[:, :], axis=mybir.AxisListType.X, op=ALU.add),
                                 reads=[meb], writes=[dnb])
                            rd, rdb = sm.next()
                            recip(rd[:, 0:1], dn[:, 0:1], [dnb], [rdb])
                            ts(G_all[:, c, :], me[:, :], rd[:, 0:1], None, ALU.mult, None, [meb, rdb], [BGa[c]])
                            for half in range(2):
                                pt, ptb = bank()
                                for q in range(4):
                                    dk = half * 4 + q
                                    S.op("tensor", lambda e, pt=pt, q=q, dk=dk, t4=t4, h32=h32: e.transpose(
                                        out=pt[:, q * 128:(q + 1) * 128], in_=h32[:, dk, t4 * 128:(t4 + 1) * 128], identity=ident[:]),
                                        reads=[Bh32[dk], Bident], writes=[ptb])
                                if half == 0:
                                    act(h_tm[:, c, 0:512], pt[:, :], AF.Copy, [ptb], [Bhtm[c]])
                                else:
                                    vcopy(h_tm[:, c, 512:1024], pt[:, :], [ptb, Bhtm[c]], [Bhtm[c]])
                    sm2 = Rot(S, al1, "sm2", 24, [128, 8], F32)
                    pc_, pcb = bank()
                    for c in range(32):
                        mm(pc_[:, 0:8], ones[:], M_all[:, c, :], c == 0, c == 31, [Bones, BM[c]], pcb)
                    keep = Rot(S, al1, "keep", 5, [128, 8], F32)
                    cnt, cntb = keep.next()
                    vcopy(cnt[:, :], pc_[:, 0:8], [pcb], [cntb])
                    prk, prkb = bank()
                    for c in range(32):
                        mm(prk[:, c * 8:(c + 1) * 8], utri[:], M_all[:, c, :], True, c == 0, [Butri, BM[c]], prkb)
                        for c2 in range(c):
                            mm(prk[:, c * 8:(c + 1) * 8], ones[:], M_all[:, c2, :], False, c2 == c - 1, [Bones, BM[c2]], prkb)
                    vcopy(R_all[:].rearrange("p a b -> p (a b)"), prk[:, 0:256], [prkb], [BR])
                    tl, tlb = keep.next()
                    ts(tl[:, :], cnt[:, :], 0.0, None, ALU.is_gt, None, [cntb], [tlb])
                    for m in range(1, 8):
                        stt(tl[:, :], cnt[:, :], 512.0 * m, tl[:, :], ALU.is_gt, ALU.add, [cntb, tlb], [tlb])
                    pcv, pcvb = keep.next()
                    ts(pcv[:, :], tl[:, :], 512.0, None, ALU.mult, None, [tlb], [pcvb])
                    off, offb = keep.next()
                    memset(off[:, 0:1], 0.0, [offb])
                    for e_ in range(1, 8):
                        tt(off[:, e_:e_ + 1], off[:, e_ - 1:e_], pcv[:, e_ - 1:e_], ALU.add, [offb, pcvb], [offb])
                    endv, endb = keep.next()
                    tt(endv[:, :], off[:, :], pcv[:, :], ALU.add, [offb, pcvb], [endb])
                    ej = al1("ej", [128, JT]); Bej = S.buf("ej")
                    for j in range(JT):
                        cm, cmb = sm2.next()
                        ts(cm[:, :], endv[:, :], float(j * 512), None, ALU.is_le, None, [endb], [cmb])
                        S.op("vector", lambda e, cm=cm, j=j: e.tensor_reduce(out=ej[:, j:j + 1], in_=cm[:, :], axis=mybir.AxisListType.X, op=ALU.add),
                             reads=[cmb], writes=[Bej])
                    ej2 = al1("ej2", [128, JT]); Bej2 = S.buf("ej2")
                    ts(ej2[:, :], ej[:, :], 7.0, 1792.0, ALU.min, ALU.mult, [Bej], [Bej2])
                    for j in range(JT):
                        ts(idxf[:, j * 7:(j + 1) * 7], ibase[:, :], ej2[:, j:j + 1], None, ALU.add, None, [Bib, Bej2, Bidxf], [Bidxf])
                    vcopy(idxi[:, :], idxf[:, :], [Bidxf], [Bidxi])
                    for c in range(32):
                        a1, a1b = sm2.next()
                        stt(a1[:, :], R_all[:, c, :], 1.0, off[:, :], ALU.add, ALU.add, [BR, offb], [a1b])
                        a2, a2b = sm2.next()
                        tt(a2[:, :], a1[:, :], M_all[:, c, :], ALU.mult, [a1b, BM[c]], [a2b])
                        pm, pmb = sm2.next()
                        ts(pm[:, :], a2[:, :], -1.0, None, ALU.add, None, [a2b], [pmb])
                        mx, mxb = sm2.next()
                        S.op("vector", lambda e, mx=mx, pm=pm: e.max(out=mx[:, :], in_=pm[:, :]), reads=[pmb], writes=[mxb])
                        vcopy(P12f[:, c * 2:c * 2 + 2], mx[:, 0:2], [mxb], [BP12f[c]])
                        for k2 in range(2):
                            eq, eqb = sm2.next()
                            ts(eq[:, :], pm[:, :], mx[:, k2:k2 + 1], None, ALU.is_equal, None, [pmb, mxb], [eqb])
                            eg, egb = sm2.next()
                            tt(eg[:, :], eq[:, :], G_all[:, c, :], ALU.mult, [eqb, BGa[c]], [egb])
                            S.op("vector", lambda e, eg=eg, c=c, k2=k2: e.tensor_reduce(out=G12[:, c * 2 + k2:c * 2 + k2 + 1], in_=eg[:, :],
                                                                                         axis=mybir.AxisListType.X, op=ALU.add),
                                 reads=[egb, BG12[c]], writes=[BG12[c]])
                    vcopy(P12i[:, :], P12f[:, :], BP12f, [BP12i])
                    dbg_dump("cnt", cnt[:, :], [128, 8], [cntb])
                    dbg_dump("P12f", P12f[:, :], [128, 64], BP12f)
                    dbg_dump("G12", G12[:, :], [128, 64], BG12)
                    dbg_dump("ej", ej[:, :], [128, JT], [Bej])
                    dbg_dump("off", off[:, :], [128, 8], [offb])
                    dbg_dump("idxf", idxf[:, :], [128, JT * 7], [Bidxf])
                    dbg_dump("M_all", M_all[:].rearrange("p a b -> p (a b)"), [128, 256], BM)
                    dbg_dump("R_all", R_all[:].rearrange("p a b -> p (a b)"), [128, 256], [BR])
                    dbg_dump("h_tm", h_tm[:].rearrange("p a b -> p (a b)"), [128, 32 * 1024], Bhtm, dt=BF16)
                    for c in range(32):
                        for k2 in range(2):
                            def sc(e, c=c, k2=k2):
                                return e.indirect_dma_start(out=hsort, out_offset=bass.IndirectOffsetOnAxis(ap=P12i[:, c * 2 + k2:c * 2 + k2 + 1], axis=0),
                                                            in_=h_tm[:, c, :], in_offset=None)
                            S.dma_fn("gpsimd", sc, reads=[BP12i, Bhtm[c], Bhz], writes=[Bhs], join=True)

                            def sci(e, c=c, k2=k2):
                                return e.indirect_dma_start(out=slot2tok, out_offset=bass.IndirectOffsetOnAxis(ap=P12i[:, c * 2 + k2:c * 2 + k2 + 1], axis=0),
                                                            in_=tokid[:, c * 2 + k2, :], in_offset=None)
                            S.dma_fn("gpsimd", sci, reads=[BP12i, Btokid, Bs2z], writes=[Bs2t], join=True)
                S.barrier()
                with ExitStack() as p4:
                    al4 = mk_alloc(p4)
                    st = ffn_engine(al4)
                    hsr = Rot(S, al4, "hs_tm", 2, [128, 4, 1024], BF16)
                    hsTr = [al4(f"hsT{i}", [128, 8, 512], BF16) for i in range(2)]
                    BhsT = [S.bufs(f"hsT{i}_", 8) for i in range(2)]
                    yaccr = [al4(f"yaccs{i}", [128, 8, 512]) for i in range(2)]
                    Byr = [S.bufs(f"ys{i}_", 8) for i in range(2)]
                    ysr = Rot(S, al4, "ys_tm", 2, [128, 4, 1024], F32)
                    idr = Rot(S, al4, "idt", 2, [128, 64], I32)
                    Bscat = S.bufs("scat", 2)

                    def dyn_loader(j):
                        def load(f0, gs, w1t, w1b, w3t, w3b, w2t, w2b):
                            fg = f0 // 4
                            col = j * 7 + fg
                            for (tab, wt_, wb_) in ((x_w1, w1t, w1b), (x_w3, w3t, w3b), (x_w2, w2t, w2b)):
                                flat = wt_[:].rearrange("p a b -> p (a b)")
                                for hf in range(2):
                                    def g(e, tab=tab, flat=flat, hf=hf, col=col):
                                        return e.indirect_dma_start(out=flat[:, hf * 2048:(hf + 1) * 2048], out_offset=None, in_=tab,
                                                                    in_offset=bass.IndirectOffsetOnAxis(ap=idxi[:, col:col + 1], axis=0),
                                                                    element_offset=hf * 2048)
                                    S.dma_fn("gpsimd", g, reads=[Bidxi], writes=[wb_], join=(hf == 1))
                        return load

                    def prologue(j):
                        hs_t, hs_b = hsr.next()
                        S.dma("sync", hs_t[:], hsort[j * 512:(j + 1) * 512, :].rearrange("(r p) d -> p r d", p=128), reads=[Bhz, Bhs], writes=[hs_b])
                        hsT, Bh_ = hsTr[j % 2], BhsT[j % 2]
                        for dk in range(8):
                            pt, ptb = bank()
                            for r in range(4):
                                mm(pt[:, r * 128:(r + 1) * 128], hs_t[:, r, dk * 128:(dk + 1) * 128], identb[:], True, True, [hs_b, Bidb], ptb)
                            if dk % 2 == 0:
                                act(hsT[:, dk, :], pt[:, :], AF.Copy, [ptb], [Bh_[dk]])
                            else:
                                vcopy(hsT[:, dk, :], pt[:, :], [ptb], [Bh_[dk]])

                    def epilogue(j):
                        yacc, By = yaccr[j % 2], Byr[j % 2]
                        ys_t, ys_b = ysr.next()
                        for r in range(4):
                            for half in range(2):
                                pt, ptb = bank()
                                for q in range(4):
                                    dm = half * 4 + q
                                    S.op("tensor", lambda e, pt=pt, q=q, dm=dm, r=r, yacc=yacc: e.transpose(
                                        out=pt[:, q * 128:(q + 1) * 128], in_=yacc[:, dm, r * 128:(r + 1) * 128], identity=ident[:]),
                                        reads=[By[dm], Bident], writes=[ptb])
                                if half == 0:
                                    act(ys_t[:, r, 0:512], pt[:, :], AF.Copy, [ptb, ys_b], [ys_b])
                                else:
                                    vcopy(ys_t[:, r, 512:1024], pt[:, :], [ptb, ys_b], [ys_b])
                        idt, idb = idr.next()
                        S.dma("sync", idt[:].rearrange("p (r o) -> p r o", r=4), slot2tok[j * 512:(j + 1) * 512, :].rearrange("(r p) o -> p r o", p=128), reads=[Bs2z, Bs2t], writes=[idb])
                        for r in range(4):
                            def scy(e, r=r, idt=idt, ys_t=ys_t):
                                return e.indirect_dma_start(out=ytok, out_offset=bass.IndirectOffsetOnAxis(ap=idt[:, r * 16:r * 16 + 1], axis=0),
                                                            in_=ys_t[:, r, :], in_offset=None)
                            S.dma_fn("gpsimd", scy, reads=[idb, ys_b], writes=[Bscat[j % 2]], join=True)

                    prologue(0)
                    for j in range(JT):
                        if j + 1 < JT:
                            prologue(j + 1)
                        ffn_run(st, hsTr[j % 2], BhsT[j % 2], 512, [dyn_loader(j)], 28, None, yaccr[j % 2], Byr[j % 2],
                                next_loader=dyn_loader(j + 1) if j + 1 < JT else None)
                        if j >= 1:
                            epilogue(j - 1)
                    epilogue(JT - 1)
                S.barrier()
                if DEBUG:
                    tq = nc.dram_tensor("dbg_hsort", [NS, D], BF16, kind="ExternalOutput").ap()
                    S.dma("sync", tq, hsort, reads=[Bhz, Bhs]); DBG_NAMES.append("dbg_hsort")
                    tq2 = nc.dram_tensor("dbg_ysort", [NS, D], F32, kind="ExternalOutput").ap()
                    S.dma("sync", tq2, ysort, reads=[Bys]); DBG_NAMES.append("dbg_ysort")
                with ExitStack() as p5:
                    al5 = mk_alloc(p5)
                    xr = Rot(S, al5, "xd5", 2, [128, 8, 512], F32)
                    y12r = Rot(S, al5, "y12", 8, [128, 2, 1024], F32)
                    ytr = Rot(S, al5, "yt", 3, [128, 1024], F32)
                    oo = Rot(S, al5, "oo", 2, [128, 8, 512], F32)
                    sqr = Rot(S, al5, "sq5", 2, [128, 512], F32)
                    rst = Rot(S, al5, "rs5", 2, [128, 512], F32)
                    v = dview(xs)
                    ov = dview(outT)
                    pre5 = {}

                    def prefetch5(tb):
                        if tb >= 8:
                            return
                        xb_, xbb_ = xr.next()
                        S.dma("sync", xb_[:], v[:, :, tb * 512:(tb + 1) * 512], reads=[Bxs[tb]], writes=[xbb_])
                        ys_ = []
                        for t4 in range(4):
                            c = tb * 4 + t4
                            y12_, y12b_ = y12r.next()
                            S.dma("sync", y12_[:], ytok[0:2 * T, :].rearrange("(t k) d -> t k d", k=2)[c * 128:(c + 1) * 128], reads=[Bytok], writes=[y12b_])
                            ys_.append((y12_, y12b_))
                        pre5[tb] = (xb_, xbb_, ys_)

                    prefetch5(0)
                    for tb in range(8):
                        prefetch5(tb + 1)
                        xb, xbb, ys_ = pre5.pop(tb)
                        for t4 in range(4):
                            c = tb * 4 + t4
                            y12, y12b = ys_[t4]
                            y1, y1b, y2, y2b = y12[:, 0, :], y12b, y12[:, 1, :], y12b
                            yt, ytb = ytr.next()
                            ts(yt[:, :], y1, G12[:, c * 2:c * 2 + 1], None, ALU.mult, None, [y1b, BG12[c]], [ytb])
                            stt(yt[:, :], y2, G12[:, c * 2 + 1:c * 2 + 2], yt[:, :], ALU.mult, ALU.add, [y2b, BG12[c], ytb], [ytb])
                            for dm in range(8):
                                S.op("tensor", lambda e, dm=dm, t4=t4, yt=yt: e.transpose(
                                    out=banks[dm][:, t4 * 128:(t4 + 1) * 128], in_=yt[:, dm * 128:(dm + 1) * 128], identity=ident[:]),
                                    reads=[ytb, Bident], writes=[bank_b[dm]])
                        for dm in range(8):
                            stt(xb[:, dm, :], banks[dm][:, :], mod(1, 5, dm, 0), xb[:, dm, :], ALU.mult, ALU.add, [bank_b[dm], Bmod, xbb], [xbb])
                        r, rb = rms_stat(lambda j: xb[:, j, :], xbb, 512, (sqr, rst), 8, 1.0 / D)
                        ot, otb = oo.next()
                        for dm in range(8):
                            stt(ot[:, dm, :], xb[:, dm, :], vv("fg", dm), r[:, :], ALU.mult, ALU.mult, [xbb, Bvecs, rb], [otb])
                        S.dma("sync", ov[:, :, tb * 512:(tb + 1) * 512], ot[:], reads=[otb], writes=[Bout[tb]])

        Bxs = S.bufs("xs", 8)
        Bcs = S.bufs("cs", 1)
        Bout = S.bufs("out", 8)
        phases = [("A", phase_A), ("B", phase_B), ("C", phase_C), ("D", phase_D2)]
        for name, fn in phases:
            fn()
            S.barrier()
            if name == stop_after:
                break
        if stop_after != "D":
            for tb in range(8):
                S.dma("sync", outT[:, tb * 512:(tb + 1) * 512], xs[:, tb * 512:(tb + 1) * 512], reads=[Bxs[tb]], writes=[Bout[tb]])
        S.barrier()
        S.emit()
        print(f"[build] insts={S.n_inst} waits={S.n_wait} sems={S.nsem}", flush=True)
    return nc


def _rope_tables():
    rows = T // 64
    row = np.repeat(np.arange(rows), 64).astype(np.float32)
    col = np.tile(np.arange(64), rows).astype(np.float32)
    inv = (10000.0 ** (-np.arange(16, dtype=np.float32) / 16)).astype(np.float32)
    ang = np.stack([row[:, None] * inv, col[:, None] * inv], axis=1).astype(np.float32)
    cos, sin = np.cos(ang).astype(np.float32), np.sin(ang).astype(np.float32)
    C = np.zeros((64, T), np.float32)
    Sg = np.zeros((64, T), np.float32)
    for p in range(64):
        ax, half, f = p // 32, (p // 16) % 2, p % 16
        C[p] = cos[:, ax, f]
        Sg[p] = sin[:, ax, f] * (-1.0 if half == 0 else 1.0)
    tab = np.stack([np.concatenate([C, C], 0), np.concatenate([Sg, Sg], 0)], axis=1)
    return np.ascontiguousarray(tab)


def _prep_shared(inp):
    f = lambda a: np.ascontiguousarray(np.asarray(a, dtype=np.float32))
    sh = {}
    sh["ident"] = np.eye(128, dtype=np.float32)
    sh["lnv"] = f(np.stack([np.broadcast_to(inp["e_ln_v_g"][0], (128, 512)), np.broadcast_to(inp["e_ln_v_b"][0], (128, 512))], axis=1))
    bs = inp["e_b_s"][0]
    sh["bs"] = f(np.repeat(bs.reshape(4, 2, 1, 128), 64, axis=2).reshape(4, 128, 128).transpose(1, 0, 2))
    sh["wsT"] = f(inp["e_w_s"][0].transpose(2, 0, 1))
    sh["rope"] = _rope_tables()
    sel = np.zeros((8, 8, 128), np.float32)
    for e in range(8):
        sel[e, e, :] = 1.0
    sh["sel"] = sel
    sh["w_ada"] = f(inp["w_ada"])
    sh["e_w_in"] = f(inp["e_w_in"][0])
    sh["e_w_out"] = f(inp["e_w_out"][0])
    sh["e_ffn_w1"] = f(inp["e_ffn_w1"][0])
    sh["e_ffn_w3"] = f(inp["e_ffn_w3"][0])
    sh["e_ffn_w2"] = f(inp["e_ffn_w2"][0])
    perm = np.arange(64) ^ 16
    wi = inp["o_w_in"][0]
    kp = wi[:, 640:704]
    sh["o_w_in_x"] = f(np.concatenate([wi[:, :640], kp, kp, kp[:, perm], kp[:, perm]], axis=1))
    wq = inp["o_w_uq"][0].reshape(384, 8, 192)
    nope = wq[:, :, :128].reshape(384, 1024)
    rp = wq[:, :, 128:]
    sh["o_wuq"] = f(np.concatenate([nope, rp.reshape(384, 512), rp[:, :, perm].reshape(384, 512)], axis=1))
    wkv = inp["o_w_ukv"][0].reshape(256, 8, 256)
    sh["o_wukv"] = f(np.concatenate([wkv[:, :, :128].reshape(256, 1024), wkv[:, :, 128:].reshape(256, 1024)], axis=1))
    sh["o_w_o"] = f(inp["o_w_o"][0])
    sh["o_router"] = f(inp["o_router"][0])
    w1 = np.asarray(inp["o_exp_w1"][0], np.float32).reshape(8, 8, 128, 7, 512)
    sh["xw1r"] = np.ascontiguousarray(w1.transpose(0, 3, 2, 1, 4)).reshape(8 * 7 * 128 * 2, 2048)
    w3 = np.asarray(inp["o_exp_w3"][0], np.float32).reshape(8, 8, 128, 7, 512)
    sh["xw3r"] = np.ascontiguousarray(w3.transpose(0, 3, 2, 1, 4)).reshape(8 * 7 * 128 * 2, 2048)
    w2 = np.asarray(inp["o_exp_w2"][0], np.float32).reshape(8, 7, 4, 128, 1024)
    sh["xw2r"] = np.ascontiguousarray(w2.transpose(0, 1, 3, 2, 4)).reshape(8 * 7 * 128 * 2, 2048)
    tid = ((np.arange(32)[None, :, None] * 128 + np.arange(128)[:, None, None]) * 2 + np.arange(2)[None, None, :]).reshape(128, 64)
    sh["tokid"] = np.ascontiguousarray(np.repeat(tid[:, :, None], 16, axis=2).astype(np.int32))
    sh["oobfill"] = np.ascontiguousarray(np.repeat((2 * T + np.arange(128 * 96, dtype=np.int32)).reshape(128, 96, 1), 16, axis=2).reshape(128, 96 * 16))
    sh["utri"] = np.triu(np.ones((128, 128), np.float32), 1)
    sh["ibase"] = (np.arange(7, dtype=np.float32)[None, :] * 256 + 2 * np.arange(128, dtype=np.float32)[:, None]).astype(np.float32)
    return sh


def _prep_vecs(inp, b):
    v = np.zeros((128, NV), np.float32)

    def put(name, arr):
        arr = np.asarray(arr, np.float32).reshape(128, -1)
        v[:, VOFF[name]:VOFF[name] + arr.shape[1]] = arr
    cp = lambda a: np.asarray(a, np.float32).reshape(-1, 128).T
    put("sc", np.stack([cp(inp["c"][b]), cp(inp["c_ctx"])], axis=-1))
    ba = np.asarray(inp["b_ada"], np.float32).reshape(2, 6, 8, 128).transpose(3, 0, 1, 2)
    put("bada", np.repeat(ba[..., None], 2, axis=-1))
    ng = np.asarray(inp["norm_g"], np.float32).reshape(2, 2, 8, 128).transpose(3, 0, 1, 2)
    put("ng", np.repeat(ng[..., None], 2, axis=-1))
    put("convw", np.asarray(inp["e_conv_w"][0], np.float32).reshape(31, 4, 128).transpose(2, 1, 0))
    put("convb", cp(inp["e_conv_b"][0]))
    put("lnag", cp(inp["e_ln_a_g"][0]))
    put("lnab", cp(inp["e_ln_a_b"][0]))
    put("gq", cp(inp["o_g_q"][0]))
    put("gkv", cp(inp["o_g_kv"][0]))
    put("fg", cp(inp["final_g"]))
    return v


def run(inputs, stop_after="D", cores=None):
    inp = {k: np.asarray(v) for k, v in inputs.items()}
    cores = list(range(NCORES)) if cores is None else cores
    sh = _prep_shared(inp)
    in_maps = []
    for b in cores:
        m = dict(sh)
        m["xT"] = np.ascontiguousarray(inp["x"][b].T.astype(np.float32))
        m["cT"] = np.ascontiguousarray(inp["ctx"][b].T.astype(np.float32))
        m["vecs"] = _prep_vecs(inp, b)
        in_maps.append(m)
    nc = build(stop_after)
    res = run_bass_kernel_spmd(nc, in_maps, core_ids=list(range(len(cores))))
    if DEBUG:
        for n in DBG_NAMES:
            DBG_OUT[n] = np.asarray(res.results[0][n])
    return np.stack([np.ascontiguousarray(r["outT"].T) for r in res.results], axis=0)


def kernel(**inputs):
    return run(inputs).astype(np.float32)
```

```python
import numpy as np
from contextlib import ExitStack
import concourse.bass as bass
import concourse.mybir as mybir
from concourse.bass_utils import run_bass_kernel_spmd

F32 = mybir.dt.float32
BF16 = mybir.dt.bfloat16
AF = mybir.ActivationFunctionType
ALU = mybir.AluOpType

T, TC, D, KD = 4096, 256, 1024, 8
EPS = 1e-6
NCORES = 8
SEM_LIMIT = 30000
DEBUG = False
DBG_NAMES = []
DBG_OUT = {}


class Buf:
    __slots__ = ("name", "w", "r", "dsem", "dcnt")

    def __init__(self, name):
        self.name = name
        self.w = {}
        self.r = {}
        self.dsem = None
        self.dcnt = 0


class Sched:
    def __init__(self, nc, ctx):
        self.nc = nc
        self.ctx = ctx
        self.prog = {e: [] for e in ("tensor", "vector", "scalar", "gpsimd", "sync")}
        self.esem = {}
        self.ecnt = {}
        self.nsem = 0
        self.allsems = {}
        for e in ("tensor", "vector", "scalar", "gpsimd"):
            self._new_esem(e)
        self.seen = {e: {} for e in self.prog}
        self.n_inst = 0
        self.n_wait = 0

    def _alloc_sem(self, name):
        self.nsem += 1
        s = self.ctx.enter_context(self.nc.semaphore(f"{name}_{self.nsem}"))
        self.allsems[id(s)] = [s, 0]
        return s

    def _new_esem(self, e):
        self.esem[e] = self._alloc_sem(f"e_{e}")
        self.ecnt[e] = 0

    def buf(self, name):
        return Buf(name)

    def bufs(self, name, n):
        return [Buf(f"{name}{i}") for i in range(n)]

    def _need(self, eng, toks):
        out = {}
        for (sem, val, teng) in toks:
            if teng == eng and eng == "tensor":
                continue
            k = id(sem)
            if self.seen[eng].get(k, 0) >= val:
                continue
            if k not in out or out[k][1] < val:
                out[k] = (sem, val)
        for k, (sem, val) in out.items():
            self.seen[eng][k] = val
        return list(out.values())

    def _deps(self, eng, reads, writes, join=False):
        toks = []
        for b in reads:
            toks += b.w.values()
        for b in writes:
            if not join:
                toks += b.w.values()
            toks += b.r.values()
        return self._need(eng, toks)

    def _record(self, tok, reads, writes, join=False):
        k = id(tok[0])
        self.allsems[k][1] = max(self.allsems[k][1], tok[1])
        for b in reads:
            b.r[k] = tok
        for b in writes:
            if join:
                b.w[k] = tok
            else:
                b.w = {k: tok}
                b.r = {}

    def op(self, eng, fn, reads=(), writes=()):
        reads = [b for b in reads if b is not None]
        writes = [b for b in writes if b is not None]
        waits = self._deps(eng, reads, writes)
        if self.ecnt[eng] >= SEM_LIMIT:
            self._new_esem(eng)
        sem = self.esem[eng]
        self.ecnt[eng] += 1
        tok = (sem, self.ecnt[eng], eng)
        self._record(tok, reads, writes)
        self.n_inst += 1
        self.n_wait += len(waits)

        def run(e, waits=waits, fn=fn, sem=sem):
            for (s, v) in waits:
                e.wait_ge(s, v)
            fn(e).then_inc(sem, 1)
        self.prog[eng].append(run)

    def dma(self, queue, out, in_, reads=(), writes=(), join=False, **kw):
        reads = [b for b in reads if b is not None]
        writes = [b for b in writes if b is not None]
        waits = self._deps(queue, reads, writes, join)
        sb = writes[0] if writes else reads[0]
        if sb.dsem is None:
            sb.dsem = self._alloc_sem("d")
        sb.dcnt += 16
        tok = (sb.dsem, sb.dcnt, "dma")
        self._record(tok, reads, writes, join)
        self.n_inst += 1
        self.n_wait += len(waits)

        def run(e, waits=waits, sem=sb.dsem):
            for (s, v) in waits:
                e.wait_ge(s, v)
            e.dma_start(out=out, in_=in_, **kw).then_inc(sem, 16)
        self.prog[queue].append(run)

    def dma_fn(self, queue, fn, reads=(), writes=(), join=False):
        reads = [b for b in reads if b is not None]
        writes = [b for b in writes if b is not None]
        waits = self._deps(queue, reads, writes, join)
        sb = writes[0] if writes else reads[0]
        if sb.dsem is None:
            sb.dsem = self._alloc_sem("d")
        sb.dcnt += 16
        tok = (sb.dsem, sb.dcnt, "dma")
        self._record(tok, reads, writes, join)
        self.n_inst += 1

        def run(e, waits=waits, sem=sb.dsem, fn=fn):
            for (s, v) in waits:
                e.wait_ge(s, v)
            fn(e).then_inc(sem, 16)
        self.prog[queue].append(run)

    def barrier(self):
        toks = [(s, v, "x") for (s, v) in self.allsems.values() if v > 0]
        for eng in self.prog:
            waits = self._need(eng, toks)

            def run(e, waits=waits):
                for (s, v) in waits:
                    e.wait_ge(s, v)
            self.prog[eng].append(run)

    def emit(self):
        with self.nc.Block() as block:
            @block.sync
            def _(e):
                for f in self.prog["sync"]:
                    f(e)

            @block.tensor
            def _(e):
                for f in self.prog["tensor"]:
                    f(e)

            @block.vector
            def _(e):
                for f in self.prog["vector"]:
                    f(e)

            @block.scalar
            def _(e):
                for f in self.prog["scalar"]:
                    f(e)

            @block.gpsimd
            def _(e):
                for f in self.prog["gpsimd"]:
                    f(e)


class Rot:
    def __init__(self, S, alloc, name, k, shape, dt):
        self.t = [alloc(f"{name}{i}", shape, dt) for i in range(k)]
        self.b = S.bufs(name, k)
        self.i = 0

    def next(self):
        i = self.i
        self.i = (i + 1) % len(self.t)
        return self.t[i], self.b[i]


VOFF = {}


def _vlayout():
    off = 0
    for name, n in [("sc", 16), ("bada", 192), ("ng", 64), ("convw", 124), ("convb", 4), ("lnag", 4),
                    ("lnab", 4), ("gq", 3), ("gkv", 2), ("fg", 8)]:
        VOFF[name] = off
        off += n
    return off


NV = _vlayout()


def build(stop_after="D"):
    nc = bass.Bass("TRN2", target_bir_lowering=False)

    def din(name, shape):
        return nc.dram_tensor(name, list(shape), F32, kind="ExternalInput").ap()

    xT_in = din("xT", [D, T])
    cT_in = din("cT", [D, TC])
    vecs_in = din("vecs", [128, NV])
    ident_in = din("ident", [128, 128])
    lnv_in = din("lnv", [128, 2, 512])
    bs_in = din("bs", [128, 4, 128])
    wsT_in = din("wsT", [128, 8, 128])
    rope_in = din("rope", [128, 2, T])
    sel_in = din("sel", [8, 8, 128])
    w_ada = din("w_ada", [2, D, 6 * D])
    e_w_in = din("e_w_in", [D, 2048])
    e_w_out = din("e_w_out", [D, D])
    f_w1 = din("e_ffn_w1", [D, 2816])
    f_w3 = din("e_ffn_w3", [D, 2816])
    f_w2 = din("e_ffn_w2", [2816, D])
    o_w_in = din("o_w_in_x", [D, 896])
    o_wuq = din("o_wuq", [384, 2048])
    o_wukv = din("o_wukv", [256, 2048])
    o_w_o = din("o_w_o", [D, D])
    o_router = din("o_router", [D, 8])
    NROW = 8 * 7 * 128 * 2
    x_w1 = din("xw1r", [NROW, 2048])
    x_w3 = din("xw3r", [NROW, 2048])
    x_w2 = din("xw2r", [NROW, 2048])
    utri_in = din("utri", [128, 128])
    tokid_in = nc.dram_tensor("tokid", [128, 64, 16], mybir.dt.int32, kind="ExternalInput").ap()
    oobfill_in = nc.dram_tensor("oobfill", [128, 96 * 16], mybir.dt.int32, kind="ExternalInput").ap()
    slot2tok = nc.dram_tensor("slot2tok", [24 * 512, 16], mybir.dt.int32).ap()
    ytok = nc.dram_tensor("ytok", [2 * T + 24 * 512, D], F32).ap()
    ibase_in = din("ibase", [128, 7])
    NS = 24 * 512
    hsort = nc.dram_tensor("hsort", [NS, D], BF16).ap()
    ysort = nc.dram_tensor("ysort", [NS, D], F32).ap()
    outT = nc.dram_tensor("outT", [D, T], F32, kind="ExternalOutput").ap()
    xs = nc.dram_tensor("xs", [D, T], F32).ap()
    cs = nc.dram_tensor("cs", [D, TC], F32).ap()

    def dview(ap):
        return ap.rearrange("(j p) n -> p j n", p=128)

    with ExitStack() as ctx:
        S = Sched(nc, ctx)

        uid = [0]

        def dbg_dump(name, ap, shape, reads, dt=F32):
            if not DEBUG:
                return
            t = nc.dram_tensor("dbg_" + name, list(shape), dt, kind="ExternalOutput").ap()
            S.dma("sync", t, ap, reads=reads)
            DBG_NAMES.append("dbg_" + name)

        def mk_alloc(stack):
            def alloc(name, shape, dt=F32):
                uid[0] += 1
                return stack.enter_context(nc.sbuf_tensor(f"sb{uid[0]}_{name}", list(shape), dt))
            return alloc
        galloc = mk_alloc(ctx)

        banks = [ctx.enter_context(nc.psum_tensor(f"pb{i}", [128, 512], F32)) for i in range(8)]
        bank_b = S.bufs("pb", 8)
        bstate = {"i": 0}

        def bank(subset=None):
            if subset is None:
                i = bstate["i"]
                bstate["i"] = (i + 1) % 8
            else:
                i = subset[bstate.setdefault(id(subset), 0) % len(subset)]
                bstate[id(subset)] += 1
            return banks[i], bank_b[i]

        def mm(out, lhsT, rhs, start, stop, reads, wb):
            S.op("tensor", lambda e: e.matmul(out, lhsT=lhsT, rhs=rhs, start=start, stop=stop), reads=reads, writes=[wb])

        def act(out, in_, func, reads, writes, **kw):
            S.op("scalar", lambda e: e.activation(out=out, in_=in_, func=func, **kw), reads=reads, writes=writes)

        def tt(out, a, b, op, reads, writes, eng="vector"):
            S.op(eng, lambda e: e.tensor_tensor(out=out, in0=a, in1=b, op=op), reads=reads, writes=writes)

        def stt(out, in0, scalar, in1, op0, op1, reads, writes, eng="vector"):
            S.op(eng, lambda e: e.scalar_tensor_tensor(out=out, in0=in0, scalar=scalar, in1=in1, op0=op0, op1=op1),
                 reads=reads, writes=writes)

        def ts(out, in0, s1, s2, op0, op1, reads, writes, eng="vector"):
            if s2 is None:
                S.op(eng, lambda e: e.tensor_scalar(out=out, in0=in0, scalar1=s1, scalar2=None, op0=op0), reads=reads, writes=writes)
            else:
                S.op(eng, lambda e: e.tensor_scalar(out=out, in0=in0, scalar1=s1, scalar2=s2, op0=op0, op1=op1),
                     reads=reads, writes=writes)

        def vcopy(out, in_, reads, writes, eng="vector"):
            S.op(eng, lambda e: e.tensor_copy(out=out, in_=in_), reads=reads, writes=writes)

        def recip(out, in_, reads, writes):
            S.op("vector", lambda e: e.reciprocal(out=out, in_=in_), reads=reads, writes=writes)

        def memset(ap, val, writes):
            S.op("vector", lambda e: e.memset(ap, val), writes=writes)

        vecs = galloc("vecs", [128, NV]); Bvecs = S.buf("vecs")
        ident = galloc("ident", [128, 128]); Bident = S.buf("ident")
        ones = galloc("ones", [128, 128]); Bones = S.buf("ones")
        onesb = galloc("onesb", [128, 128], BF16); Bonesb = S.buf("onesb")
        epsc = galloc("epsc", [128, 1]); Beps = S.buf("eps")
        modT = galloc("modT", [128, 2 * 6 * 8 * 2]); Bmod = S.buf("modT")
        Gt = galloc("Gt", [128, 2 * 2 * 8 * 2]); BG = S.buf("Gt")
        scs = galloc("scs", [128, 16]); Bscs = S.buf("scs")
        S.dma("sync", vecs[:], vecs_in, writes=[Bvecs])
        S.dma("sync", ident[:], ident_in, writes=[Bident])
        memset(ones[:], 1.0, [Bones])
        memset(onesb[:], 1.0, [Bonesb])
        memset(epsc[:], EPS, [Beps])

        def vv(name, idx, n=1):
            o = VOFF[name] + idx
            return vecs[:, o:o + n]

        def mod(i, v, j, col):
            o = ((i * 6 + v) * 8 + j) * 2 + col
            return modT[:, o:o + 1]

        def Gs(i, n, j, col):
            o = ((i * 2 + n) * 8 + j) * 2 + col
            return Gt[:, o:o + 1]

        with ExitStack() as px:
            alloc = mk_alloc(px)
            wa = Rot(S, alloc, "wa", 2, [128, 8, 1024], F32)
            act(scs[:], vv("sc", 0, 16), AF.Silu, [Bvecs], [Bscs])
            for i in range(2):
                wv = w_ada[i].rearrange("(k p) n -> p k n", p=128)
                for v in range(6):
                    wt, wb = wa.next()
                    S.dma("sync", wt[:], wv[:, :, v * 1024:(v + 1) * 1024], writes=[wb])
                    pb, pbb = bank()
                    for j in range(8):
                        for k in range(8):
                            mm(pb[:, j * 2:j * 2 + 2], wt[:, k, j * 128:(j + 1) * 128], scs[:, k * 2:k * 2 + 2],
                               k == 0, k == 7, [wb, Bscs], pbb)
                    o = (i * 6 + v) * 16
                    tt(modT[:, o:o + 16], pb[:, 0:16], vecs[:, VOFF["bada"] + o:VOFF["bada"] + o + 16], ALU.add,
                       [pbb, Bvecs], [Bmod])
            for i in range(2):
                for n in range(2):
                    vs = 1 if n == 0 else 4
                    o = (i * 2 + n) * 16
                    om = (i * 6 + vs) * 16
                    stt(Gt[:, o:o + 16], modT[:, om:om + 16], 1.0, vecs[:, VOFF["ng"] + o:VOFF["ng"] + o + 16],
                        ALU.add, ALU.mult, [Bmod, Bvecs], [BG])
        S.barrier()

        def rms_stat(xt, Bx, n, tmps, nchunks, inv_d):
            sqr, rst = tmps
            pb, pbb = bank()
            for j in range(nchunks):
                sq, sqb = sqr.next()
                act(sq[:, :n], xt(j), AF.Square, [Bx(j) if callable(Bx) else Bx], [sqb])
                mm(pb[:, :n], ones[:], sq[:, :n], j == 0, j == nchunks - 1, [Bones, sqb], pbb)
            r1, r1b = rst.next()
            act(r1[:, :n], pb[:, :n], AF.Sqrt, [pbb, Beps], [r1b], bias=epsc[:], scale=inv_d)
            r2, r2b = rst.next()
            recip(r2[:, :n], r1[:, :n], [r1b], [r2b])
            return r2, r2b

        def norm_mod(xblk, Bx, n, i, nn, col, hT, Bh, tmps, h32=None, Bh32=None):
            sqr, rst, tmr = tmps
            r, rb = rms_stat(lambda j: xblk[:, j, :n], Bx, n, (sqr, rst), 8, 1.0 / D)
            vsh = 0 if nn == 0 else 3
            for j in range(8):
                tm, tmb = tmr.next()
                stt(tm[:, :n], xblk[:, j, :n], Gs(i, nn, j, col), r[:, :n], ALU.mult, ALU.mult, [Bx, BG, rb], [tmb])
                if h32 is not None:
                    act(h32[:, j, :n], tm[:, :n], AF.Identity, [tmb, Bmod], [Bh32[j]], bias=mod(i, vsh, j, col), scale=1.0)
                    if hT is not None:
                        vcopy(hT[:, j, :n], h32[:, j, :n], [Bh32[j]], [Bh[j]], eng="gpsimd")
                else:
                    act(hT[:, j, :n], tm[:, :n], AF.Identity, [tmb, Bmod], [Bh[j]], bias=mod(i, vsh, j, col), scale=1.0)

        def load_w_bf(dst, dst_b, src_view, nparts=1, axis=1):
            S.dma("gpsimd", dst, src_view, writes=[dst_b])

        def phase_A():
            with ExitStack() as px:
                alloc = mk_alloc(px)
                win = alloc("win", [128, 8, 2048], BF16); Bwin = S.buf("win")
                wout = alloc("wout", [128, 8, 1024], BF16); Bwout = S.buf("wout")
                wsT = alloc("wsT", [128, 8, 128], BF16); BwsT = S.buf("wsT")
                lnv = alloc("lnv", [128, 2, 512]); Blnv = S.buf("lnv")
                bsb = alloc("bsb", [128, 4, 128]); Bbs = S.buf("bs")
                a_pad = alloc("a_pad", [128, 4, T + 30], BF16); Bap = S.bufs("ap", 4)
                xr = Rot(S, alloc, "xa", 2, [128, 8, 512], F32)
                hT = alloc("hT", [128, 8, 512], BF16); Bh = S.bufs("h", 8)
                uT = alloc("uT", [128, 4, 512], BF16); Bu = S.bufs("u", 4)
                vtm = alloc("vtm", [128, 4, 512], BF16); Bv = S.bufs("v", 4)
                ac = alloc("ac", [128, 4, 512]); Bac = S.bufs("ac", 4)
                a2T = alloc("a2T", [128, 4, 512], BF16); Ba2 = S.bufs("a2", 4)
                boT = alloc("boT", [128, 4, 512], BF16); Bbo = S.bufs("bo", 4)
                sqr = Rot(S, alloc, "sqa", 2, [128, 512], F32)
                rst = Rot(S, alloc, "rsa", 4, [128, 512], F32)
                tmr = Rot(S, alloc, "tma", 3, [128, 512], F32)
                diag = alloc("diag", [128, 4, 31, 128], BF16); Bdiag = S.buf("diag")
                for c in range(4):
                    for k in range(31):
                        ts(diag[:, c, k, :], ident[:], vv("convw", c * 31 + k), None, ALU.mult, None, [Bident, Bvecs, Bdiag], [Bdiag])
                smr = Rot(S, alloc, "sma", 8, [128, 8], F32)
                tmps = (sqr, rst, tmr)
                S.dma("gpsimd", win[:], e_w_in.rearrange("(k p) n -> p k n", p=128), writes=[Bwin])
                S.dma("gpsimd", wout[:], e_w_out.rearrange("(k p) n -> p k n", p=128), writes=[Bwout])
                S.dma("gpsimd", wsT[:], wsT_in, writes=[BwsT])
                S.dma("sync", lnv[:], lnv_in, writes=[Blnv])
                S.dma("sync", bsb[:], bs_in, writes=[Bbs])

                def seq(src, dst, dst_bufs, ntok, col):
                    nb = min(512, ntok)
                    nblk = ntok // nb
                    sv, dv = dview(src), dview(dst)
                    for c in range(4):
                        memset(a_pad[:, c, :], 0.0, [Bap[c]])
                    xq = {}

                    def xload(tb):
                        if tb < nblk:
                            xb_, xbb_ = xr.next()
                            S.dma("sync", xb_[:, :, :nb], sv[:, :, tb * nb:(tb + 1) * nb], writes=[xbb_])
                            xq[tb] = (xb_, xbb_)
                    xload(0)
                    for tb in range(nblk):
                        xload(tb + 1)
                        xb, xbb = xq.pop(tb)
                        norm_mod(xb, xbb, nb, 0, 0, col, hT, Bh, tmps)
                        for c in range(4):
                            pv, pvb = bank()
                            pg, pgb = bank()
                            for k in range(8):
                                mm(pv[:, :nb], win[:, k, c * 128:(c + 1) * 128], hT[:, k, :nb], k == 0, k == 7, [Bwin, Bh[k]], pvb)
                            for k in range(8):
                                mm(pg[:, :nb], win[:, k, 512 + c * 128:512 + (c + 1) * 128], hT[:, k, :nb], k == 0, k == 7,
                                   [Bwin, Bh[k]], pgb)
                            sg, sgb = tmr.next()
                            act(sg[:, :nb], pg[:, :nb], AF.Sigmoid, [pgb], [sgb])
                            tt(a_pad[:, c, 15 + tb * nb:15 + (tb + 1) * nb], pv[:, :nb], sg[:, :nb], ALU.mult, [pvb, sgb], [Bap[c]])
                    xload(0)
                    for tb in range(nblk):
                        xload(tb + 1)
                        xb, xbb = xq.pop(tb)
                        norm_mod(xb, xbb, nb, 0, 0, col, hT, Bh, tmps)
                        for c in range(4):
                            pu, pub = bank()
                            for k in range(8):
                                mm(pu[:, :nb], win[:, k, 1024 + c * 128:1024 + (c + 1) * 128], hT[:, k, :nb], k == 0, k == 7,
                                   [Bwin, Bh[k]], pub)
                            act(uT[:, c, :nb], pu[:, :nb], AF.Gelu_apprx_tanh, [pub], [Bu[c]])
                        for t4 in range(nb // 128):
                            pv, pvb = bank()
                            for k in range(8):
                                mm(pv[:, :], hT[:, k, t4 * 128:(t4 + 1) * 128], win[:, k, 1536:2048], k == 0, k == 7, [Bh[k], Bwin], pvb)
                            gv, gvb = tmr.next()
                            act(gv[:, :], pv[:, :], AF.Gelu_apprx_tanh, [pvb], [gvb])
                            st, stb = smr.next()
                            S.op("vector", lambda e, st=st, gv=gv: e.bn_stats(out=st[:, 0:6], in_=gv[:, :]), reads=[gvb], writes=[stb])
                            mv, mvb = smr.next()
                            S.op("vector", lambda e, st=st, mv=mv: e.bn_aggr(out=mv[:, 0:2], in_=st[:, 0:6]), reads=[stb], writes=[mvb])
                            sd, sdb = smr.next()
                            act(sd[:, 0:1], mv[:, 1:2], AF.Sqrt, [mvb, Beps], [sdb], bias=epsc[:], scale=1.0)
                            rs, rsb = smr.next()
                            recip(rs[:, 0:1], sd[:, 0:1], [sdb], [rsb])
                            g2, g2b = tmr.next()
                            ts(g2[:, :], gv[:, :], mv[:, 0:1], rs[:, 0:1], ALU.subtract, ALU.mult, [gvb, mvb, rsb], [g2b])
                            g3, g3b = tmr.next()
                            tt(g3[:, :], g2[:, :], lnv[:, 0, :], ALU.mult, [g2b, Blnv], [g3b], eng="gpsimd")
                            tt(vtm[:, t4, :], g3[:, :], lnv[:, 1, :], ALU.add, [g3b, Blnv], [Bv[t4]], eng="gpsimd")
                        for c in range(4):
                            base = tb * nb
                            pcv_, pcvb_ = bank()
                            for k in range(31):
                                mm(pcv_[:, :nb], diag[:, c, k, :], a_pad[:, c, base + k:base + k + nb], k == 0, k == 30, [Bdiag, Bap[c]], pcvb_)
                            act(ac[:, c, :nb], pcv_[:, :nb], AF.Identity, [pcvb_, Bvecs], [Bac[c]], bias=vv("convb", c), scale=1.0)
                        p1, p1b = bank()
                        p2, p2b = bank()
                        for c in range(4):
                            mm(p1[:, :nb], ones[:], ac[:, c, :nb], c == 0, c == 3, [Bones, Bac[c]], p1b)
                        for c in range(4):
                            sq, sqb = sqr.next()
                            act(sq[:, :nb], ac[:, c, :nb], AF.Square, [Bac[c]], [sqb])
                            mm(p2[:, :nb], ones[:], sq[:, :nb], c == 0, c == 3, [Bones, sqb], p2b)
                        mean, meanb = rst.next()
                        act(mean[:, :nb], p1[:, :nb], AF.Copy, [p1b], [meanb], scale=1.0 / 512)
                        msq, msqb = rst.next()
                        tt(msq[:, :nb], mean[:, :nb], mean[:, :nb], ALU.mult, [meanb], [msqb])
                        var, varb = rst.next()
                        stt(var[:, :nb], p2[:, :nb], 1.0 / 512, msq[:, :nb], ALU.mult, ALU.subtract, [p2b, msqb], [varb])
                        sd, sdb = tmr.next()
                        act(sd[:, :nb], var[:, :nb], AF.Sqrt, [varb, Beps], [sdb], bias=epsc[:], scale=1.0)
                        rs, rsb = rst.next()
                        recip(rs[:, :nb], sd[:, :nb], [sdb], [rsb])
                        for c in range(4):
                            y1, y1b = tmr.next()
                            tt(y1[:, :nb], ac[:, c, :nb], mean[:, :nb], ALU.subtract, [Bac[c], meanb], [y1b])
                            y2, y2b = tmr.next()
                            tt(y2[:, :nb], y1[:, :nb], rs[:, :nb], ALU.mult, [y1b, rsb], [y2b])
                            act(a2T[:, c, :nb], y2[:, :nb], AF.Silu, [y2b, Bvecs], [Ba2[c]], bias=vv("lnab", c), scale=vv("lnag", c))
                        for t4 in range(nb // 128):
                            ps_, psb = bank()
                            for jp in range(4):
                                for hh in range(2):
                                    h = 2 * jp + hh
                                    mm(ps_[hh * 64:(hh + 1) * 64, jp * 128:(jp + 1) * 128], vtm[:, t4, h * 64:(h + 1) * 64], wsT[:, h, :],
                                       True, True, [Bv[t4], BwsT], psb)
                            sv1, sv1b = tmr.next()
                            tt(sv1[:, :], ps_[:, :], bsb[:].rearrange("p a b -> p (a b)"), ALU.add, [psb, Bbs], [sv1b])
                            tt(boT[:, :, t4 * 128:(t4 + 1) * 128], sv1[:, :].rearrange("p (a b) -> p a b", a=4),
                               uT[:, :, t4 * 128:(t4 + 1) * 128], ALU.mult, [sv1b] + Bu, Bbo)
                        xn, xnb = xb, xbb
                        for dm in range(8):
                            po, pob = bank()
                            for c in range(4):
                                mm(po[:, :nb], wout[:, c, dm * 128:(dm + 1) * 128], a2T[:, c, :nb], c == 0, False, [Bwout, Ba2[c]], pob)
                            for c in range(4):
                                mm(po[:, :nb], wout[:, 4 + c, dm * 128:(dm + 1) * 128], boT[:, c, :nb], False, c == 3, [Bwout, Bbo[c]], pob)
                            stt(xn[:, dm, :nb], po[:, :nb], mod(0, 2, dm, col), xb[:, dm, :nb], ALU.mult, ALU.add,
                                [pob, Bmod, xbb], [xnb])
                        S.dma("sync", dv[:, :, tb * nb:(tb + 1) * nb], xn[:, :, :nb], reads=[xnb], writes=[dst_bufs[tb]])

                seq(cT_in, cs, Bcs, TC, 1)
                seq(xT_in, xs, Bxs, T, 0)

        def ffn_engine(alloc):
            st = {}
            st["w1"] = Rot(S, alloc, "w1g", 3, [128, 8, 512], BF16)
            st["w3"] = Rot(S, alloc, "w3g", 3, [128, 8, 512], BF16)
            st["w2"] = Rot(S, alloc, "w2g", 3, [128, 4, 1024], BF16)
            st["g"] = Rot(S, alloc, "gg", 8, [128, 512], BF16)
            st["s1"] = Rot(S, alloc, "s1", 3, [128, 512], F32)
            st["t2"] = Rot(S, alloc, "t2", 3, [128, 512], F32)
            return st

        def static_loader(w1, w3, w2):
            w1v = w1.rearrange("(k p) f -> p k f", p=128)
            w3v = w3.rearrange("(k p) f -> p k f", p=128)

            def load(f0, gs, w1t, w1b, w3t, w3b, w2t, w2b):
                S.dma("gpsimd", w1t[:, :, :gs * 128], w1v[:, :, f0 * 128:(f0 + gs) * 128], writes=[w1b])
                S.dma("gpsimd", w3t[:, :, :gs * 128], w3v[:, :, f0 * 128:(f0 + gs) * 128], writes=[w3b])
                S.dma("gpsimd", w2t[:, :gs, :], w2[f0 * 128:(f0 + gs) * 128, :].rearrange("(f p) d -> p f d", p=128), writes=[w2b])
            return load

        def ffn_run(st, hT, Bh, n, loaders, nchunks, gb, yacc, By, next_loader=None):
            groups = []
            f0 = 0
            while f0 < nchunks:
                gs = min(4, nchunks - f0)
                groups.append((f0, gs))
                f0 += gs
            items = [(ei, ld, f0, gs) for ei, ld in enumerate(loaders) for (f0, gs) in groups]
            nI = len(items)
            wt = {}

            def issue_load(i):
                ei, ld, f0, gs = items[i]
                w1t, w1b = st["w1"].next()
                w3t, w3b = st["w3"].next()
                w2t, w2b = st["w2"].next()
                ld(f0, gs, w1t, w1b, w3t, w3b, w2t, w2b)
                wt[i] = (w1t, w1b, w3t, w3b, w2t, w2b)

            def up(i):
                ei, ld, f0, gs = items[i]
                w1t, w1b, w3t, w3b, w2t, w2b = wt[i]
                gts = []
                for fi in range(gs):
                    pa, pab = bank()
                    pb_, pbb = bank()
                    for k in range(8):
                        mm(pa[:, :n], w1t[:, k, fi * 128:(fi + 1) * 128], hT[:, k, :n], k == 0, k == 7, [w1b, Bh[k]], pab)
                    for k in range(8):
                        mm(pb_[:, :n], w3t[:, k, fi * 128:(fi + 1) * 128], hT[:, k, :n], k == 0, k == 7, [w3b, Bh[k]], pbb)
                    s1, s1b = st["s1"].next()
                    act(s1[:, :n], pa[:, :n], AF.Silu, [pab], [s1b])
                    g, gbuf = st["g"].next()
                    if gb is None:
                        tt(g[:, :n], s1[:, :n], pb_[:, :n], ALU.mult, [s1b, pbb], [gbuf])
                    else:
                        t2, t2b = st["t2"].next()
                        tt(t2[:, :n], s1[:, :n], pb_[:, :n], ALU.mult, [s1b, pbb], [t2b])
                        gap, gapb = gb(ei)
                        tt(g[:, :n], t2[:, :n], gap, ALU.mult, [t2b, gapb], [gbuf], eng="gpsimd")
                    gts.append((g, gbuf))
                return gts

            def down(i, gts):
                ei, ld, f0, gs = items[i]
                w1t, w1b, w3t, w3b, w2t, w2b = wt.pop(i)
                for dm in range(8):
                    pd, pdb = bank()
                    for fi in range(gs):
                        mm(pd[:, :n], w2t[:, fi, dm * 128:(dm + 1) * 128], gts[fi][0][:, :n], fi == 0, fi == gs - 1,
                           [w2b, gts[fi][1]], pdb)
                    if i == 0:
                        act(yacc[:, dm, :n], pd[:, :n], AF.Copy, [pdb], [By[dm]])
                    else:
                        tt(yacc[:, dm, :n], yacc[:, dm, :n], pd[:, :n], ALU.add, [By[dm], pdb], [By[dm]])

            pre = st.pop("pre", None)
            if pre is not None:
                wt.update(pre)
            else:
                issue_load(0)
                issue_load(1)
            nxt = {}
            prev = None
            for i in range(nI + 1):
                cur = up(i) if i < nI else None
                if prev is not None:
                    down(i - 1, prev)
                if i + 2 < nI:
                    issue_load(i + 2)
                elif next_loader is not None and i + 2 - nI < 2:
                    kk = i + 2 - nI
                    f0n, gsn = groups[kk]
                    w1t, w1b = st["w1"].next()
                    w3t, w3b = st["w3"].next()
                    w2t, w2b = st["w2"].next()
                    next_loader(f0n, gsn, w1t, w1b, w3t, w3b, w2t, w2b)
                    nxt[kk] = (w1t, w1b, w3t, w3b, w2t, w2b)
                prev = cur
            if next_loader is not None:
                st["pre"] = nxt

        def phase_B():
            with ExitStack() as px:
                alloc = mk_alloc(px)
                st = ffn_engine(alloc)
                xr = Rot(S, alloc, "xb", 3, [128, 8, 512], F32)
                hTs = [alloc(f"hTb{i}", [128, 8, 512], BF16) for i in range(2)]
                Bhs_ = [S.bufs(f"hb{i}_", 8) for i in range(2)]
                yacc = alloc("yaccb", [128, 8, 512]); By = S.bufs("yb", 8)
                sqr = Rot(S, alloc, "sqb", 2, [128, 512], F32)
                rst = Rot(S, alloc, "rsb", 2, [128, 512], F32)
                tmr = Rot(S, alloc, "tmb", 3, [128, 512], F32)
                tmps = (sqr, rst, tmr)

                fl = static_loader(f_w1, f_w3, f_w2)
                blocks = [(cs, Bcs, 0, TC, 1)] + [(xs, Bxs, tb, 512, 0) for tb in range(8)]

                xl = {}

                def load(i):
                    if i >= len(blocks):
                        return
                    buf_ap, bufs, tb, nb, col = blocks[i]
                    v = dview(buf_ap)
                    xb, xbb = xr.next()
                    S.dma("sync", xb[:, :, :nb], v[:, :, tb * nb:(tb + 1) * nb], reads=[bufs[tb]], writes=[xbb])
                    xl[i] = (xb, xbb)

                def prep(i):
                    buf_ap, bufs, tb, nb, col = blocks[i]
                    xb, xbb = xl[i]
                    norm_mod(xb, xbb, nb, 0, 1, col, hTs[i % 2], Bhs_[i % 2], tmps)
                    return xb, xbb

                load(0)
                load(1)
                cur = prep(0)
                for i in range(len(blocks)):
                    buf_ap, bufs, tb, nb, col = blocks[i]
                    v = dview(buf_ap)
                    load(i + 2)
                    nxt = prep(i + 1) if i + 1 < len(blocks) else None
                    xb, xbb = cur
                    ffn_run(st, hTs[i % 2], Bhs_[i % 2], nb, [fl], 22, None, yacc, By, next_loader=fl if i + 1 < len(blocks) else None)
                    for dm in range(8):
                        stt(xb[:, dm, :nb], yacc[:, dm, :nb], mod(0, 5, dm, col), xb[:, dm, :nb], ALU.mult, ALU.add,
                            [By[dm], Bmod, xbb], [xbb])
                    S.dma("sync", v[:, :, tb * nb:(tb + 1) * nb], xb[:, :, :nb], reads=[xbb], writes=[bufs[tb]])
                    cur = nxt

        NK = TC + T
        SCALE = 192.0 ** -0.5

        def phase_C():
            with ExitStack() as px:
                alloc = mk_alloc(px)
                qn = alloc("qn", [128, 3, T], BF16); Bqn = S.bufs("qn", 8)
                kvn = alloc("kvn", [128, 2, NK], BF16); Bkvn = S.bufs("kvn", 9)
                kpe2 = alloc("kpe2", [128, 2, NK], BF16); Bkpe = S.bufs("kpe", 9); Bkz = S.buf("kpez")
                S.op("gpsimd", lambda e: e.memset(kpe2[:], 0.0), writes=[Bkz])
                qrp = alloc("qrp", [128, 4, T], BF16); Bqrp = S.bufs("qrp", 8)
                Batt = [S.bufs(f"att{h}_", 8) for h in range(8)]
                wuq = alloc("wuq", [128, 3, 1024], BF16); Bwuq = S.buf("wuq")
                wukv = alloc("wukv", [128, 2, 2048], BF16); Bwukv = S.buf("wukv")
                S.dma("gpsimd", wuq[:], o_wuq.rearrange("(k p) n -> p k n", p=128)[:, :, 0:1024], writes=[Bwuq])
                S.dma("gpsimd", wukv[:], o_wukv.rearrange("(k p) n -> p k n", p=128), writes=[Bwukv])
                with ExitStack() as p1:
                    al1 = mk_alloc(p1)
                    wuqr = al1("wuqr", [128, 3, 1024], BF16); Bwuqr = S.buf("wuqr")
                    S.dma("gpsimd", wuqr[:], o_wuq.rearrange("(k p) n -> p k n", p=128)[:, :, 1024:2048], writes=[Bwuqr])
                    wi = al1("wi", [128, 8, 896], BF16); Bwi = S.buf("wi")
                    S.dma("gpsimd", wi[:], o_w_in.rearrange("(k p) n -> p k n", p=128), writes=[Bwi])
                    xr = Rot(S, al1, "xc", 1, [128, 8, 512], F32)
                    hTs = [al1(f"hTc{i}", [128, 8, 512], BF16) for i in range(2)]
                    Bhs2 = [S.bufs(f"hc{i}_", 8) for i in range(2)]
                    zl = al1("zl", [128, 5, 512]); Bzl = S.bufs("zl", 5)
                    rp = Rot(S, al1, "rp", 1, [128, 2, 512], F32)
                    sqr = Rot(S, al1, "sqc", 2, [128, 512], F32)
                    rst = Rot(S, al1, "rsc", 4, [128, 512], F32)
                    tmr = Rot(S, al1, "tmc", 4, [128, 512], F32)
                    tmps = (sqr, rst, tmr)

                    lblocks = [(cs, Bcs, 0, TC, 1, 0, 0, False)] + [(xs, Bxs, tb, 512, 0, TC, 1, True) for tb in range(8)]

                    def lprep(i):
                        src, src_bufs, tb, nb, col, kbase, kb0, is_x = lblocks[i]
                        v = dview(src)
                        xb, xbb = xr.next()
                        S.dma("sync", xb[:, :, :nb], v[:, :, tb * nb:(tb + 1) * nb], reads=[src_bufs[tb]], writes=[xbb])
                        norm_mod(xb, xbb, nb, 1, 0, col, hTs[i % 2], Bhs2[i % 2], tmps)

                    lprep(0)
                    for li in range(len(lblocks)):
                        src, src_bufs, tb, nb, col, kbase, kb0, is_x = lblocks[li]
                        hT, Bh = hTs[li % 2], Bhs2[li % 2]
                        if li + 1 < len(lblocks):
                            lprep(li + 1)
                        if True:
                            kc0 = kbase + tb * nb
                            for c in (range(5) if is_x else range(3, 5)):
                                pz, pzb = bank()
                                for k in range(8):
                                    mm(pz[:, :nb], wi[:, k, c * 128:(c + 1) * 128], hT[:, k, :nb], k == 0, k == 7, [Bwi, Bh[k]], pzb)
                                act(zl[:, c, :nb], pz[:, :nb], AF.Copy, [pzb], [Bzl[c]])
                            if is_x:
                                r, rb = rms_stat(lambda j: zl[:, j, :nb], lambda j: Bzl[j], nb, (sqr, rst), 3, 1.0 / 384)
                                for c in range(3):
                                    stt(qn[:, c, tb * nb:(tb + 1) * nb], zl[:, c, :nb], vv("gq", c), r[:, :nb], ALU.mult, ALU.mult,
                                        [Bzl[c], Bvecs, rb], [Bqn[tb]])
                            r, rb = rms_stat(lambda j: zl[:, 3 + j, :nb], lambda j: Bzl[3 + j], nb, (sqr, rst), 2, 1.0 / 256)
                            for c in range(2):
                                stt(kvn[:, c, kc0:kc0 + nb], zl[:, 3 + c, :nb], vv("gkv", c), r[:, :nb], ALU.mult, ALU.mult,
                                    [Bzl[3 + c], Bvecs, rb], [Bkvn[kb0 + tb]])
                            pk, pkb = bank()
                            for k in range(8):
                                mm(pk[:, :nb], wi[:, k, 640:768], hT[:, k, :nb], k == 0, k == 7, [Bwi, Bh[k]], pkb)
                            if not is_x:
                                act(kpe2[0:64, 0, kc0:kc0 + nb], pk[0:64, :nb], AF.Copy, [pkb, Bkz], [Bkpe[kb0 + tb]])
                                vcopy(kpe2[64:128, 1, kc0:kc0 + nb], pk[64:128, :nb], [pkb, Bkz, Bkpe[kb0 + tb]], [Bkpe[kb0 + tb]])
                            else:
                                pw, pwb = bank()
                                for k in range(8):
                                    mm(pw[:, :nb], wi[:, k, 768:896], hT[:, k, :nb], k == 0, k == 7, [Bwi, Bh[k]], pwb)
                                rt, rtb = rp.next()
                                S.dma("sync", rt[:, :, :nb], rope_in[:, :, tb * nb:(tb + 1) * nb], writes=[rtb])
                                t1, t1b = tmr.next()
                                tt(t1[:, :nb], pk[:, :nb], rt[:, 0, :nb], ALU.mult, [pkb, rtb], [t1b])
                                t2, t2b = tmr.next()
                                tt(t2[:, :nb], pw[:, :nb], rt[:, 1, :nb], ALU.mult, [pwb, rtb], [t2b])
                                tt(kpe2[0:64, 0, kc0:kc0 + nb], t1[0:64, :nb], t2[0:64, :nb], ALU.add, [t1b, t2b, Bkz], [Bkpe[kb0 + tb]], eng="gpsimd")
                                tt(kpe2[64:128, 1, kc0:kc0 + nb], t1[64:128, :nb], t2[64:128, :nb], ALU.add, [t1b, t2b, Bkz, Bkpe[kb0 + tb]],
                                   [Bkpe[kb0 + tb]], eng="gpsimd")
                                for hp in range(4):
                                    pq, pqb = bank()
                                    pq2, pq2b = bank()
                                    for k in range(3):
                                        mm(pq[:, :nb], wuqr[:, k, hp * 128:(hp + 1) * 128], qn[:, k, tb * nb:(tb + 1) * nb],
                                           k == 0, k == 2, [Bwuqr, Bqn[tb]], pqb)
                                    for k in range(3):
                                        mm(pq2[:, :nb], wuqr[:, k, 512 + hp * 128:512 + (hp + 1) * 128], qn[:, k, tb * nb:(tb + 1) * nb],
                                           k == 0, k == 2, [Bwuqr, Bqn[tb]], pq2b)
                                    t1, t1b = tmr.next()
                                    tt(t1[:, :nb], pq[:, :nb], rt[:, 0, :nb], ALU.mult, [pqb, rtb], [t1b])
                                    t2, t2b = tmr.next()
                                    tt(t2[:, :nb], pq2[:, :nb], rt[:, 1, :nb], ALU.mult, [pq2b, rtb], [t2b])
                                    tt(qrp[:, hp, tb * nb:(tb + 1) * nb], t1[:, :nb], t2[:, :nb], ALU.add, [t1b, t2b], [Bqrp[tb]], eng="gpsimd")
                S.barrier()
                att = alloc("att", [128, 8, T], BF16)
                with ExitStack() as p2:
                    al2 = mk_alloc(p2)
                    KT = al2("KT", [128, NK], BF16); BKT = S.buf("KT")
                    Vh = al2("Vh", [128, 34, 128], BF16); BVh = S.buf("Vh")
                    Qh = al2("Qh", [128, T], BF16); BQh = S.buf("Qh")
                    pr = Rot(S, al2, "pT", 4, [128, 512], BF16)
                    rr = Rot(S, al2, "rr", 1, [128, 512], F32)
                    sb_banks = (0, 1, 2, 3)
                    acc_o = (4, 5)
                    acc_s = (6, 7)
                    allkv = Bkvn
                    for h in range(8):
                        hh, hp = h % 2, h // 2
                        for cb in range(0, NK, 512):
                            w = min(512, NK - cb)
                            pk, pkb = bank(sb_banks)
                            for k in range(2):
                                mm(pk[:, :w], wukv[:, k, h * 128:(h + 1) * 128], kvn[:, k, cb:cb + w], k == 0, k == 1, [Bwukv] + allkv, pkb)
                            act(KT[:, cb:cb + w], pk[:, :w], AF.Copy, [pkb], [BKT])
                        for kt0 in range(0, 34, 4):
                            nt = min(4, 34 - kt0)
                            pv, pvb = bank(sb_banks)
                            for i4 in range(nt):
                                kt = kt0 + i4
                                for k in range(2):
                                    mm(pv[:, i4 * 128:(i4 + 1) * 128], kvn[:, k, kt * 128:(kt + 1) * 128],
                                       wukv[:, k, 1024 + h * 128:1024 + (h + 1) * 128], k == 0, k == 1, [Bwukv] + allkv, pvb)
                            vcopy(Vh[:, kt0:kt0 + nt, :], pv[:, :nt * 128].rearrange("p (a b) -> p a b", a=nt), [pvb], [BVh])
                        for qb in range(8):
                            pq, pqb = bank(sb_banks)
                            for k in range(3):
                                mm(pq[:, :], wuq[:, k, h * 128:(h + 1) * 128], qn[:, k, qb * 512:(qb + 1) * 512], k == 0, k == 2,
                                   [Bwuq, Bqn[qb]], pqb)
                            act(Qh[:, qb * 512:(qb + 1) * 512], pq[:, :], AF.Copy, [pqb], [BQh])
                        for qb in range(8):
                            po, pob = bank(acc_o)
                            pS, pSb = bank(acc_s)
                            LAG = 2
                            pts = {}
                            for kt in range(34 + LAG):
                                if kt < 34:
                                    ps_, psb = bank(sb_banks)
                                    mm(ps_[:, :], KT[:, kt * 128:(kt + 1) * 128], Qh[:, qb * 512:(qb + 1) * 512], True, False, [BKT, BQh], psb)
                                    mm(ps_[:, :], kpe2[:, hh, kt * 128:(kt + 1) * 128],
                                       qrp[:, hp, qb * 512:(qb + 1) * 512], False, True, Bkpe + [Bqrp[qb]], psb)
                                    pT, pTb = pr.next()
                                    act(pT[:, :], ps_[:, :], AF.Exp, [psb], [pTb], scale=SCALE)
                                    pts[kt] = (pT, pTb)
                                if kt >= LAG:
                                    k2 = kt - LAG
                                    pT, pTb = pts.pop(k2)
                                    mm(po[:, :], Vh[:, k2, :], pT[:, :], k2 == 0, k2 == 33, [BVh, pTb], pob)
                                    mm(pS[:, :], onesb[:], pT[:, :], k2 == 0, k2 == 33, [Bonesb, pTb], pSb)
                            rc, rcb = rr.next()
                            recip(rc[:, :], pS[:, :], [pSb], [rcb])
                            tt(att[:, h, qb * 512:(qb + 1) * 512], po[:, :], rc[:, :], ALU.mult, [pob, rcb], [Batt[h][qb]])
                S.barrier()
                with ExitStack() as p3:
                    al3 = mk_alloc(p3)
                    wo = al3("wo", [128, 8, 1024], BF16); Bwo = S.buf("wo")
                    S.dma("gpsimd", wo[:], o_w_o.rearrange("(k p) n -> p k n", p=128), writes=[Bwo])
                    xr = Rot(S, al3, "xc3", 1, [128, 8, 512], F32)
                    v = dview(xs)
                    for tb in range(8):
                        xb, xbb = xr.next()
                        S.dma("sync", xb[:], v[:, :, tb * 512:(tb + 1) * 512], reads=[Bxs[tb]], writes=[xbb])
                        xn, xnb = xb, xbb
                        for dm in range(8):
                            po, pob = bank()
                            for h in range(8):
                                mm(po[:, :], wo[:, h, dm * 128:(dm + 1) * 128], att[:, h, tb * 512:(tb + 1) * 512], h == 0, h == 7,
                                   [Bwo, Batt[h][tb]], pob)
                            stt(xn[:, dm, :], po[:, :], mod(1, 2, dm, 0), xb[:, dm, :], ALU.mult, ALU.add, [pob, Bmod, xbb], [xnb])
                        S.dma("sync", v[:, :, tb * 512:(tb + 1) * 512], xn[:], reads=[xnb], writes=[Bxs[tb]])

        def phase_D():
            with ExitStack() as px:
                alloc = mk_alloc(px)
                st = ffn_engine(alloc)
                rt32 = alloc("rt32", [128, 8, 8]); Brt = S.buf("rt32")
                selt = alloc("selt", [8, 8, 128]); Bsel = S.buf("sel")
                S.dma("sync", rt32[:], o_router.rearrange("(k p) e -> p k e", p=128), writes=[Brt])
                S.dma("sync", selt[:], sel_in, writes=[Bsel])
                xr = Rot(S, alloc, "xd", 2, [128, 8, 512], F32)
                hT = alloc("hTd", [128, 8, 512], BF16); Bh = S.bufs("hd", 8)
                h32 = alloc("h32", [128, 8, 512]); Bh32 = S.bufs("h32_", 8)
                yacc = alloc("yaccd", [128, 8, 512]); By = S.bufs("yd", 8)
                gbt = alloc("gbt", [128, 8, 512]); Bgb = S.bufs("gb", 8)
                gT = alloc("gT", [8, 512]); BgT = S.buf("gT")
                sqr = Rot(S, alloc, "sqd", 2, [128, 512], F32)
                rst = Rot(S, alloc, "rsd", 2, [128, 512], F32)
                tmr = Rot(S, alloc, "tmd", 3, [128, 512], F32)
                sm = Rot(S, alloc, "smd", 12, [128, 8], F32)
                tmps = (sqr, rst, tmr)
                v = dview(xs)
                ov = dview(outT)
                experts = []
                for tb in range(8):
                    xb, xbb = xr.next()
                    S.dma("sync", xb[:], v[:, :, tb * 512:(tb + 1) * 512], reads=[Bxs[tb]], writes=[xbb])
                    norm_mod(xb, xbb, 512, 1, 1, 0, hT, Bh, tmps, h32=h32, Bh32=Bh32)
                    for t4 in range(4):
                        pl, plb = bank()
                        for k in range(8):
                            mm(pl[:, 0:8], h32[:, k, t4 * 128:(t4 + 1) * 128], rt32[:, k, :], k == 0, k == 7, [Bh32[k], Brt], plb)
                        lg, lgb = sm.next()
                        vcopy(lg[:, :], pl[:, 0:8], [plb], [lgb])
                        mx8, mxb = sm.next()
                        S.op("vector", lambda e, mx8=mx8, lg=lg: e.max(out=mx8[:, :], in_=lg[:, :]), reads=[lgb], writes=[mxb])
                        nm, nmb = sm.next()
                        ts(nm[:, 0:1], mx8[:, 0:1], -1.0, None, ALU.mult, None, [mxb], [nmb])
                        ex, exb = sm.next()
                        act(ex[:, :], lg[:, :], AF.Exp, [lgb, nmb], [exb], bias=nm[:, 0:1], scale=1.0)
                        mk, mkb = sm.next()
                        ts(mk[:, :], lg[:, :], mx8[:, 1:2], None, ALU.is_ge, None, [lgb, mxb], [mkb])
                        me, meb = sm.next()
                        tt(me[:, :], mk[:, :], ex[:, :], ALU.mult, [mkb, exb], [meb])
                        dn, dnb = sm.next()
                        S.op("vector", lambda e, dn=dn, me=me: e.tensor_reduce(out=dn[:, 0:1], in_=me[:, :], axis=mybir.AxisListType.X, op=ALU.add),
                             reads=[meb], writes=[dnb])
                        rd, rdb = sm.next()
                        recip(rd[:, 0:1], dn[:, 0:1], [dnb], [rdb])
                        gt, gtb = sm.next()
                        ts(gt[:, :], me[:, :], rd[:, 0:1], None, ALU.mult, None, [meb, rdb], [gtb])
                        ptr, ptrb = bank()
                        S.op("tensor", lambda e, ptr=ptr, gt=gt: e.transpose(out=ptr[0:8, 0:128], in_=gt[:, :], identity=ident[:]),
                             reads=[gtb, Bident], writes=[ptrb])
                        vcopy(gT[:, t4 * 128:(t4 + 1) * 128], ptr[0:8, 0:128], [ptrb], [BgT])
                    for e_ in range(8):
                        pg, pgb = bank()
                        mm(pg[:, :], selt[:, e_, :], gT[:, :], True, True, [Bsel, BgT], pgb)
                        act(gbt[:, e_, :], pg[:, :], AF.Copy, [pgb], [Bgb[e_]])
                    ffn_run(st, hT, Bh, 512, experts, 28, lambda ei: (gbt[:, ei, :], Bgb[ei]), yacc, By)
                    xn, xnb = xb, xbb
                    for dm in range(8):
                        stt(xn[:, dm, :], yacc[:, dm, :], mod(1, 5, dm, 0), xb[:, dm, :], ALU.mult, ALU.add, [By[dm], Bmod, xbb], [xnb])
                    r, rb = rms_stat(lambda j: xn[:, j, :], xnb, 512, (sqr, rst), 8, 1.0 / D)
                    for dm in range(8):
                        stt(yacc[:, dm, :], xn[:, dm, :], vv("fg", dm), r[:, :], ALU.mult, ALU.mult, [xnb, Bvecs, rb], [By[dm]])
                    S.dma("sync", ov[:, :, tb * 512:(tb + 1) * 512], yacc[:], reads=By, writes=[Bout[tb]])


        I32 = mybir.dt.int32
        JT = 24

        def phase_D2():
            with ExitStack() as px:
                alloc = mk_alloc(px)
                rt32 = alloc("rt32", [128, 8, 8]); Brt = S.buf("rt32")
                utri = alloc("utri", [128, 128]); Butri = S.buf("utri")
                ibase = alloc("ibase", [128, 7]); Bib = S.buf("ibase")
                identb = alloc("identb", [128, 128], BF16); Bidb = S.buf("identb")
                S.dma("sync", rt32[:], o_router.rearrange("(k p) e -> p k e", p=128), writes=[Brt])
                S.dma("sync", utri[:], utri_in, writes=[Butri])
                S.dma("sync", ibase[:], ibase_in, writes=[Bib])
                vcopy(identb[:], ident[:], [Bident], [Bidb])
                M_all = alloc("M_all", [128, 32, 8]); BM = S.bufs("Mall", 32)
                G_all = alloc("G_all", [128, 32, 8]); BGa = S.bufs("Gall", 32)
                R_all = alloc("R_all", [128, 32, 8]); BR = S.buf("Rall")
                P12f = alloc("P12f", [128, 64]); BP12f = S.bufs("P12f", 32)
                P12i = alloc("P12i", [128, 64], I32); BP12i = S.bufs("P12i", 32)
                G12 = alloc("G12", [128, 64]); BG12 = S.bufs("G12", 32)
                idxf = alloc("idxf", [128, JT * 7]); Bidxf = S.buf("idxf")
                idxi = alloc("idxi", [128, JT * 7], I32); Bidxi = S.buf("idxi")
                Bhz = S.buf("hz"); Bhs = S.buf("hs"); Bys = S.buf("ys")
                Bs2z = S.buf("s2z"); Bs2t = S.buf("s2t"); Bytok = S.buf("ytok")
                tokid = alloc("tokid", [128, 64, 16], I32); Btokid = S.buf("tokid")
                S.dma("sync", tokid[:], tokid_in, writes=[Btokid])
                with ExitStack() as p1:
                    al1 = mk_alloc(p1)
                    h_tm = al1("h_tm", [128, 32, 1024], BF16); Bhtm = S.bufs("htm", 32)
                    zt = al1("zt", [128, 8, 1024], BF16); Bzt = S.buf("zt")
                    S.op("gpsimd", lambda e: e.memset(zt[:], 0.0), writes=[Bzt])
                    for r in range(NS // 1024):
                        S.dma("sync", hsort[r * 1024:(r + 1) * 1024, :].rearrange("(r p) d -> p r d", p=128), zt[:],
                              reads=[Bzt], writes=[Bhz], join=True)
                    oobt = al1("oobt", [128, 96 * 16], I32); Boob = S.buf("oobt")
                    S.dma("sync", oobt[:], oobfill_in, writes=[Boob])
                    S.dma("sync", slot2tok.rearrange("(p r) o -> p (r o)", p=128), oobt[:], reads=[Boob], writes=[Bs2z])
                    xr = Rot(S, al1, "xd", 2, [128, 8, 512], F32)
                    h32s = [al1(f"h32_{i}", [128, 8, 512]) for i in range(2)]
                    Bh32s = [S.bufs(f"h32_{i}_", 8) for i in range(2)]
                    sqr = Rot(S, al1, "sqd", 2, [128, 512], F32)
                    rst = Rot(S, al1, "rsd", 2, [128, 512], F32)
                    tmr = Rot(S, al1, "tmd", 3, [128, 512], F32)
                    sm = Rot(S, al1, "smd", 48, [128, 8], F32)
                    tmps = (sqr, rst, tmr)
                    v = dview(xs)
                    for tb in range(8):
                        xb, xbb = xr.next()
                        S.dma("sync", xb[:], v[:, :, tb * 512:(tb + 1) * 512], reads=[Bxs[tb]], writes=[xbb])
                        h32, Bh32 = h32s[tb % 2], Bh32s[tb % 2]
                        norm_mod(xb, xbb, 512, 1, 1, 0, None, None, tmps, h32=h32, Bh32=Bh32)
                        for t4 in range(4):
                            c = tb * 4 + t4
                            pl, plb = bank()
                            for k in range(8):
                                mm(pl[:, 0:8], h32[:, k, t4 * 128:(t4 + 1) * 128], rt32[:, k, :], k == 0, k == 7, [Bh32[k], Brt], plb)
                            lg, lgb = sm.next()
                            vcopy(lg[:, :], pl[:, 0:8], [plb], [lgb])
                            mx8, mxb = sm.next()
                            S.op("vector", lambda e, mx8=mx8, lg=lg: e.max(out=mx8[:, :], in_=lg[:, :]), reads=[lgb], writes=[mxb])
                            nm, nmb = sm.next()
                            ts(nm[:, 0:1], mx8[:, 0:1], -1.0, None, ALU.mult, None, [mxb], [nmb])
                            ex, exb = sm.next()
                            act(ex[:, :], lg[:, :], AF.Exp, [lgb, nmb], [exb], bias=nm[:, 0:1], scale=1.0)
                            ts(M_all[:, c, :], lg[:, :], mx8[:, 1:2], None, ALU.is_ge, None, [lgb, mxb], [BM[c]])
                            me, meb = sm.next()
                            tt(me[:, :], M_all[:, c, :], ex[:, :], ALU.mult, [BM[c], exb], [meb])
                            dn, dnb = sm.next()
                            S.op("vector", lambda e, dn=dn, me=me: e.tensor_reduce(out=dn[:, 0:1], in_=me[:, :], axis=mybir.AxisListType.X, op=ALU.add),
                                 reads=[meb], writes=[dnb])
                            rd, rdb = sm.next()
                            recip(rd[:, 0:1], dn[:, 0:1], [dnb], [rdb])
                            ts(G_all[:, c, :], me[:, :], rd[:, 0:1], None, ALU.mult, None, [meb, rdb], [BGa[c]])
                            for half in range(2):
                                pt, ptb = bank()
                                for q in range(4):
                                    dk = half * 4 + q
                                    S.op("tensor", lambda e, pt=pt, q=q, dk=dk, t4=t4, h32=h32: e.transpose(
                                        out=pt[:, q * 128:(q + 1) * 128], in_=h32[:, dk, t4 * 128:(t4 + 1) * 128], identity=ident[:]),
                                        reads=[Bh32[dk], Bident], writes=[ptb])
                                if half == 0:
                                    act(h_tm[:, c, 0:512], pt[:, :], AF.Copy, [ptb], [Bhtm[c]])
                                else:
                                    vcopy(h_tm[:, c, 512:1024], pt[:, :], [ptb, Bhtm[c]], [Bhtm[c]])
                    sm2 = Rot(S, al1, "sm2", 24, [128, 8], F32)
                    pc_, pcb = bank()
                    for c in range(32):
                        mm(pc_[:, 0:8], ones[:], M_all[:, c, :], c == 0, c == 31, [Bones, BM[c]], pcb)
                    keep = Rot(S, al1, "keep", 5, [128, 8], F32)
                    cnt, cntb = keep.next()
                    vcopy(cnt[:, :], pc_[:, 0:8], [pcb], [cntb])
                    prk, prkb = bank()
                    for c in range(32):
                        mm(prk[:, c * 8:(c + 1) * 8], utri[:], M_all[:, c, :], True, c == 0, [Butri, BM[c]], prkb)
                        for c2 in range(c):
                            mm(prk[:, c * 8:(c + 1) * 8], ones[:], M_all[:, c2, :], False, c2 == c - 1, [Bones, BM[c2]], prkb)
                    vcopy(R_all[:].rearrange("p a b -> p (a b)"), prk[:, 0:256], [prkb], [BR])
                    tl, tlb = keep.next()
                    ts(tl[:, :], cnt[:, :], 0.0, None, ALU.is_gt, None, [cntb], [tlb])
                    for m in range(1, 8):
                        stt(tl[:, :], cnt[:, :], 512.0 * m, tl[:, :], ALU.is_gt, ALU.add, [cntb, tlb], [tlb])
                    pcv, pcvb = keep.next()
                    ts(pcv[:, :], tl[:, :], 512.0, None, ALU.mult, None, [tlb], [pcvb])
                    off, offb = keep.next()
                    memset(off[:, 0:1], 0.0, [offb])
                    for e_ in range(1, 8):
                        tt(off[:, e_:e_ + 1], off[:, e_ - 1:e_], pcv[:, e_ - 1:e_], ALU.add, [offb, pcvb], [offb])
                    endv, endb = keep.next()
                    tt(endv[:, :], off[:, :], pcv[:, :], ALU.add, [offb, pcvb], [endb])
                    ej = al1("ej", [128, JT]); Bej = S.buf("ej")
                    for j in range(JT):
                        cm, cmb = sm2.next()
                        ts(cm[:, :], endv[:, :], float(j * 512), None, ALU.is_le, None, [endb], [cmb])
                        S.op("vector", lambda e, cm=cm, j=j: e.tensor_reduce(out=ej[:, j:j + 1], in_=cm[:, :], axis=mybir.AxisListType.X, op=ALU.add),
                             reads=[cmb], writes=[Bej])
                    ej2 = al1("ej2", [128, JT]); Bej2 = S.buf("ej2")
                    ts(ej2[:, :], ej[:, :], 7.0, 1792.0, ALU.min, ALU.mult, [Bej], [Bej2])
                    for j in range(JT):
                        ts(idxf[:, j * 7:(j + 1) * 7], ibase[:, :], ej2[:, j:j + 1], None, ALU.add, None, [Bib, Bej2, Bidxf], [Bidxf])
                    vcopy(idxi[:, :], idxf[:, :], [Bidxf], [Bidxi])
                    for c in range(32):
                        a1, a1b = sm2.next()
                        stt(a1[:, :], R_all[:, c, :], 1.0, off[:, :], ALU.add, ALU.add, [BR, offb], [a1b])
                        a2, a2b = sm2.next()
                        tt(a2[:, :], a1[:, :], M_all[:, c, :], ALU.mult, [a1b, BM[c]], [a2b])
                        pm, pmb = sm2.next()
                        ts(pm[:, :], a2[:, :], -1.0, None, ALU.add, None, [a2b], [pmb])
                        mx, mxb = sm2.next()
                        S.op("vector", lambda e, mx=mx, pm=pm: e.max(out=mx[:, :], in_=pm[:, :]), reads=[pmb], writes=[mxb])
                        vcopy(P12f[:, c * 2:c * 2 + 2], mx[:, 0:2], [mxb], [BP12f[c]])
                        for k2 in range(2):
                            eq, eqb = sm2.next()
                            ts(eq[:, :], pm[:, :], mx[:, k2:k2 + 1], None, ALU.is_equal, None, [pmb, mxb], [eqb])
                            eg, egb = sm2.next()
                            tt(eg[:, :], eq[:, :], G_all[:, c, :], ALU.mult, [eqb, BGa[c]], [egb])
                            S.op("vector", lambda e, eg=eg, c=c, k2=k2: e.tensor_reduce(out=G12[:, c * 2 + k2:c * 2 + k2 + 1], in_=eg[:, :],
                                                                                         axis=mybir.AxisListType.X, op=ALU.add),
                                 reads=[egb, BG12[c]], writes=[BG12[c]])
                        vcopy(P12i[:, c * 2:c * 2 + 2], P12f[:, c * 2:c * 2 + 2], [BP12f[c]], [BP12i[c]])
                        for k2 in range(2):
                            def sc(e, c=c, k2=k2):
                                return e.indirect_dma_start(out=hsort, out_offset=bass.IndirectOffsetOnAxis(ap=P12i[:, c * 2 + k2:c * 2 + k2 + 1], axis=0),
                                                            in_=h_tm[:, c, :], in_offset=None)
                            S.dma_fn("gpsimd", sc, reads=[BP12i[c], Bhtm[c], Bhz], writes=[Bhs], join=True)

                            def sci(e, c=c, k2=k2):
                                return e.indirect_dma_start(out=slot2tok, out_offset=bass.IndirectOffsetOnAxis(ap=P12i[:, c * 2 + k2:c * 2 + k2 + 1], axis=0),
                                                            in_=tokid[:, c * 2 + k2, :], in_offset=None)
                            S.dma_fn("gpsimd", sci, reads=[BP12i[c], Btokid, Bs2z], writes=[Bs2t], join=True)
                    dbg_dump("cnt", cnt[:, :], [128, 8], [cntb])
                    dbg_dump("P12f", P12f[:, :], [128, 64], BP12f)
                    dbg_dump("G12", G12[:, :], [128, 64], BG12)
                    dbg_dump("ej", ej[:, :], [128, JT], [Bej])
                    dbg_dump("off", off[:, :], [128, 8], [offb])
                    dbg_dump("idxf", idxf[:, :], [128, JT * 7], [Bidxf])
                    dbg_dump("M_all", M_all[:].rearrange("p a b -> p (a b)"), [128, 256], BM)
                    dbg_dump("R_all", R_all[:].rearrange("p a b -> p (a b)"), [128, 256], [BR])
                    dbg_dump("h_tm", h_tm[:].rearrange("p a b -> p (a b)"), [128, 32 * 1024], Bhtm, dt=BF16)
                S.barrier()
                with ExitStack() as p4:
                    al4 = mk_alloc(p4)
                    st = ffn_engine(al4)
                    hsr = Rot(S, al4, "hs_tm", 2, [128, 4, 1024], BF16)
                    hsTr = [al4(f"hsT{i}", [128, 8, 512], BF16) for i in range(2)]
                    BhsT = [S.bufs(f"hsT{i}_", 8) for i in range(2)]
                    yaccr = [al4(f"yaccs{i}", [128, 8, 512]) for i in range(2)]
                    Byr = [S.bufs(f"ys{i}_", 8) for i in range(2)]
                    ysr = Rot(S, al4, "ys_tm", 2, [128, 4, 1024], F32)
                    idr = Rot(S, al4, "idt", 2, [128, 64], I32)
                    Bscat = S.bufs("scat", 2)

                    def dyn_loader(j):
                        def load(f0, gs, w1t, w1b, w3t, w3b, w2t, w2b):
                            fg = f0 // 4
                            col = j * 7 + fg
                            for (tab, wt_, wb_) in ((x_w1, w1t, w1b), (x_w3, w3t, w3b), (x_w2, w2t, w2b)):
                                flat = wt_[:].rearrange("p a b -> p (a b)")
                                for hf in range(2):
                                    def g(e, tab=tab, flat=flat, hf=hf, col=col):
                                        return e.indirect_dma_start(out=flat[:, hf * 2048:(hf + 1) * 2048], out_offset=None, in_=tab,
                                                                    in_offset=bass.IndirectOffsetOnAxis(ap=idxi[:, col:col + 1], axis=0),
                                                                    element_offset=hf * 2048)
                                    S.dma_fn("gpsimd", g, reads=[Bidxi], writes=[wb_], join=(hf == 1))
                        return load

                    def prologue(j):
                        hs_t, hs_b = hsr.next()
                        S.dma("sync", hs_t[:], hsort[j * 512:(j + 1) * 512, :].rearrange("(r p) d -> p r d", p=128), reads=[Bhz, Bhs], writes=[hs_b])
                        hsT, Bh_ = hsTr[j % 2], BhsT[j % 2]
                        for dk in range(8):
                            pt, ptb = bank()
                            for r in range(4):
                                mm(pt[:, r * 128:(r + 1) * 128], hs_t[:, r, dk * 128:(dk + 1) * 128], identb[:], True, True, [hs_b, Bidb], ptb)
                            if dk % 2 == 0:
                                act(hsT[:, dk, :], pt[:, :], AF.Copy, [ptb], [Bh_[dk]])
                            else:
                                vcopy(hsT[:, dk, :], pt[:, :], [ptb], [Bh_[dk]])

                    def epilogue(j):
                        yacc, By = yaccr[j % 2], Byr[j % 2]
                        ys_t, ys_b = ysr.next()
                        for r in range(4):
                            for half in range(2):
                                pt, ptb = bank()
                                for q in range(4):
                                    dm = half * 4 + q
                                    S.op("tensor", lambda e, pt=pt, q=q, dm=dm, r=r, yacc=yacc: e.transpose(
                                        out=pt[:, q * 128:(q + 1) * 128], in_=yacc[:, dm, r * 128:(r + 1) * 128], identity=ident[:]),
                                        reads=[By[dm], Bident], writes=[ptb])
                                if half == 0:
                                    act(ys_t[:, r, 0:512], pt[:, :], AF.Copy, [ptb, ys_b], [ys_b])
                                else:
                                    vcopy(ys_t[:, r, 512:1024], pt[:, :], [ptb, ys_b], [ys_b])
                        idt, idb = idr.next()
                        S.dma("sync", idt[:].rearrange("p (r o) -> p r o", r=4), slot2tok[j * 512:(j + 1) * 512, :].rearrange("(r p) o -> p r o", p=128), reads=[Bs2z, Bs2t], writes=[idb])
                        for r in range(4):
                            def scy(e, r=r, idt=idt, ys_t=ys_t):
                                return e.indirect_dma_start(out=ytok, out_offset=bass.IndirectOffsetOnAxis(ap=idt[:, r * 16:r * 16 + 1], axis=0),
                                                            in_=ys_t[:, r, :], in_offset=None)
                            S.dma_fn("gpsimd", scy, reads=[idb, ys_b], writes=[Bscat[j % 2]], join=True)

                    prologue(0)
                    for j in range(JT):
                        if j + 1 < JT:
                            prologue(j + 1)
                        ffn_run(st, hsTr[j % 2], BhsT[j % 2], 512, [dyn_loader(j)], 28, None, yaccr[j % 2], Byr[j % 2],
                                next_loader=dyn_loader(j + 1) if j + 1 < JT else None)
                        if j >= 1:
                            epilogue(j - 1)
                    epilogue(JT - 1)
                S.barrier()
                if DEBUG:
                    tq = nc.dram_tensor("dbg_hsort", [NS, D], BF16, kind="ExternalOutput").ap()
                    S.dma("sync", tq, hsort, reads=[Bhz, Bhs]); DBG_NAMES.append("dbg_hsort")
                    tq2 = nc.dram_tensor("dbg_ysort", [NS, D], F32, kind="ExternalOutput").ap()
                    S.dma("sync", tq2, ysort, reads=[Bys]); DBG_NAMES.append("dbg_ysort")
                with ExitStack() as p5:
                    al5 = mk_alloc(p5)
                    xr = Rot(S, al5, "xd5", 2, [128, 8, 512], F32)
                    y12r = Rot(S, al5, "y12", 8, [128, 2, 1024], F32)
                    ytr = Rot(S, al5, "yt", 3, [128, 1024], F32)
                    oo = Rot(S, al5, "oo", 2, [128, 8, 512], F32)
                    sqr = Rot(S, al5, "sq5", 2, [128, 512], F32)
                    rst = Rot(S, al5, "rs5", 2, [128, 512], F32)
                    v = dview(xs)
                    ov = dview(outT)
                    pre5 = {}

                    def prefetch5(tb):
                        if tb >= 8:
                            return
                        xb_, xbb_ = xr.next()
                        S.dma("sync", xb_[:], v[:, :, tb * 512:(tb + 1) * 512], reads=[Bxs[tb]], writes=[xbb_])
                        ys_ = []
                        for t4 in range(4):
                            c = tb * 4 + t4
                            y12_, y12b_ = y12r.next()
                            S.dma("sync", y12_[:], ytok[0:2 * T, :].rearrange("(t k) d -> t k d", k=2)[c * 128:(c + 1) * 128], reads=[Bytok], writes=[y12b_])
                            ys_.append((y12_, y12b_))
                        pre5[tb] = (xb_, xbb_, ys_)

                    prefetch5(0)
                    for tb in range(8):
                        prefetch5(tb + 1)
                        xb, xbb, ys_ = pre5.pop(tb)
                        for t4 in range(4):
                            c = tb * 4 + t4
                            y12, y12b = ys_[t4]
                            y1, y1b, y2, y2b = y12[:, 0, :], y12b, y12[:, 1, :], y12b
                            yt, ytb = ytr.next()
                            ts(yt[:, :], y1, G12[:, c * 2:c * 2 + 1], None, ALU.mult, None, [y1b, BG12[c]], [ytb])
                            stt(yt[:, :], y2, G12[:, c * 2 + 1:c * 2 + 2], yt[:, :], ALU.mult, ALU.add, [y2b, BG12[c], ytb], [ytb])
                            for dm in range(8):
                                S.op("tensor", lambda e, dm=dm, t4=t4, yt=yt: e.transpose(
                                    out=banks[dm][:, t4 * 128:(t4 + 1) * 128], in_=yt[:, dm * 128:(dm + 1) * 128], identity=ident[:]),
                                    reads=[ytb, Bident], writes=[bank_b[dm]])
                        for dm in range(8):
                            stt(xb[:, dm, :], banks[dm][:, :], mod(1, 5, dm, 0), xb[:, dm, :], ALU.mult, ALU.add, [bank_b[dm], Bmod, xbb], [xbb])
                        r, rb = rms_stat(lambda j: xb[:, j, :], xbb, 512, (sqr, rst), 8, 1.0 / D)
                        ot, otb = oo.next()
                        for dm in range(8):
                            stt(ot[:, dm, :], xb[:, dm, :], vv("fg", dm), r[:, :], ALU.mult, ALU.mult, [xbb, Bvecs, rb], [otb])
                        S.dma("sync", ov[:, :, tb * 512:(tb + 1) * 512], ot[:], reads=[otb], writes=[Bout[tb]])

        Bxs = S.bufs("xs", 8)
        Bcs = S.bufs("cs", 1)
        Bout = S.bufs("out", 8)
        phases = [("A", phase_A), ("B", phase_B), ("C", phase_C), ("D", phase_D2)]
        for name, fn in phases:
            fn()
            S.barrier()
            if name == stop_after:
                break
        if stop_after != "D":
            for tb in range(8):
                S.dma("sync", outT[:, tb * 512:(tb + 1) * 512], xs[:, tb * 512:(tb + 1) * 512], reads=[Bxs[tb]], writes=[Bout[tb]])
        S.barrier()
        S.emit()
        print(f"[build] insts={S.n_inst} waits={S.n_wait} sems={S.nsem}", flush=True)
    return nc


def _rope_tables():
    rows = T // 64
    row = np.repeat(np.arange(rows), 64).astype(np.float32)
    col = np.tile(np.arange(64), rows).astype(np.float32)
    inv = (10000.0 ** (-np.arange(16, dtype=np.float32) / 16)).astype(np.float32)
    ang = np.stack([row[:, None] * inv, col[:, None] * inv], axis=1).astype(np.float32)
    cos, sin = np.cos(ang).astype(np.float32), np.sin(ang).astype(np.float32)
    C = np.zeros((64, T), np.float32)
    Sg = np.zeros((64, T), np.float32)
    for p in range(64):
        ax, half, f = p // 32, (p // 16) % 2, p % 16
        C[p] = cos[:, ax, f]
        Sg[p] = sin[:, ax, f] * (-1.0 if half == 0 else 1.0)
    tab = np.stack([np.concatenate([C, C], 0), np.concatenate([Sg, Sg], 0)], axis=1)
    return np.ascontiguousarray(tab)


def _prep_shared(inp):
    f = lambda a: np.ascontiguousarray(np.asarray(a, dtype=np.float32))
    sh = {}
    sh["ident"] = np.eye(128, dtype=np.float32)
    sh["lnv"] = f(np.stack([np.broadcast_to(inp["e_ln_v_g"][0], (128, 512)), np.broadcast_to(inp["e_ln_v_b"][0], (128, 512))], axis=1))
    bs = inp["e_b_s"][0]
    sh["bs"] = f(np.repeat(bs.reshape(4, 2, 1, 128), 64, axis=2).reshape(4, 128, 128).transpose(1, 0, 2))
    sh["wsT"] = f(inp["e_w_s"][0].transpose(2, 0, 1))
    sh["rope"] = _rope_tables()
    sel = np.zeros((8, 8, 128), np.float32)
    for e in range(8):
        sel[e, e, :] = 1.0
    sh["sel"] = sel
    sh["w_ada"] = f(inp["w_ada"])
    sh["e_w_in"] = f(inp["e_w_in"][0])
    sh["e_w_out"] = f(inp["e_w_out"][0])
    sh["e_ffn_w1"] = f(inp["e_ffn_w1"][0])
    sh["e_ffn_w3"] = f(inp["e_ffn_w3"][0])
    sh["e_ffn_w2"] = f(inp["e_ffn_w2"][0])
    perm = np.arange(64) ^ 16
    wi = inp["o_w_in"][0]
    kp = wi[:, 640:704]
    sh["o_w_in_x"] = f(np.concatenate([wi[:, :640], kp, kp, kp[:, perm], kp[:, perm]], axis=1))
    wq = inp["o_w_uq"][0].reshape(384, 8, 192)
    nope = wq[:, :, :128].reshape(384, 1024)
    rp = wq[:, :, 128:]
    sh["o_wuq"] = f(np.concatenate([nope, rp.reshape(384, 512), rp[:, :, perm].reshape(384, 512)], axis=1))
    wkv = inp["o_w_ukv"][0].reshape(256, 8, 256)
    sh["o_wukv"] = f(np.concatenate([wkv[:, :, :128].reshape(256, 1024), wkv[:, :, 128:].reshape(256, 1024)], axis=1))
    sh["o_w_o"] = f(inp["o_w_o"][0])
    sh["o_router"] = f(inp["o_router"][0])
    w1 = np.asarray(inp["o_exp_w1"][0], np.float32).reshape(8, 8, 128, 7, 512)
    sh["xw1r"] = np.ascontiguousarray(w1.transpose(0, 3, 2, 1, 4)).reshape(8 * 7 * 128 * 2, 2048)
    w3 = np.asarray(inp["o_exp_w3"][0], np.float32).reshape(8, 8, 128, 7, 512)
    sh["xw3r"] = np.ascontiguousarray(w3.transpose(0, 3, 2, 1, 4)).reshape(8 * 7 * 128 * 2, 2048)
    w2 = np.asarray(inp["o_exp_w2"][0], np.float32).reshape(8, 7, 4, 128, 1024)
    sh["xw2r"] = np.ascontiguousarray(w2.transpose(0, 1, 3, 2, 4)).reshape(8 * 7 * 128 * 2, 2048)
    tid = ((np.arange(32)[None, :, None] * 128 + np.arange(128)[:, None, None]) * 2 + np.arange(2)[None, None, :]).reshape(128, 64)
    sh["tokid"] = np.ascontiguousarray(np.repeat(tid[:, :, None], 16, axis=2).astype(np.int32))
    sh["oobfill"] = np.ascontiguousarray(np.repeat((2 * T + np.arange(128 * 96, dtype=np.int32)).reshape(128, 96, 1), 16, axis=2).reshape(128, 96 * 16))
    sh["utri"] = np.triu(np.ones((128, 128), np.float32), 1)
    sh["ibase"] = (np.arange(7, dtype=np.float32)[None, :] * 256 + 2 * np.arange(128, dtype=np.float32)[:, None]).astype(np.float32)
    return sh


def _prep_vecs(inp, b):
    v = np.zeros((128, NV), np.float32)

    def put(name, arr):
        arr = np.asarray(arr, np.float32).reshape(128, -1)
        v[:, VOFF[name]:VOFF[name] + arr.shape[1]] = arr
    cp = lambda a: np.asarray(a, np.float32).reshape(-1, 128).T
    put("sc", np.stack([cp(inp["c"][b]), cp(inp["c_ctx"])], axis=-1))
    ba = np.asarray(inp["b_ada"], np.float32).reshape(2, 6, 8, 128).transpose(3, 0, 1, 2)
    put("bada", np.repeat(ba[..., None], 2, axis=-1))
    ng = np.asarray(inp["norm_g"], np.float32).reshape(2, 2, 8, 128).transpose(3, 0, 1, 2)
    put("ng", np.repeat(ng[..., None], 2, axis=-1))
    put("convw", np.asarray(inp["e_conv_w"][0], np.float32).reshape(31, 4, 128).transpose(2, 1, 0))
    put("convb", cp(inp["e_conv_b"][0]))
    put("lnag", cp(inp["e_ln_a_g"][0]))
    put("lnab", cp(inp["e_ln_a_b"][0]))
    put("gq", cp(inp["o_g_q"][0]))
    put("gkv", cp(inp["o_g_kv"][0]))
    put("fg", cp(inp["final_g"]))
    return v


def run(inputs, stop_after="D", cores=None):
    inp = {k: np.asarray(v) for k, v in inputs.items()}
    cores = list(range(NCORES)) if cores is None else cores
    sh = _prep_shared(inp)
    in_maps = []
    for b in cores:
        m = dict(sh)
        m["xT"] = np.ascontiguousarray(inp["x"][b].T.astype(np.float32))
        m["cT"] = np.ascontiguousarray(inp["ctx"][b].T.astype(np.float32))
        m["vecs"] = _prep_vecs(inp, b)
        in_maps.append(m)
    nc = build(stop_after)
    res = run_bass_kernel_spmd(nc, in_maps, core_ids=list(range(len(cores))))
    if DEBUG:
        for n in DBG_NAMES:
            DBG_OUT[n] = np.asarray(res.results[0][n])
    return np.stack([np.ascontiguousarray(r["outT"].T) for r in res.results], axis=0)


def kernel(**inputs):
    return run(inputs).astype(np.float32)
```
